# Optimizing a Trainium2 kernel written in Bass

```python
import jax
import jax.numpy as jnp
from jax import lax
import numpy as np


D_MODEL = 2048
BATCH = 8
SEQ = 2048
DEPTH = 2

HEAD_DIM = 64
GM_HEADS = 8
GM_WIDTH = GM_HEADS * HEAD_DIM
CHUNK = 128
RW_HEADS = 8
RW_WIDTH = RW_HEADS * HEAD_DIM
DECAY_LORA = 64
AAA_LORA = 64
GATE_LORA = 128
SB_HEADS = 16
SB_WIDTH = SB_HEADS * HEAD_DIM
Q_BLOCK = 128
N_BRANCH = 3
D_FF = 5632
N_EXPERTS = 8
TOP_K = 2
D_FF_EXPERT = D_FF // TOP_K
N_DENSE = (DEPTH + 1) // 2
N_MOE = DEPTH // 2
RMS_EPS = 1e-6
LN_EPS = 1e-5
RW_GN_EPS = 64e-5

GM_COLS = 2 * GM_WIDTH
RW_COLS = 3 * RW_WIDTH + DECAY_LORA + AAA_LORA + GATE_LORA
SB_COLS = 3 * SB_WIDTH
GATE_COLS = N_BRANCH * D_MODEL
GM_OFF = 0
RW_OFF = GM_OFF + GM_COLS
SB_OFF = RW_OFF + RW_COLS
GATE_OFF = SB_OFF + SB_COLS
N_IN = GATE_OFF + GATE_COLS
RW_SPLITS = [RW_WIDTH, 2 * RW_WIDTH, 3 * RW_WIDTH, 3 * RW_WIDTH + DECAY_LORA, 3 * RW_WIDTH + DECAY_LORA + AAA_LORA]

F32 = jnp.float32

kernel_name = "hybrid_gmlp_rwkv7_stickbreak_moe_block"


def rmsnorm(x, g):
    xf = x.astype(F32)
    y = xf * lax.rsqrt(jnp.mean(xf * xf, axis=-1, keepdims=True) + RMS_EPS)
    return (y * g.astype(F32)).astype(x.dtype)


def layernorm(x, g, b, eps):
    xf = x.astype(F32)
    mu = jnp.mean(xf, axis=-1, keepdims=True)
    var = jnp.mean(jnp.square(xf - mu), axis=-1, keepdims=True)
    return ((xf - mu) * lax.rsqrt(var + eps) * g.astype(F32) + b.astype(F32)).astype(x.dtype)


def swiglu(h, w1, w3, w2):
    return (jax.nn.silu(h @ w1) * (h @ w3)) @ w2


def chunked_spatial_gating(u, v, ln_g, ln_b, w_s, b_s):
    B, T, _ = u.shape
    n_chunks = T // CHUNK
    u = jax.nn.gelu(u)
    v = layernorm(jax.nn.gelu(v), ln_g, ln_b, LN_EPS)
    causal = jnp.tril(jnp.ones((CHUNK, CHUNK), dtype=bool))
    w = jnp.where(causal, w_s, jnp.zeros_like(w_s))
    vc = v.reshape(B, n_chunks, CHUNK, GM_HEADS, HEAD_DIM)
    mixed = jnp.einsum('hts,bcshd->bcthd', w, vc) + b_s.T[None, None, :, :, None]
    return u * mixed.reshape(B, T, GM_WIDTH)


def rwkv7_time_mix(p, mu, w0, w2, a0, a2, g2, k_k, k_a, r_k, ln_g, ln_b):
    B, T, _ = p.shape
    p_prev = jnp.pad(p, ((0, 0), (1, 0), (0, 0)))[:, :-1]
    p = p + (p_prev - p) * mu
    r, k, v, wl, al, gl = jnp.split(p, RW_SPLITS, axis=-1)
    d = (w0 + jnp.tanh(wl) @ w2).astype(F32)
    log_w = -jnp.exp(-jax.nn.softplus(-d) - 0.5)
    a = jax.nn.sigmoid(a0 + al @ a2)
    g = jax.nn.sigmoid(gl) @ g2

    def heads(t):
        return t.reshape(B, T, RW_HEADS, HEAD_DIM).astype(F32)

    r, k, v, a, decay = heads(r), heads(k), heads(v), heads(a), heads(jnp.exp(log_w))
    kk = k * k_k.reshape(RW_HEADS, HEAD_DIM).astype(F32)
    kk = kk / jnp.maximum(jnp.sqrt(jnp.sum(kk * kk, axis=-1, keepdims=True)), 1e-12)
    k = k * (1.0 + (a - 1.0) * k_a.reshape(RW_HEADS, HEAD_DIM).astype(F32))

    def step(S, inp):
        r_t, k_t, v_t, kk_t, a_t, w_t = inp
        sa = jnp.einsum('bhvk,bhk->bhv', S, -kk_t)
        S = (S * w_t[:, :, None, :]
             + sa[..., None] * (kk_t * a_t)[:, :, None, :]
             + v_t[..., None] * k_t[:, :, None, :])
        y_t = jnp.einsum('bhvk,bhk->bhv', S, r_t)
        return S, y_t

    def seq_first(t):
        return jnp.moveaxis(t, 1, 0)

    S0 = jnp.zeros((B, RW_HEADS, HEAD_DIM, HEAD_DIM), F32)
    _, y = lax.scan(step, S0, (seq_first(r), seq_first(k), seq_first(v),
                               seq_first(kk), seq_first(a), seq_first(decay)))
    y = jnp.moveaxis(y, 0, 1)
    m = jnp.mean(y, axis=-1, keepdims=True)
    var = jnp.mean(jnp.square(y - m), axis=-1, keepdims=True)
    y = ((y - m) * lax.rsqrt(var + RW_GN_EPS)).reshape(B, T, RW_WIDTH) * ln_g.astype(F32) + ln_b.astype(F32)
    bonus = jnp.sum(r * k * r_k.astype(F32), axis=-1, keepdims=True) * v
    y = (y + bonus.reshape(B, T, RW_WIDTH)) * g.astype(F32)
    return y.astype(p.dtype)


def stick_breaking_attention(q, k, v):
    B, T, H, Dh = q.shape
    scale = Dh ** -0.5
    outs = []
    for i in range(T // Q_BLOCK):
        q0, q1 = i * Q_BLOCK, (i + 1) * Q_BLOCK
        qb, kb, vb = q[:, q0:q1], k[:, :q1], v[:, :q1]
        z = jnp.einsum('bthd,bshd->bhts', qb, kb, preferred_element_type=F32) * scale
        t_pos = q0 + jnp.arange(Q_BLOCK)[:, None]
        s_pos = jnp.arange(q1)[None, :]
        causal = s_pos < t_pos
        log_not = jnp.where(causal, jax.nn.log_sigmoid(-z), 0.0)
        after = lax.cumsum(log_not, axis=3, reverse=True) - log_not
        att = jnp.where(causal, jnp.exp(jax.nn.log_sigmoid(z) + after), 0.0)
        outs.append(jnp.einsum('bhts,bshd->bthd', att.astype(v.dtype), vb))
    return jnp.concatenate(outs, axis=1)


def moe_swiglu(h, router_w, router_b, w1, w3, w2):
    B, T, Dm = h.shape
    hf = h.reshape(B * T, Dm)
    logits = (hf @ router_w).astype(F32) + router_b.astype(F32)
    top_val, top_idx = lax.top_k(logits, TOP_K)
    top_p = jax.nn.softmax(top_val, axis=-1)
    combine = jnp.sum(jax.nn.one_hot(top_idx, N_EXPERTS, dtype=F32) * top_p[..., None], axis=1)
    out = jnp.zeros_like(hf)
    for e in range(N_EXPERTS):
        out = out + combine[:, e:e + 1].astype(h.dtype) * swiglu(hf, w1[e], w3[e], w2[e])
    return out.reshape(B, T, Dm)


def setup_inputs(seed: int = 0) -> dict:
    key = jax.random.key(seed)
    keys = jax.random.split(key, 40)
    counter = [0]

    def nxt():
        kk = keys[counter[0]]
        counter[0] += 1
        return kk

    def nrm(shape, scale):
        return scale * jax.random.normal(nxt(), shape, F32)

    L = DEPTH
    return {
        "x": nrm((BATCH, SEQ, D_MODEL), 1.0),
        "norm_mix": 1.0 + nrm((L, D_MODEL), 0.02),
        "w_in": nrm((L, D_MODEL, N_IN), D_MODEL ** -0.5),
        "gate_b": nrm((L, N_BRANCH, D_MODEL), 0.1),
        "gm_ln_g": 1.0 + nrm((L, GM_WIDTH), 0.02),
        "gm_ln_b": nrm((L, GM_WIDTH), 0.02),
        "gm_ws": nrm((L, GM_HEADS, CHUNK, CHUNK), CHUNK ** -0.5),
        "gm_bs": 1.0 + nrm((L, GM_HEADS, CHUNK), 0.02),
        "rw_mu": jax.random.uniform(nxt(), (L, RW_COLS), F32),
        "rw_w0": jax.random.uniform(nxt(), (L, RW_WIDTH), F32, -6.0, 1.0),
        "rw_w2": nrm((L, DECAY_LORA, RW_WIDTH), 0.5 * DECAY_LORA ** -0.5),
        "rw_a0": nrm((L, RW_WIDTH), 0.1),
        "rw_a2": nrm((L, AAA_LORA, RW_WIDTH), 0.5 * AAA_LORA ** -0.5),
        "rw_g2": nrm((L, GATE_LORA, RW_WIDTH), GATE_LORA ** -0.5),
        "rw_kk": 0.85 + nrm((L, RW_WIDTH), 0.05),
        "rw_ka": 1.0 + nrm((L, RW_WIDTH), 0.05),
        "rw_rk": nrm((L, RW_HEADS, HEAD_DIM), 0.1),
        "rw_ln_g": 1.0 + nrm((L, RW_WIDTH), 0.02),
        "rw_ln_b": nrm((L, RW_WIDTH), 0.02),
        "p_gm": nrm((L, GM_WIDTH, D_MODEL), GM_WIDTH ** -0.5),
        "p_rw": nrm((L, RW_WIDTH, D_MODEL), RW_WIDTH ** -0.5),
        "p_sb": nrm((L, SB_WIDTH, D_MODEL), SB_WIDTH ** -0.5),
        "w_o": nrm((L, D_MODEL, D_MODEL), D_MODEL ** -0.5),
        "norm_ffn": 1.0 + nrm((L, D_MODEL), 0.02),
        "ffn_w1": nrm((N_DENSE, D_MODEL, D_FF), D_MODEL ** -0.5),
        "ffn_w3": nrm((N_DENSE, D_MODEL, D_FF), D_MODEL ** -0.5),
        "ffn_w2": nrm((N_DENSE, D_FF, D_MODEL), D_FF ** -0.5),
        "router_w": nrm((N_MOE, D_MODEL, N_EXPERTS), D_MODEL ** -0.5),
        "router_b": nrm((N_MOE, N_EXPERTS), 0.01),
        "moe_w1": nrm((N_MOE, N_EXPERTS, D_MODEL, D_FF_EXPERT), D_MODEL ** -0.5),
        "moe_w3": nrm((N_MOE, N_EXPERTS, D_MODEL, D_FF_EXPERT), D_MODEL ** -0.5),
        "moe_w2": nrm((N_MOE, N_EXPERTS, D_FF_EXPERT, D_MODEL), D_FF_EXPERT ** -0.5),
        "norm_out": 1.0 + nrm((D_MODEL,), 0.02),
    }


def reference(x, norm_mix, w_in, gate_b, gm_ln_g, gm_ln_b, gm_ws, gm_bs, rw_mu, rw_w0, rw_w2,
              rw_a0, rw_a2, rw_g2, rw_kk, rw_ka, rw_rk, rw_ln_g, rw_ln_b, p_gm, p_rw, p_sb, w_o,
              norm_ffn, ffn_w1, ffn_w3, ffn_w2, router_w, router_b, moe_w1, moe_w3, moe_w2, norm_out):
    B, T, _ = x.shape
    h = x
    for l in range(DEPTH):
        n = rmsnorm(h, norm_mix[l])
        proj = n @ w_in[l]
        y_gm = chunked_spatial_gating(proj[..., GM_OFF:GM_OFF + GM_WIDTH],
                                      proj[..., GM_OFF + GM_WIDTH:RW_OFF],
                                      gm_ln_g[l], gm_ln_b[l], gm_ws[l], gm_bs[l])
        y_rw = rwkv7_time_mix(proj[..., RW_OFF:SB_OFF], rw_mu[l], rw_w0[l], rw_w2[l], rw_a0[l],
                              rw_a2[l], rw_g2[l], rw_kk[l], rw_ka[l], rw_rk[l], rw_ln_g[l], rw_ln_b[l])
        sbp = proj[..., SB_OFF:GATE_OFF].reshape(B, T, 3, SB_HEADS, HEAD_DIM)
        y_sb = stick_breaking_attention(sbp[:, :, 0], sbp[:, :, 1], sbp[:, :, 2]).reshape(B, T, SB_WIDTH)
        gates = jax.nn.sigmoid(proj[..., GATE_OFF:].reshape(B, T, N_BRANCH, D_MODEL) + gate_b[l])
        merged = (gates[:, :, 0] * (y_gm @ p_gm[l])
                  + gates[:, :, 1] * (y_rw @ p_rw[l])
                  + gates[:, :, 2] * (y_sb @ p_sb[l]))
        h = h + merged @ w_o[l]
        n = rmsnorm(h, norm_ffn[l])
        j = l // 2
        if l % 2 == 0:
            h = h + swiglu(n, ffn_w1[j], ffn_w3[j], ffn_w2[j])
        else:
            h = h + moe_swiglu(n, router_w[j], router_b[j], moe_w1[j], moe_w3[j], moe_w2[j])
    return rmsnorm(h, norm_out)
```

```python
import numpy as np
from contextlib import ExitStack
import concourse.bass as bass
import concourse.mybir as mybir
from concourse.bass_utils import run_bass_kernel_spmd

F32 = mybir.dt.float32
BF16 = mybir.dt.bfloat16
AF = mybir.ActivationFunctionType
ALU = mybir.AluOpType
AX = mybir.AxisListType

NL = 2
D = 2048
T = 2048
DC = 16
NTT = 4
N_IN = 12032
GM_OFF, RW_OFF, SB_OFF, GATE_OFF = 0, 1024, 2816, 5888
D_FF = 5632
FC = 44
NE = 8
D_FE = 2816
FCE = 22
RMS_EPS = 1e-6
LN_EPS = 1e-5
RW_GN_EPS = 64e-5

CP = {}
_o = 0
for _n, _w in [("norm_mix", 16), ("norm_ffn", 16), ("gate_b", 48), ("rw_mu", 14), ("rw_w0", 4), ("rw_a0", 4),
               ("rw_kk", 4), ("rw_ka", 4), ("rw_rk", 4), ("rw_ln_g", 4), ("rw_ln_b", 4), ("norm_out", 16),
               ("rw_1mka", 4)]:
    CP[_n] = _o
    _o += _w
NCP = _o

CS = {}
_o = 0
for _n, _w in [("ident", 128), ("ones", 128), ("blockones", 128), ("m_le", 128), ("m_lt", 128), ("m_ge", 128),
               ("m_gt", 128), ("zeros", 128), ("neg_ge", 128), ("neg_ones", 128),
               ("mask2", 512), ("mask3", 512), ("I8", 512)]:
    CS[_n] = _o
    _o += _w
NCST = _o


def make_consts():
    c = np.zeros((128, NCST), np.float32)
    p = np.arange(128)[:, None]
    f = np.arange(128)[None, :]
    c[:, CS["ident"]:CS["ident"] + 128] = (p == f)
    c[:, CS["ones"]:CS["ones"] + 128] = 1.0
    c[:, CS["blockones"]:CS["blockones"] + 128] = ((p // 64) == (f // 64))
    c[:, CS["m_le"]:CS["m_le"] + 128] = (p <= f)
    c[:, CS["m_lt"]:CS["m_lt"] + 128] = (p < f)
    c[:, CS["m_ge"]:CS["m_ge"] + 128] = (p >= f)
    c[:, CS["m_gt"]:CS["m_gt"] + 128] = (p > f)
    c[:, CS["neg_ge"]:CS["neg_ge"] + 128] = -1.0 * (p >= f)
    c[:, CS["neg_ones"]:CS["neg_ones"] + 128] = -1.0
    p64 = np.arange(128)[:, None] % 64
    f64 = np.arange(64)[None, :]
    lt = (p64 < f64).astype(np.float32)
    le = (p64 <= f64).astype(np.float32)
    gt = (p64 > f64).astype(np.float32)
    eq = (p64 == f64).astype(np.float32)
    c[:, CS["mask2"]:CS["mask2"] + 512] = np.tile(np.concatenate([lt, le], axis=1), (1, 4))
    c[:, CS["mask3"]:CS["mask3"] + 512] = np.tile(gt, (1, 8))
    c[:, CS["I8"]:CS["I8"] + 512] = np.tile(eq, (1, 8))
    return c


class Buf:
    __slots__ = ("name", "w", "r")

    def __init__(self, name=""):
        self.name = name
        self.w = []
        self.r = []


def _prune(toks):
    d = {}
    for s, v in toks:
        if d.get(s, 0) < v:
            d[s] = v
    return list(d.items())


class Sched:
    NRING = 20

    def __init__(self, nc, same_eng_sync=True):
        self.nc = nc
        self.engs = {"pe": nc.tensor, "act": nc.scalar, "dve": nc.vector, "pool": nc.gpsimd, "sp": nc.sync}
        self.sems = []
        self.esem = {}
        self.ecnt = {}
        self.known = {}
        for e in self.engs:
            self.esem[e] = self._new_sem("e_" + e)
            self.ecnt[e] = 0
            self.known[e] = {}
        self.ring = {}
        self.dcnt = {}
        self.semval = {}
        for e in ("sp", "act", "pool"):
            self.ring[e] = [self._new_sem("d_%s%d" % (e, i)) for i in range(self.NRING)]
            self.dcnt[e] = 0
            for s in self.ring[e]:
                self.semval[s] = 0
        self.same = same_eng_sync
        self.n_inst = 0
        self.max_pool_inflight = 4
        self.pool_hist = []

    def _new_sem(self, name):
        h = self.nc.semaphore(name).__enter__()
        self.sems.append(h)
        return len(self.sems) - 1

    def _wait(self, e, toks):
        eng = self.engs[e]
        kn = self.known[e]
        own = self.esem[e]
        for s, v in _prune(toks):
            if kn.get(s, 0) >= v:
                continue
            if s == own and (e == "pe" or not self.same):
                continue
            eng.wait_ge(self.sems[s], v)
            kn[s] = v

    def _deps(self, reads, writes):
        toks = []
        for b in reads:
            toks += b.w
        for b in writes:
            toks += b.w
            toks += b.r
        return toks

    def _commit(self, tok, reads, writes):
        for b in writes:
            b.w = [tok]
            b.r = []
        for b in reads:
            if b not in writes:
                b.r = _prune(b.r + [tok])

    def op(self, e, fn, reads=(), writes=()):
        self._wait(e, self._deps(reads, writes))
        inst = fn(self.engs[e])
        self.ecnt[e] += 1
        tok = (self.esem[e], self.ecnt[e])
        inst.then_inc(self.sems[tok[0]], 1)
        self._commit(tok, reads, writes)
        self.n_inst += 1
        return tok

    def mm(self, out, lhsT, rhs, start, stop, reads=(), writes=(), **kw):
        return self.op("pe", lambda en: en.matmul(out, lhsT, rhs, start=start, stop=stop, **kw), reads, writes)

    def dma(self, q, out, in_, reads=(), writes=(), **kw):
        toks = self._deps(reads, writes)
        i = self.dcnt[q] % self.NRING
        self.dcnt[q] += 1
        s = self.ring[q][i]
        prev = self.semval[s]
        if prev > 0:
            toks.append((s, prev))
        if q == "pool" and self.max_pool_inflight:
            hist = self.pool_hist
            if len(hist) >= self.max_pool_inflight:
                toks.append(hist[-self.max_pool_inflight])
        self._wait(q, toks)
        inst = self.engs[q].dma_start(out=out, in_=in_, **kw)
        inst.then_inc(self.sems[s], 16)
        self.semval[s] = prev + 16
        tok = (s, prev + 16)
        if q == "pool":
            self.pool_hist.append(tok)
        self._commit(tok, reads, writes)
        self.n_inst += 1
        return tok

    def all_tokens(self):
        toks = [(self.esem[e], self.ecnt[e]) for e in self.engs if self.ecnt[e] > 0]
        toks += [(s, v) for s, v in self.semval.items() if v > 0]
        return toks

    def barrier(self, engines=None):
        toks = self.all_tokens()
        for e in (engines or self.engs):
            kn = self.known[e]
            eng = self.engs[e]
            for s, v in toks:
                if s == self.esem[e]:
                    continue
                if kn.get(s, 0) >= v:
                    continue
                eng.wait_ge(self.sems[s], v)
                kn[s] = v


class Builder:
    def __init__(self, opts=None):
        self.o = dict(layers=NL, mixer=True, ffn=True, gm=True, rw=True, sb=True, dbg=())
        if opts:
            self.o.update(opts)
        self.nc = bass.Bass("TRN2", target_bir_lowering=False, dynamic_dma_scratch_size=8192)
        self.ctx = []

    def sb(self, name, shape, dt):
        t = self.nc.sbuf_tensor("s_" + name, shape, dt)
        h = t.__enter__()
        return h

    def tmp(self, name, shape, dt):
        self._uid = getattr(self, "_uid", 0) + 1
        return self.nc.sbuf_tensor("%s_%d" % (name, self._uid), shape, dt)

    def din(self, name, shape, dt=F32):
        return self.nc.dram_tensor(name, list(shape), dt, kind="ExternalInput").ap()

    def build(self):
        nc = self.nc
        o = self.o
        S = self.S = Sched(nc)
        I = self.I = {}
        I["x"] = self.din("x", [T, D])
        I["cst"] = self.din("cst", [128, NCST])
        I["cpk"] = self.din("cpk", [NL, 128, NCP])
        I["w_in"] = self.din("w_in", [NL, N_IN // 128, 128, D])
        I["gm_ln_g"] = self.din("gm_ln_g", [NL, 512])
        I["gm_ln_b"] = self.din("gm_ln_b", [NL, 512])
        I["gm_ws"] = self.din("gm_ws", [NL, 8, 128, 128])
        I["gm_bs"] = self.din("gm_bs", [NL, 1024])
        I["rw_w2"] = self.din("rw_w2", [NL, 64, 512])
        I["rw_a2"] = self.din("rw_a2", [NL, 64, 512])
        I["rw_g2"] = self.din("rw_g2", [NL, 128, 512])
        I["p_gm"] = self.din("p_gm", [NL, DC, 128, 512])
        I["p_rw"] = self.din("p_rw", [NL, DC, 128, 512])
        I["p_sb"] = self.din("p_sb", [NL, DC, 128, 1024])
        I["w_o"] = self.din("w_o", [NL, DC, 128, D])
        I["ffn_w1"] = self.din("ffn_w1", [1, FC, 128, D])
        I["ffn_w3"] = self.din("ffn_w3", [1, FC, 128, D])
        I["ffn_w2"] = self.din("ffn_w2", [1, DC, 128, D_FF])
        I["router_w"] = self.din("router_w", [1, D, NE])
        I["router_b"] = self.din("router_b", [1, NE])
        I["moe_w1"] = self.din("moe_w1", [1, NE, FCE, 128, D])
        I["moe_w3"] = self.din("moe_w3", [1, NE, FCE, 128, D])
        I["moe_w2"] = self.din("moe_w2", [1, NE, DC, 128, D_FE])
        self.out = nc.dram_tensor("out", [T, D], F32, kind="ExternalOutput").ap()
        self.hT = nc.dram_tensor("hT_scr", [DC, 128, T], F32, kind="Internal").ap()
        self.mT = nc.dram_tensor("mT_scr", [DC, 128, T], BF16, kind="Internal").ap()
        self.hT_b = [[Buf("hT%d_%d" % (dc, tt)) for tt in range(NTT)] for dc in range(DC)]
        self.mT_b = [Buf("mT%d" % dc) for dc in range(DC)]
        self.dbg_out = {}
        for name, shape, dt in o["dbg"]:
            self.dbg_out[name] = nc.dram_tensor("dbg_" + name, list(shape), dt, kind="ExternalOutput").ap()

        self.cst = self.sb("cst", [128, NCST], F32)
        self.cstb = self.sb("cstb", [128, NCST], BF16)
        self.cpk = self.sb("cpk", [128, NL, NCP], F32)
        self.nT = self.sb("nT", [128, DC, T], BF16)
        self.nT_b = [Buf("nT%d" % tt) for tt in range(NTT)]
        self.wbig = [self.sb("wbig%d" % i, [128, 5632], BF16) for i in range(2)]
        self.wbig_b = [Buf("wb%d" % i) for i in range(2)]
        self.wsml = [self.sb("wsml%d" % i, [128, 2048], BF16) for i in range(6)]
        self.wsml_b = [Buf("wsm%d" % i) for i in range(6)]
        self.wi_big = 0
        self.wi_sml = 0
        self.ps = [nc.psum_tensor("psb%d" % i, [128, 512], F32).__enter__() for i in range(8)]
        self.ps_b = [Buf("ps%d" % i) for i in range(8)]
        self.cst_b = Buf("cst")

        self._eps = {}
        for val in (RMS_EPS, LN_EPS, RW_GN_EPS, 1.0):
            t = self.sb("eps%d" % len(self._eps), [128, 1], F32)
            S.op("pool", lambda en, t=t, val=val: en.memset(t[:], float(val)), writes=[self.cst_b])
            self._eps[val] = t
        S.dma("sp", self.cst[:], I["cst"][:, :], writes=[self.cst_b])
        S.dma("sp", self.cpk[:], I["cpk"].rearrange("l p c -> p l c"), writes=[self.cst_b])
        S.op("dve", lambda en: en.tensor_copy(self.cstb[:], self.cst[:]), reads=[self.cst_b], writes=[self.cst_b])
        S.barrier()

        self.phase_input()
        for l in range(o["layers"]):
            if o["mixer"]:
                self.phase_norm(l, "norm_mix")
                self.phase_mixer(l)
            if o["ffn"] and not (o.get("only_moe") and l % 2 == 0):
                if l % 2 == 0:
                    for _rep in range(o.get("ffn_rep", 1)):
                        self.phase_norm(l, "norm_ffn")
                        self.phase_ffn_dense(l // 2)
                else:
                    self.phase_moe(l, l // 2)
        self.phase_output()
        S.barrier()
        return nc

    def C(self, name, n=128, rows=128, bf=True):
        t = self.cstb if bf else self.cst
        return t[0:rows, CS[name]:CS[name] + n]

    def col(self, l, name, j):
        return self.cpk[:, l, CP[name] + j:CP[name] + j + 1]

    def wload(self, src_ap, kc, ncol, extra_reads=()):
        n = kc * ncol
        if n <= 2048:
            i = self.wi_sml % 6
            self.wi_sml += 1
            slot, b = self.wsml[i], self.wsml_b[i]
        else:
            i = self.wi_big % 2
            self.wi_big += 1
            slot, b = self.wbig[i], self.wbig_b[i]
        nblk = ncol // 128
        if nblk == 1:
            dst = slot[:, 0:n]
            view = slot[:, 0:n].rearrange("p (k c) -> p k c", c=128)
        else:
            dst = slot[:, 0:n].rearrange("p (b x) -> p b x", b=nblk)
            view = slot[:, 0:n].rearrange("p (b k c) -> p k b c", b=nblk, c=128)
        self.S.dma("pool", dst, src_ap, reads=list(extra_reads), writes=[b], max_dma_last_dim=8192)
        return view, b

    @staticmethod
    def wsrc(wr, c0, ncol):
        cb = c0 // 128
        nblk = ncol // 128
        if nblk == 1:
            return wr[cb]
        return wr[cb:cb + nblk].rearrange("b p x -> p b x")

    def phase_input(self):
        S = self.S
        nc = self.nc
        with self.tmp("xin", [128, 2, D], F32) as xin, self.tmp("xst", [128, 2, DC, 128], F32) as xst:
            xin_b = [Buf(), Buf()]
            xst_b = [Buf(), Buf()]
            ident = self.C("ident", bf=False)
            for ti in range(16):
                k = ti % 2
                S.dma("sp", xin[:, k, :], self.I["x"][ti * 128:(ti + 1) * 128, :], writes=[xin_b[k]])
                for g in range(4):
                    pb = (ti * 4 + g) % 8
                    for j in range(4):
                        dc = g * 4 + j
                        S.op("pe", lambda en, dc=dc, j=j, pb=pb: en.transpose(
                            self.ps[pb][:, j * 128:(j + 1) * 128], xin[:, k, dc * 128:(dc + 1) * 128], ident),
                            reads=[xin_b[k], self.cst_b], writes=[self.ps_b[pb]])
                    eng = "act" if g % 2 == 0 else "dve"
                    dst = xst[:, k, g * 4:(g + 1) * 4, :]
                    src = self.ps[pb][:, :].rearrange("p (j t) -> p j t", t=128)
                    if eng == "act":
                        S.op("act", lambda en, dst=dst, src=src: en.copy(dst, src), reads=[self.ps_b[pb]],
                             writes=[xst_b[k]])
                    else:
                        S.op("dve", lambda en, dst=dst, src=src: en.tensor_copy(dst, src), reads=[self.ps_b[pb]],
                             writes=[xst_b[k]])
                tt = ti // 4
                S.dma("sp", self.hT.rearrange("d p t -> p d t")[:, :, ti * 128:(ti + 1) * 128], xst[:, k, :, :],
                      reads=[xst_b[k]], writes=[self.hT_b[dc][tt] for dc in range(DC)])
            S.barrier()

    def phase_norm(self, l, gname, router=None):
        S = self.S
        nc = self.nc
        ones = self.C("ones")
        with self.tmp("nh", [128, 1, DC, 512], F32) as nh, self.tmp("nsq", [128, DC, 512], BF16) as nsq, \
                self.tmp("nr", [128, 2, 512], F32) as nr:
            nh_b = [Buf(), Buf()]
            nsq_b = Buf()
            nr_b = [Buf(), Buf()]
            for tt in range(NTT):
                k = 0
                pb = tt % 2
                S.dma("sp", nh[:, k, :, :], self.hT.rearrange("d p t -> p d t")[:, :, tt * 512:(tt + 1) * 512],
                      reads=[self.hT_b[dc][tt] for dc in range(DC)], writes=[nh_b[k]])
                S.op("act", lambda en: en.activation(out=nsq[:], in_=nh[:, k, :, :], func=AF.Square),
                     reads=[nh_b[k]], writes=[nsq_b])
                for dc in range(DC):
                    S.mm(self.ps[pb][:, :], ones, nsq[:, dc, :], dc == 0, dc == DC - 1,
                         reads=[nsq_b, self.cst_b], writes=[self.ps_b[pb]])
                S.op("act", lambda en: en.activation(out=nr[:, k, :], in_=self.ps[pb][:, :], func=AF.Sqrt,
                                                     scale=1.0 / D, bias=self.eps_col(RMS_EPS)),
                     reads=[self.ps_b[pb], self.cst_b], writes=[nr_b[k]])
                S.op("dve", lambda en: en.reciprocal(nr[:, k, :], nr[:, k, :]), reads=[nr_b[k]], writes=[nr_b[k]])
                for dc in range(DC):
                    if router is None:
                        S.op("dve", lambda en, dc=dc: en.scalar_tensor_tensor(
                            out=self.nT[:, dc, tt * 512:(tt + 1) * 512], in0=nh[:, k, dc, :], scalar=self.col(l, gname, dc),
                            in1=nr[:, k, :], op0=ALU.mult, op1=ALU.mult),
                            reads=[nh_b[k], nr_b[k], self.cst_b], writes=[self.nT_b[tt]])
                    else:
                        S.op("dve", lambda en, dc=dc: en.scalar_tensor_tensor(
                            out=nh[:, k, dc, :], in0=nh[:, k, dc, :], scalar=self.col(l, gname, dc),
                            in1=nr[:, k, :], op0=ALU.mult, op1=ALU.mult),
                            reads=[nh_b[k], nr_b[k], self.cst_b], writes=[nh_b[k]])
                        S.op("act", lambda en, dc=dc: en.copy(self.nT[:, dc, tt * 512:(tt + 1) * 512], nh[:, k, dc, :]),
                             reads=[nh_b[k]], writes=[self.nT_b[tt]])
                if router is not None:
                    rw32, rw_b, lg32, lg_b = router
                    pr = 2 + tt % 2
                    for sub in range(4):
                        for dc in range(DC):
                            S.mm(self.ps[pr][:, sub * 8:(sub + 1) * 8], nh[:, k, dc, sub * 128:(sub + 1) * 128], rw32[:, dc, :],
                                 dc == 0, dc == DC - 1, reads=[nh_b[k], rw_b], writes=[self.ps_b[pr]])
                    S.op("dve", lambda en: en.tensor_copy(lg32[:, tt * 4:(tt + 1) * 4, :],
                                                          self.ps[pr][:, 0:32].rearrange("p (s e) -> p s e", e=8)),
                         reads=[self.ps_b[pr]], writes=[lg_b])
            S.barrier()

    def eps_col(self, val):
        return self._eps[val][:, 0:1]

    def resid_add(self, dc, tt, pb, stage, stage_b, scale_ap=None):
        S = self.S
        hsl = self.hT[dc, :, tt * 512:(tt + 1) * 512]
        S.dma("sp", stage, hsl, reads=[self.hT_b[dc][tt]], writes=[stage_b])
        if scale_ap is None:
            S.op("dve", lambda en: en.tensor_tensor(out=stage, in0=self.ps[pb][:, :], in1=stage, op=ALU.add),
                 reads=[self.ps_b[pb], stage_b], writes=[stage_b])
        else:
            sc_ap, sc_b, tmp, tmp_b = scale_ap
            S.op("dve", lambda en: en.tensor_tensor(out=tmp, in0=self.ps[pb][:, :], in1=sc_ap, op=ALU.mult),
                 reads=[self.ps_b[pb], sc_b], writes=[tmp_b])
            S.op("dve", lambda en: en.tensor_tensor(out=stage, in0=tmp, in1=stage, op=ALU.add),
                 reads=[tmp_b, stage_b], writes=[stage_b])
        S.dma("sp", hsl, stage, reads=[stage_b], writes=[self.hT_b[dc][tt]])

    def swiglu_up(self, w1, w3, nfc, gT, gT_b, tok0, ntok):
        S = self.S
        ntt = ntok // 512
        sa, sa_b = self.sw_sa
        if True:
            cnt = 0
            for fc in range(nfc):
                v1, b1 = self.wload(self.wsrc(w1, fc * 128, 128), 16, 128)
                v3, b3 = self.wload(self.wsrc(w3, fc * 128, 128), 16, 128)
                for t in range(ntt):
                    tg = (tok0 + t * 512) // 512
                    p1 = (cnt * 2) % 8
                    p3 = (cnt * 2 + 1) % 8
                    k = cnt % 2
                    cnt += 1
                    rhs = lambda kc: self.nT[:, kc, tok0 + t * 512: tok0 + (t + 1) * 512]
                    for kc in range(DC):
                        S.mm(self.ps[p1][:, :], v1[:, kc, :], rhs(kc), kc == 0, kc == DC - 1,
                             reads=[b1, self.nT_b[tg]], writes=[self.ps_b[p1]])
                    for kc in range(DC):
                        S.mm(self.ps[p3][:, :], v3[:, kc, :], rhs(kc), kc == 0, kc == DC - 1,
                             reads=[b3, self.nT_b[tg]], writes=[self.ps_b[p3]])
                    S.op("act", lambda en: en.activation(out=sa[:, k, :], in_=self.ps[p1][:, :], func=AF.Silu),
                         reads=[self.ps_b[p1]], writes=[sa_b[k]])
                    S.op("dve", lambda en: en.tensor_tensor(out=gT[:, fc, t * 512:(t + 1) * 512], in0=self.ps[p3][:, :],
                                                            in1=sa[:, k, :], op=ALU.mult),
                         reads=[self.ps_b[p3], sa_b[k]], writes=[gT_b])

    def swiglu_down(self, w2, nfc, gT, gT_b, tok0, ntok, scale=None):
        S = self.S
        ntt = ntok // 512
        st, st_b = self.sw_st
        tmp, tmp_b = self.sw_tmp
        if True:
            cnt = 0
            for dc in range(DC):
                v2, b2 = self.wload(self.wsrc(w2, dc * 128, 128), nfc, 128)
                for t in range(ntt):
                    tg = (tok0 + t * 512) // 512
                    pb = cnt % 8
                    k = cnt % 3
                    k2 = cnt % 2
                    cnt += 1
                    for fc in range(nfc):
                        S.mm(self.ps[pb][:, :], v2[:, fc, :], gT[:, fc, t * 512:(t + 1) * 512], fc == 0, fc == nfc - 1,
                             reads=[b2, gT_b], writes=[self.ps_b[pb]])
                    sc = None
                    if scale is not None:
                        sc = (scale[0][:, tok0 + t * 512: tok0 + (t + 1) * 512], scale[1], tmp[:, k2, :], tmp_b[k2])
                    self.resid_add(dc, tg, pb, st[:, k, :], st_b[k], sc)

    def phase_ffn_dense(self, j):
        S = self.S
        w1 = self.I["ffn_w1"][j]
        w3 = self.I["ffn_w3"][j]
        w2 = self.I["ffn_w2"][j]
        with ExitStack() as es:
            gT = es.enter_context(self.tmp("gT", [128, FC, 512], BF16))
            sa = es.enter_context(self.tmp("sw_a", [128, 2, 512], BF16))
            st = es.enter_context(self.tmp("sw_st", [128, 3, 512], F32))
            tmp = es.enter_context(self.tmp("sw_tmp", [128, 2, 512], F32))
            gT_b = Buf("gT")
            self.sw_sa = (sa, [Buf(), Buf()])
            self.sw_st = (st, [Buf(), Buf(), Buf()])
            self.sw_tmp = (tmp, [Buf(), Buf()])
            for tg in range(4):
                if "ffn_up" not in self.o.get("skip", ()):
                    self.swiglu_up(w1, w3, FC, gT, gT_b, tg * 512, 512)
                if "ffn_down" not in self.o.get("skip", ()):
                    self.swiglu_down(w2, FC, gT, gT_b, tg * 512, 512)
            S.barrier()

    def phase_moe(self, l, j):
        S = self.S
        I = self.I
        ident32 = self.C("ident", bf=False)
        ones32 = self.C("ones", bf=False)
        with ExitStack() as es:
            rw32 = es.enter_context(self.tmp("rw32", [128, DC, NE], F32))
            lg32 = es.enter_context(self.tmp("lg32", [128, 16, NE], F32))
            rb = es.enter_context(self.tmp("rb", [128, NE], F32))
            comb = es.enter_context(self.tmp("comb", [128, 16, NE], F32))
            t8 = es.enter_context(self.tmp("t8", [128, 3, 16, NE], F32))
            m12 = es.enter_context(self.tmp("m12", [128, 4, 16], F32))
            rw_b, lg_b, comb_b, t8_b, m_b = Buf("rw32"), Buf("lg32"), Buf("comb"), Buf("t8"), Buf("m12")
            S.dma("sp", rw32[:], I["router_w"][j].rearrange("(k p) e -> p k e", p=128), writes=[rw_b])
            S.dma("sp", rb[:], I["router_b"][j:j + 1, :].broadcast_to([128, NE]), writes=[rw_b])
            self.phase_norm(l, "norm_ffn", router=(rw32, rw_b, lg32, lg_b))

            def dve(fn, reads, writes):
                return S.op("dve", fn, reads, writes)

            bc8 = lambda ap: ap.unsqueeze(2).to_broadcast([128, 16, NE])
            dve(lambda en: en.tensor_tensor(out=lg32[:], in0=lg32[:], in1=rb[:, :].unsqueeze(1).to_broadcast([128, 16, NE]),
                                            op=ALU.add), [lg_b, rw_b], [lg_b])
            dve(lambda en: en.tensor_reduce(out=m12[:, 0, :], in_=lg32[:], axis=AX.X, op=ALU.max), [lg_b], [m_b])
            dve(lambda en: en.tensor_tensor(out=t8[:, 0, :, :], in0=lg32[:], in1=bc8(m12[:, 0, :]), op=ALU.is_equal),
                [lg_b, m_b], [t8_b])
            dve(lambda en: en.scalar_tensor_tensor(out=t8[:, 0, :, :], in0=t8[:, 0, :, :], scalar=-1e30, in1=lg32[:],
                                                   op0=ALU.mult, op1=ALU.add), [t8_b, lg_b], [t8_b])
            dve(lambda en: en.tensor_reduce(out=m12[:, 1, :], in_=t8[:, 0, :, :], axis=AX.X, op=ALU.max), [t8_b, m_b], [m_b])
            dve(lambda en: en.tensor_tensor(out=t8[:, 1, :, :], in0=lg32[:], in1=bc8(m12[:, 1, :]), op=ALU.is_ge),
                [lg_b, m_b, t8_b], [t8_b])
            dve(lambda en: en.tensor_tensor(out=t8[:, 2, :, :], in0=lg32[:], in1=bc8(m12[:, 0, :]), op=ALU.subtract),
                [lg_b, m_b, t8_b], [t8_b])
            S.op("act", lambda en: en.activation(out=t8[:, 2, :, :], in_=t8[:, 2, :, :], func=AF.Exp), [t8_b], [t8_b])
            dve(lambda en: en.tensor_tensor(out=m12[:, 2, :], in0=m12[:, 1, :], in1=m12[:, 0, :], op=ALU.subtract), [m_b], [m_b])
            S.op("act", lambda en: en.activation(out=m12[:, 2, :], in_=m12[:, 2, :], func=AF.Exp), [m_b], [m_b])
            dve(lambda en: en.tensor_scalar(m12[:, 2, :], m12[:, 2, :], 1.0, None, ALU.add), [m_b], [m_b])
            dve(lambda en: en.reciprocal(m12[:, 3, :], m12[:, 2, :]), [m_b], [m_b])
            dve(lambda en: en.tensor_tensor(out=t8[:, 2, :, :], in0=t8[:, 2, :, :], in1=t8[:, 1, :, :], op=ALU.mult), [t8_b], [t8_b])
            dve(lambda en: en.tensor_tensor(out=comb[:], in0=t8[:, 2, :, :], in1=bc8(m12[:, 3, :]), op=ALU.mult),
                [t8_b, m_b], [comb_b])
            if "comb" in self.dbg_out:
                S.dma("sp", self.dbg_out["comb"], comb[:], reads=[comb_b])

            gT = es.enter_context(self.tmp("gTe", [128, FCE, 1024], BF16))
            sa = es.enter_context(self.tmp("sw_a", [128, 2, 512], BF16))
            st = es.enter_context(self.tmp("sw_st", [128, 3, 512], F32))
            tmp = es.enter_context(self.tmp("sw_tmp", [128, 2, 512], F32))
            combB = es.enter_context(self.tmp("combB", [128, T], F32))
            dg = es.enter_context(self.tmp("dg", [128, 2, 128], F32))
            gT_b = Buf("gTe")
            cB_b = Buf("combB")
            dg_b = [Buf(), Buf()]
            self.sw_sa = (sa, [Buf(), Buf()])
            self.sw_st = (st, [Buf(), Buf(), Buf()])
            self.sw_tmp = (tmp, [Buf(), Buf()])
            for e in range(NE):
                for tt in range(NTT):
                    pb = self.bank()
                    for sub in range(4):
                        ti = tt * 4 + sub
                        k = ti % 2
                        dve(lambda en: en.tensor_scalar(dg[:, k, :], ident32, comb[:, ti, e:e + 1], None, ALU.mult),
                            [comb_b, self.cst_b, dg_b[k]], [dg_b[k]])
                        S.mm(self.ps[pb][:, sub * 128:(sub + 1) * 128], ones32, dg[:, k, :], True, True,
                             reads=[dg_b[k], self.cst_b], writes=[self.ps_b[pb]])
                    S.op("act", lambda en: en.copy(combB[:, tt * 512:(tt + 1) * 512], self.ps[pb][:, :]), [self.ps_b[pb]], [cB_b])
                for th in range(2):
                    self.swiglu_up(I["moe_w1"][j, e], I["moe_w3"][j, e], FCE, gT, gT_b, th * 1024, 1024)
                    self.swiglu_down(I["moe_w2"][j, e], FCE, gT, gT_b, th * 1024, 1024, scale=(combB, cB_b))
            S.barrier()

    def evac(self, eng, dst, src, reads, writes, scale=None):
        S = self.S
        if eng == "act":
            if scale is None:
                S.op("act", lambda en: en.copy(dst, src), reads=reads, writes=writes)
            else:
                S.op("act", lambda en: en.mul(dst, src, scale), reads=reads, writes=writes)
        else:
            if scale is None:
                S.op("dve", lambda en: en.tensor_copy(dst, src), reads=reads, writes=writes)
            else:
                S.op("dve", lambda en: en.tensor_scalar(dst, src, scale, None, ALU.mult), reads=reads, writes=writes)

    def phase_mixer(self, l):
        S = self.S
        o = self.o
        with self.tmp("yrwT", [128, 4, T], BF16) as yrwT:
            yrw_b = Buf("yrw")
            if o["rw"]:
                self.phase_rw(l, yrwT, yrw_b)
            else:
                S.op("pool", lambda en: en.memset(yrwT[:], 0.0), writes=[yrw_b])
            S.barrier()
            with self.tmp("ygmT", [128, 4, T], BF16) as ygmT:
                ygm_b = Buf("ygm")
                if o["gm"]:
                    self.phase_gm(l, ygmT, ygm_b)
                else:
                    S.op("pool", lambda en: en.memset(ygmT[:], 0.0), writes=[ygm_b])
                S.barrier()
                with self.tmp("ysbT", [128, 8, T], BF16) as ysbT:
                    ysb_b = Buf("ysb")
                    if o["sb"]:
                        self.phase_sb(l, ysbT, ysb_b)
                    else:
                        S.op("pool", lambda en: en.memset(ysbT[:], 0.0), writes=[ysb_b])
                    S.barrier()
                    for nm, tns in (("ygm", ygmT), ("yrw", yrwT), ("ysb", ysbT)):
                        if nm in self.dbg_out:
                            S.dma("sp", self.dbg_out[nm].rearrange("c p t -> p c t"), tns[:], reads=[ygm_b, yrw_b, ysb_b])
                    if "merge" not in o.get("skip", ()):
                        self.phase_merge(l, ygmT, ygm_b, yrwT, yrw_b, ysbT, ysb_b)
                    S.barrier()
        if "wo" not in o.get("skip", ()):
            self.phase_wo(l)

    def phase_sb(self, l, ysbT, ysb_b):
        S = self.S
        w = self.I["w_in"][l]
        m_lt = self.C("m_lt")
        neg_ge = self.C("neg_ge")
        neg_ones = self.C("neg_ones")
        zeros64 = self.C("zeros", n=64)
        with self.tmp("v_all", [128, 16, 256], BF16) as v_all, self.tmp("qT", [128, T], BF16) as qT, \
                self.tmp("kT", [128, T], BF16) as kT, \
                self.tmp("sp", [128, 3, 512], BF16) as sp, self.tmp("att", [128, 3, 512], BF16) as att, \
                self.tmp("ssum", [128, 512], BF16) as ssum:
            v_b = Buf("v_all")
            q_b = Buf("qT")
            k_b = Buf("kT")
            ez_b = [Buf(), Buf()]
            sp_b = [Buf(), Buf(), Buf()]
            att_b = [Buf(), Buf(), Buf()]
            ss_b = Buf("ssum")
            cnt = 0
            pi = 0
            for hp in range(8):
                if hp % 2 == 0:
                    wv, wb = self.wload(self.wsrc(w, SB_OFF + 2048 + (hp // 2) * 256, 256), 16, 256)
                    for ti in range(16):
                        pb = 6 + cnt % 2
                        cnt += 1
                        for kc in range(DC):
                            S.mm(self.ps[pb][:, 0:256], self.nT[:, kc, ti * 128:(ti + 1) * 128], wv[:, kc, :], kc == 0,
                                 kc == DC - 1, reads=[wb, self.nT_b[ti // 4]], writes=[self.ps_b[pb]])
                        self.evac("act" if cnt % 2 else "dve", v_all[:, ti, :],
                                  self.ps[pb][:, 0:256], [self.ps_b[pb]], [v_b])
                wq, wqb = self.wload(self.wsrc(w, SB_OFF + hp * 128, 128), 16, 128)
                wk, wkb = self.wload(self.wsrc(w, SB_OFF + 1024 + hp * 128, 128), 16, 128)
                for tt in range(NTT):
                    pb = 6 + (tt % 2)
                    for kc in range(DC):
                        S.mm(self.ps[pb][:, :], wq[:, kc, :], self.nT[:, kc, tt * 512:(tt + 1) * 512], kc == 0, kc == DC - 1,
                             reads=[wqb, self.nT_b[tt]], writes=[self.ps_b[pb]])
                    self.evac("dve", qT[:, tt * 512:(tt + 1) * 512], self.ps[pb][:, :], [self.ps_b[pb]], [q_b], scale=0.125)
                for tt in range(NTT):
                    pb = 6 + (tt % 2)
                    for kc in range(DC):
                        S.mm(self.ps[pb][:, :], wk[:, kc, :], self.nT[:, kc, tt * 512:(tt + 1) * 512], kc == 0, kc == DC - 1,
                             reads=[wkb, self.nT_b[tt]], writes=[self.ps_b[pb]])
                    self.evac("dve", kT[:, tt * 512:(tt + 1) * 512], self.ps[pb][:, :], [self.ps_b[pb]], [k_b])
                for h2 in range(2):
                    hb = h2 * 64
                    h = hp * 2 + h2
                    for qt in range(NTT):
                        po = 4 + (pi % 2)
                        S.mm(self.ps[po][hb:hb + 64, :], zeros64, self.nT[:, 0, 0:512], True, False,
                             reads=[self.cst_b, self.nT_b[0]], writes=[self.ps_b[po]])
                        S.op("pool", lambda en: en.memset(ssum[:], 0.0), writes=[ss_b])
                        first = True
                        for kb in range(4 * qt + 3, -1, -1):
                            kl = kb - 4 * qt
                            tq0 = max(kl, 0) * 128
                            i = pi % 3
                            za = (0, 1, 6)[pi % 3]
                            zb = (2, 3, 7)[pi % 3]
                            pi += 1
                            q_ap = qT[hb:hb + 64, qt * 512 + tq0:(qt + 1) * 512]
                            k_ap = kT[hb:hb + 64, kb * 128:(kb + 1) * 128]
                            S.mm(self.ps[za][:, tq0:512], k_ap, q_ap, True, True, reads=[q_b, k_b], writes=[self.ps_b[za]])
                            S.op("act", lambda en: en.activation(out=self.ps[za][:, tq0:512], in_=self.ps[za][:, tq0:512],
                                                                 func=AF.Exp), reads=[self.ps_b[za]], writes=[self.ps_b[za]])
                            S.op("act", lambda en: en.activation(out=sp[:, i, tq0:512], in_=self.ps[za][:, tq0:512], func=AF.Ln,
                                                                 bias=self.eps_col(1.0)),
                                 reads=[self.ps_b[za], self.cst_b], writes=[sp_b[i]])
                            if kl >= 0:
                                S.op("pool", lambda en: en.tensor_tensor(out=sp[:, i, tq0:tq0 + 128], in0=sp[:, i, tq0:tq0 + 128],
                                                                         in1=m_lt, op=ALU.mult),
                                     reads=[sp_b[i], self.cst_b], writes=[sp_b[i]])
                            S.mm(self.ps[zb][:, tq0:512], k_ap, q_ap, True, False, reads=[q_b, k_b], writes=[self.ps_b[zb]])
                            S.mm(self.ps[zb][:, tq0:512], neg_ge, sp[:, i, tq0:512], False, first,
                                 reads=[sp_b[i], self.cst_b], writes=[self.ps_b[zb]])
                            if not first:
                                S.mm(self.ps[zb][:, tq0:512], neg_ones, ssum[:, tq0:512], False, True,
                                     reads=[ss_b, self.cst_b], writes=[self.ps_b[zb]])
                            S.op("act", lambda en: en.activation(out=att[:, i, tq0:512], in_=self.ps[zb][:, tq0:512],
                                                                 func=AF.Exp), reads=[self.ps_b[zb]], writes=[att_b[i]])
                            if kl >= 0:
                                S.op("pool", lambda en: en.tensor_tensor(out=att[:, i, tq0:tq0 + 128], in0=att[:, i, tq0:tq0 + 128],
                                                                         in1=m_lt, op=ALU.mult),
                                     reads=[att_b[i], self.cst_b], writes=[att_b[i]])
                            S.mm(self.ps[po][hb:hb + 64, tq0:512], v_all[:, kb, (h % 4) * 64:(h % 4 + 1) * 64], att[:, i, tq0:512],
                                 False, kb == 0, reads=[v_b, att_b[i]], writes=[self.ps_b[po]])
                            if kb > 0:
                                S.op("pool", lambda en: en.tensor_tensor(out=ssum[:, tq0:512], in0=ssum[:, tq0:512],
                                                                         in1=sp[:, i, tq0:512], op=ALU.add),
                                     reads=[sp_b[i], ss_b], writes=[ss_b])
                            first = False
                        self.evac("dve", ysbT[hb:hb + 64, hp, qt * 512:(qt + 1) * 512], self.ps[po][hb:hb + 64, :],
                                  [self.ps_b[po]], [ysb_b])
            S.barrier()

    def gelu(self, dst, pb, ncol, tmp, tmp_b, writes):
        S = self.S
        src = self.ps[pb][:, 0:ncol]
        S.op("act", lambda en: en.activation(out=tmp, in_=src, func=AF.Square), reads=[self.ps_b[pb]], writes=[tmp_b])
        S.op("dve", lambda en: en.tensor_scalar(tmp, tmp, 0.0713548163, 1.5957691216, ALU.mult, ALU.add),
             reads=[tmp_b], writes=[tmp_b])
        S.op("dve", lambda en: en.tensor_tensor(out=tmp, in0=src, in1=tmp, op=ALU.mult), reads=[tmp_b, self.ps_b[pb]],
             writes=[tmp_b])
        S.op("act", lambda en: en.activation(out=tmp, in_=tmp, func=AF.Sigmoid), reads=[tmp_b], writes=[tmp_b])
        S.op("dve", lambda en: en.tensor_tensor(out=dst, in0=src, in1=tmp, op=ALU.mult), reads=[tmp_b, self.ps_b[pb]],
             writes=writes)

    def phase_gm(self, l, ygmT, ygm_b):
        S = self.S
        w = self.I["w_in"][l]
        I = self.I
        with self.tmp("ug", [128, 4, T], BF16) as ug, self.tmp("vln", [128, 16, 512], BF16) as vln, \
                self.tmp("wsT", [128, 8, 128], BF16) as wsT, self.tmp("lng", [128, 512], F32) as lng, \
                self.tmp("lnb", [128, 512], F32) as lnb, self.tmp("bs32", [1, 1024], F32) as bs32, \
                self.tmp("bsr", [1, 1024], BF16) as bsr, self.tmp("gt", [128, 2, 512], F32) as gt, \
                self.tmp("vg", [128, 2, 512], F32) as vg, self.tmp("ws32", [128, 2, 128], F32) as ws32, \
                self.tmp("st6", [128, 2, 8], F32) as st6:
            ug_b = Buf("ug")
            vln_b = Buf("vln")
            wsT_b = Buf("wsT")
            par_b = Buf("gmpar")
            gt_b = [Buf(), Buf()]
            vg_b = [Buf(), Buf()]
            ws32_b = [Buf(), Buf()]
            st_b = [Buf(), Buf()]
            S.dma("sp", lng[:], I["gm_ln_g"][l:l + 1, :].broadcast_to([128, 512]), writes=[par_b])
            S.dma("sp", lnb[:], I["gm_ln_b"][l:l + 1, :].broadcast_to([128, 512]), writes=[par_b])
            S.dma("sp", bs32[:], I["gm_bs"][l:l + 1, :], writes=[par_b])
            S.op("dve", lambda en: en.tensor_copy(bsr[:], bs32[:]), reads=[par_b], writes=[par_b])
            ident = self.C("ident", bf=False)
            m_le = self.C("m_le", bf=False)
            for h in range(8):
                k = h % 2
                pb = h % 2
                S.dma("sp", ws32[:, k, :], I["gm_ws"][l, h], writes=[ws32_b[k]])
                S.op("pe", lambda en: en.transpose(self.ps[pb][:, 0:128], ws32[:, k, :], ident),
                     reads=[ws32_b[k], self.cst_b], writes=[self.ps_b[pb]])
                S.op("dve", lambda en: en.tensor_tensor(out=wsT[:, h, :], in0=self.ps[pb][:, 0:128], in1=m_le, op=ALU.mult),
                     reads=[self.ps_b[pb], self.cst_b], writes=[wsT_b])
            cnt = 0
            for c in range(4):
                wv, wb = self.wload(self.wsrc(w, GM_OFF + c * 128, 128), 16, 128)
                for tt in range(NTT):
                    pb = 2 + cnt % 6
                    k = cnt % 2
                    cnt += 1
                    for kc in range(DC):
                        S.mm(self.ps[pb][:, :], wv[:, kc, :], self.nT[:, kc, tt * 512:(tt + 1) * 512], kc == 0, kc == DC - 1,
                             reads=[wb, self.nT_b[tt]], writes=[self.ps_b[pb]])
                    self.gelu(ug[:, c, tt * 512:(tt + 1) * 512], pb, 512, gt[:, k, :], gt_b[k], [ug_b])
            wv0, wb0 = self.wload(self.wsrc(w, GM_OFF + 512, 256), 16, 256)
            wv1, wb1 = self.wload(self.wsrc(w, GM_OFF + 768, 256), 16, 256)
            for ti in range(16):
                pb = 2 + cnt % 6
                k = cnt % 2
                cnt += 1
                for cb, (wv, wb) in enumerate(((wv0, wb0), (wv1, wb1))):
                    for kc in range(DC):
                        S.mm(self.ps[pb][:, cb * 256:(cb + 1) * 256], self.nT[:, kc, ti * 128:(ti + 1) * 128], wv[:, kc, :],
                             kc == 0, kc == DC - 1, reads=[wb, self.nT_b[ti // 4]], writes=[self.ps_b[pb]])
                self.gelu(vg[:, k, :], pb, 512, gt[:, k, :], gt_b[k], [vg_b[k]])
                S.op("dve", lambda en: en.bn_stats(st6[:, k, 0:6], vg[:, k, :]), reads=[vg_b[k]], writes=[st_b[k]])
                S.op("dve", lambda en: en.bn_aggr(st6[:, k, 6:8], st6[:, k, 0:6]), reads=[st_b[k]], writes=[st_b[k]])
                S.op("act", lambda en: en.activation(out=st6[:, k, 7:8], in_=st6[:, k, 7:8], func=AF.Sqrt,
                                                     bias=self.eps_col(LN_EPS)), reads=[st_b[k], self.cst_b], writes=[st_b[k]])
                S.op("dve", lambda en: en.reciprocal(st6[:, k, 7:8], st6[:, k, 7:8]), reads=[st_b[k]], writes=[st_b[k]])
                S.op("dve", lambda en: en.tensor_scalar(vg[:, k, :], vg[:, k, :], st6[:, k, 6:7], st6[:, k, 7:8],
                                                        ALU.subtract, ALU.mult), reads=[vg_b[k], st_b[k]], writes=[vg_b[k]])
                S.op("dve", lambda en: en.tensor_tensor(out=vg[:, k, :], in0=vg[:, k, :], in1=lng[:], op=ALU.mult),
                     reads=[vg_b[k], par_b], writes=[vg_b[k]])
                S.op("dve", lambda en: en.tensor_tensor(out=vln[:, ti, :], in0=vg[:, k, :], in1=lnb[:], op=ALU.add),
                     reads=[vg_b[k], par_b], writes=[vln_b])
            ones_row = self.cstb[0:1, CS["ones"]:CS["ones"] + 64]
            for cg in range(4):
                for hp in range(4):
                    pb = 2 + cnt % 6
                    cnt += 1
                    for cl in range(4):
                        c = cg * 4 + cl
                        for h2 in range(2):
                            h = hp * 2 + h2
                            outp = self.ps[pb][h2 * 64:(h2 + 1) * 64, cl * 128:(cl + 1) * 128]
                            S.mm(outp, vln[:, c, h * 64:(h + 1) * 64], wsT[:, h, :], True, False,
                                 reads=[vln_b, wsT_b], writes=[self.ps_b[pb]])
                            S.mm(outp, ones_row, bsr[0:1, h * 128:(h + 1) * 128], False, True,
                                 reads=[par_b, self.cst_b], writes=[self.ps_b[pb]])
                    S.op("dve", lambda en: en.tensor_tensor(out=ygmT[:, hp, cg * 512:(cg + 1) * 512], in0=self.ps[pb][:, :],
                                                            in1=ug[:, hp, cg * 512:(cg + 1) * 512], op=ALU.mult),
                         reads=[self.ps_b[pb], ug_b], writes=[ygm_b])
            S.barrier()

    def phase_merge(self, l, ygmT, ygm_b, yrwT, yrw_b, ysbT, ysb_b):
        S = self.S
        I = self.I
        w = I["w_in"][l]
        with self.tmp("gate", [128, 2, 3, 512], BF16) as gate, self.tmp("mst", [128, 2, T], BF16) as mst, \
                self.tmp("mt", [128, 2, 2, 512], F32) as mt:
            gate_b = [[Buf() for _ in range(3)] for _ in range(2)]
            mst_b = [Buf(), Buf()]
            mt_b = [[Buf(), Buf()], [Buf(), Buf()]]
            cnt = 0
            gi = 0
            for dc in range(DC):
                wg = [self.wload(self.wsrc(w, GATE_OFF + b * 2048 + dc * 128, 128), 16, 128) for b in range(3)]
                wp = [self.wload(self.wsrc(I["p_gm"][l], dc * 128, 128), 4, 128),
                      self.wload(self.wsrc(I["p_rw"][l], dc * 128, 128), 4, 128),
                      self.wload(self.wsrc(I["p_sb"][l], dc * 128, 128), 8, 128)]
                ys = [(ygmT, ygm_b, 4), (yrwT, yrw_b, 4), (ysbT, ysb_b, 8)]
                km = dc % 2
                for tt in range(NTT):
                    kg = gi % 2
                    gi += 1
                    for b in range(3):
                        pb = cnt % 8
                        cnt += 1
                        for kc in range(DC):
                            S.mm(self.ps[pb][:, :], wg[b][0][:, kc, :], self.nT[:, kc, tt * 512:(tt + 1) * 512], kc == 0,
                                 kc == DC - 1, reads=[wg[b][1], self.nT_b[tt]], writes=[self.ps_b[pb]])
                        S.op("act", lambda en: en.activation(out=gate[:, kg, b, :], in_=self.ps[pb][:, :], func=AF.Sigmoid,
                                                             bias=self.col(l, "gate_b", b * 16 + dc)),
                             reads=[self.ps_b[pb], self.cst_b], writes=[gate_b[kg][b]])
                    pbs = []
                    for b in range(3):
                        pb = cnt % 8
                        cnt += 1
                        pbs.append(pb)
                        yt, yb, nk = ys[b]
                        for kc in range(nk):
                            S.mm(self.ps[pb][:, :], wp[b][0][:, kc, :], yt[:, kc, tt * 512:(tt + 1) * 512], kc == 0, kc == nk - 1,
                                 reads=[wp[b][1], yb], writes=[self.ps_b[pb]])
                    t0 = mt[:, kg, 0, :]
                    t1 = mt[:, kg, 1, :]
                    b0, b1 = mt_b[kg]
                    S.op("dve", lambda en: en.tensor_tensor(out=t0, in0=self.ps[pbs[0]][:, :], in1=gate[:, kg, 0, :], op=ALU.mult),
                         reads=[self.ps_b[pbs[0]], gate_b[kg][0]], writes=[b0])
                    S.op("dve", lambda en: en.tensor_tensor(out=t1, in0=self.ps[pbs[1]][:, :], in1=gate[:, kg, 1, :], op=ALU.mult),
                         reads=[self.ps_b[pbs[1]], gate_b[kg][1]], writes=[b1])
                    S.op("dve", lambda en: en.tensor_tensor(out=t0, in0=t0, in1=t1, op=ALU.add), reads=[b0, b1], writes=[b0])
                    S.op("dve", lambda en: en.tensor_tensor(out=t1, in0=self.ps[pbs[2]][:, :], in1=gate[:, kg, 2, :], op=ALU.mult),
                         reads=[self.ps_b[pbs[2]], gate_b[kg][2]], writes=[b1])
                    S.op("dve", lambda en: en.tensor_tensor(out=mst[:, km, tt * 512:(tt + 1) * 512], in0=t0, in1=t1, op=ALU.add),
                         reads=[b0, b1], writes=[mst_b[km]])
                S.dma("sp", self.mT[dc], mst[:, km, :], reads=[mst_b[km]], writes=[self.mT_b[dc]])
            S.barrier()

    def phase_wo(self, l):
        S = self.S
        wo = self.I["w_o"][l]
        with self.tmp("wo_st", [128, 3, 512], F32) as st:
            st_b = [Buf(), Buf(), Buf()]
            for dc in range(DC):
                S.dma("sp", self.nT[:, dc, :], self.mT[dc], reads=[self.mT_b[dc]], writes=self.nT_b)
            cnt = 0
            for dc in range(DC):
                wv, wb = self.wload(self.wsrc(wo, dc * 128, 128), 16, 128)
                for tt in range(NTT):
                    pb = cnt % 8
                    k = cnt % 3
                    cnt += 1
                    for kc in range(DC):
                        S.mm(self.ps[pb][:, :], wv[:, kc, :], self.nT[:, kc, tt * 512:(tt + 1) * 512], kc == 0, kc == DC - 1,
                             reads=[wb, self.nT_b[tt]], writes=[self.ps_b[pb]])
                    self.resid_add(dc, tt, pb, st[:, k, :], st_b[k])
            S.barrier()

    def bank(self):
        self.pbi = getattr(self, "pbi", 0) + 1
        return self.pbi % 8

    def phase_rw(self, l, yrwT, yrw_b):
        S = self.S
        I = self.I
        w = I["w_in"][l]
        TW = 256
        NTW = T // TW
        sdec = -0.6065306597126334
        ps = self.ps
        psb = self.ps_b
        cstb_ = self.cst_b
        blockones = self.C("blockones")
        identb = self.C("ident")
        ident32 = self.cst[0:64, CS["ident"]:CS["ident"] + 64]
        mask2 = self.cst[0:64, CS["mask2"]:CS["mask2"] + 512]
        mask3 = self.cst[0:64, CS["mask3"]:CS["mask3"] + 512]
        I8 = self.cstb[0:64, CS["I8"]:CS["I8"] + 512]

        def dve(fn, reads, writes):
            return S.op("dve", fn, reads, writes)

        def act(fn, reads, writes):
            return S.op("act", fn, reads, writes)

        with ExitStack() as _es:
            lwa = _es.enter_context(self.tmp("lwa", [128, 512], BF16))
            lg = _es.enter_context(self.tmp("lg", [128, 512], BF16))
            l32 = _es.enter_context(self.tmp("l32", [128, 2, 512], F32))
            rmask = _es.enter_context(self.tmp("rmask", [128, TW], F32))
            prevcol = _es.enter_context(self.tmp("prevcol", [128, 16], F32))
            S32 = _es.enter_context(self.tmp("S32", [128, 4, 64], F32))
            Sbf = _es.enter_context(self.tmp("Sbf", [128, 4, 64], BF16))
            gC = _es.enter_context(self.tmp("gC", [128, 4, 32], F32))
            ar = _es.enter_context(self.tmp("ar", [128, 4, 2, TW], BF16))
            bk = _es.enter_context(self.tmp("bk", [128, 4, 2, TW], BF16))
            vb = _es.enter_context(self.tmp("vb", [128, 4, TW], BF16))
            bh = _es.enter_context(self.tmp("bh", [128, 4, TW], BF16))
            kh = _es.enter_context(self.tmp("kh", [128, 4, TW], BF16))
            gT = _es.enter_context(self.tmp("gT", [128, 4, TW], BF16))
            bon = _es.enter_context(self.tmp("bon", [128, 4, TW], BF16))
            ynT = _es.enter_context(self.tmp("ynT", [128, 4, TW], F32))
            ar_bd = _es.enter_context(self.tmp("ar_bd", [128, 4, 2, 2, TW], BF16))
            bt_bd = _es.enter_context(self.tmp("bt_bd", [128, 4, 2, TW], BF16))
            Sbd = _es.enter_context(self.tmp("Sbd", [128, 4, 2, 64], BF16))
            bd_b = Buf("bd")
            par_b = Buf("rwpar")
            pc_b = Buf("prevcol")
            S_b = Buf("S")
            S_gb = [Buf("S0"), Buf("S1")]
            gC_b = Buf("gC")
            ar_b = Buf("ar")
            bk_b = Buf("bk")
            tok_b = Buf("vbbhkh")
            gT_b = Buf("gT")
            bon_b = Buf("bon")
            ynT_b = Buf("ynT")
            S.dma("sp", l32[0:64, 0, :], I["rw_w2"][l], writes=[par_b])
            S.dma("sp", l32[64:128, 0, :], I["rw_a2"][l], writes=[par_b])
            S.dma("sp", l32[:, 1, :], I["rw_g2"][l], writes=[par_b])
            dve(lambda en: en.tensor_copy(lwa[:], l32[:, 0, :]), [par_b], [par_b])
            dve(lambda en: en.tensor_copy(lg[:], l32[:, 1, :]), [par_b], [par_b])
            S.op("pool", lambda en: en.memset(rmask[:], 1.0), writes=[par_b])
            S.op("pool", lambda en: en.memset(rmask[:, :].rearrange("p (c t) -> p c t", t=64)[:, :, 0:1], 0.0), writes=[par_b])
            S.op("pool", lambda en: en.memset(S32[:], 0.0), writes=[S_b, S_gb[0], S_gb[1]])
            S.op("pool", lambda en: en.memset(Sbf[:], 0.0), writes=[S_b, S_gb[0], S_gb[1]])
            S.op("pool", lambda en: en.memset(Sbd[:], 0.0), writes=[S_b, S_gb[0], S_gb[1]])
            S.op("pool", lambda en: en.memset(ar_bd[:], 0.0), writes=[bd_b])
            S.op("pool", lambda en: en.memset(bt_bd[:], 0.0), writes=[bd_b])
            S.op("pool", lambda en: en.memset(prevcol[:], 0.0), writes=[pc_b])

            for ti in range(NTW):
                t0 = ti * TW
                tg = t0 // 512
                with ExitStack() as _es:
                    p32 = _es.enter_context(self.tmp("p32", [128, 2, TW + 1], F32))
                    dd = _es.enter_context(self.tmp("dd", [128, 2, TW], F32))
                    twl = _es.enter_context(self.tmp("twl", [128, TW], BF16))
                    sgl = _es.enter_context(self.tmp("sgl", [128, TW], BF16))
                    r32 = _es.enter_context(self.tmp("r32", [128, TW], F32))
                    k32 = _es.enter_context(self.tmp("k32", [128, TW], F32))
                    v32 = _es.enter_context(self.tmp("v32", [128, TW], F32))
                    sg = _es.enter_context(self.tmp("sg", [128, TW], F32))
                    a32 = _es.enter_context(self.tmp("a32", [128, TW], F32))
                    kk = _es.enter_context(self.tmp("kk", [128, TW], F32))
                    kp = _es.enter_context(self.tmp("kp", [128, TW], F32))
                    bt = _es.enter_context(self.tmp("bt", [128, TW], F32))
                    Lc = _es.enter_context(self.tmp("Lc", [128, TW], F32))
                    E1 = _es.enter_context(self.tmp("E1", [128, TW], F32))
                    E2 = _es.enter_context(self.tmp("E2", [128, TW], F32))
                    x16 = _es.enter_context(self.tmp("x16", [128, TW], BF16))
                    p32_b = [Buf(), Buf()]
                    dd_b = [Buf(), Buf()]
                    lo_b = Buf("lora_in")
                    r_b, k_b, v_b, sg_b, a_b, kk_b, kp_b, bt_b, Lc_b, E1_b, E2_b, x16_b = [Buf() for _ in range(12)]
                    self._shi = 0

                    def shifted(j, dst, dst_b, func=None, rows=None):
                        wv, wb = self.wload(self.wsrc(w, RW_OFF + j * 128, 128), 16, 128)
                        pb = self.bank()
                        q = self._shi % 2
                        self._shi += 1
                        for kc in range(DC):
                            S.mm(ps[pb][:, 0:TW], wv[:, kc, :], self.nT[:, kc, t0:t0 + TW], kc == 0, kc == DC - 1,
                                 reads=[wb, self.nT_b[tg]], writes=[psb[pb]])
                        act(lambda en: en.copy(p32[:, q, 1:TW + 1], ps[pb][:, 0:TW]), [psb[pb]], [p32_b[q]])
                        act(lambda en: en.copy(p32[:, q, 0:1], prevcol[:, j:j + 1]), [pc_b], [p32_b[q]])
                        act(lambda en: en.copy(prevcol[:, j:j + 1], p32[:, q, TW:TW + 1]), [p32_b[q]], [pc_b])
                        dve(lambda en: en.tensor_tensor(out=dd[:, q, :], in0=p32[:, q, 0:TW], in1=p32[:, q, 1:TW + 1],
                                                        op=ALU.subtract), [p32_b[q]], [dd_b[q]])
                        if func is None:
                            dve(lambda en: en.scalar_tensor_tensor(out=dst, in0=dd[:, q, :], scalar=self.col(l, "rw_mu", j),
                                                                   in1=p32[:, q, 1:TW + 1], op0=ALU.mult, op1=ALU.add),
                                [dd_b[q], p32_b[q], cstb_], [dst_b])
                        else:
                            dve(lambda en: en.scalar_tensor_tensor(out=dd[:, q, :], in0=dd[:, q, :], scalar=self.col(l, "rw_mu", j),
                                                                   in1=p32[:, q, 1:TW + 1], op0=ALU.mult, op1=ALU.add),
                                [dd_b[q], p32_b[q], cstb_], [dd_b[q]])
                            for (r0, r1, f) in func:
                                if f is None:
                                    act(lambda en: en.copy(dst[r0:r1, :], dd[r0:r1, q, :]), [dd_b[q]], [dst_b])
                                else:
                                    act(lambda en: en.activation(out=dst[r0:r1, :], in_=dd[r0:r1, q, :], func=f),
                                        [dd_b[q]], [dst_b])

                    shifted(12, twl, lo_b, func=[(0, 64, AF.Tanh), (64, 128, None)])
                    shifted(13, sgl, lo_b, func=[(0, 128, AF.Sigmoid)])
                    for fc in range(4):
                        shifted(fc, r32[:], r_b)
                        shifted(4 + fc, k32[:], k_b)
                        shifted(8 + fc, v32[:], v_b)
                        cs128 = slice(fc * 128, (fc + 1) * 128)
                        pb = self.bank()
                        S.mm(ps[pb][:, 0:TW], lwa[0:64, cs128], twl[0:64, :], True, True, reads=[par_b, lo_b], writes=[psb[pb]])
                        act(lambda en: en.activation(out=sg[:], in_=ps[pb][:, 0:TW], func=AF.Sigmoid,
                                                     bias=self.col(l, "rw_w0", fc)), [psb[pb], cstb_], [sg_b])
                        pb = self.bank()
                        S.mm(ps[pb][:, 0:TW], lwa[64:128, cs128], twl[64:128, :], True, True, reads=[par_b, lo_b], writes=[psb[pb]])
                        act(lambda en: en.activation(out=a32[:], in_=ps[pb][:, 0:TW], func=AF.Sigmoid,
                                                     bias=self.col(l, "rw_a0", fc)), [psb[pb], cstb_], [a_b])
                        pb = self.bank()
                        S.mm(ps[pb][:, 0:TW], lg[:, cs128], sgl[:, :], True, True, reads=[par_b, lo_b], writes=[psb[pb]])
                        act(lambda en: en.copy(gT[:, fc, :], ps[pb][:, 0:TW]), [psb[pb]], [gT_b])
                        dve(lambda en: en.tensor_scalar(kk[:], k32[:], self.col(l, "rw_kk", fc), None, ALU.mult),
                            [k_b, cstb_], [kk_b])
                        act(lambda en: en.activation(out=x16[:], in_=kk[:], func=AF.Square), [kk_b], [x16_b])
                        pb = self.bank()
                        S.mm(ps[pb][:, 0:TW], blockones, x16[:], True, True, reads=[x16_b, cstb_], writes=[psb[pb]])
                        act(lambda en: en.activation(out=E1[:], in_=ps[pb][:, 0:TW], func=AF.Sqrt), [psb[pb]], [E1_b])
                        dve(lambda en: en.tensor_scalar(E1[:], E1[:], 1e-12, None, ALU.max), [E1_b], [E1_b])
                        dve(lambda en: en.reciprocal(E1[:], E1[:]), [E1_b], [E1_b])
                        dve(lambda en: en.tensor_tensor(out=kk[:], in0=kk[:], in1=E1[:], op=ALU.mult), [kk_b, E1_b], [kk_b])
                        dve(lambda en: en.tensor_scalar(kp[:], a32[:], -1.0, self.col(l, "rw_ka", fc), ALU.add, ALU.mult),
                            [a_b, cstb_], [kp_b])
                        dve(lambda en: en.scalar_tensor_tensor(out=kp[:], in0=kp[:], scalar=1.0, in1=k32[:], op0=ALU.add,
                                                               op1=ALU.mult), [kp_b, k_b], [kp_b])
                        dve(lambda en: en.tensor_tensor(out=bt[:], in0=kk[:], in1=a32[:], op=ALU.mult), [kk_b, a_b], [bt_b])
                        dve(lambda en: en.scalar_tensor_tensor(out=x16[:], in0=r32[:], scalar=self.col(l, "rw_rk", fc), in1=kp[:],
                                                               op0=ALU.mult, op1=ALU.mult), [r_b, kp_b, cstb_], [x16_b])
                        pb = self.bank()
                        S.mm(ps[pb][:, 0:TW], blockones, x16[:], True, True, reads=[x16_b, cstb_], writes=[psb[pb]])
                        dve(lambda en: en.tensor_tensor(out=bon[:, fc, :], in0=ps[pb][:, 0:TW], in1=v32[:], op=ALU.mult),
                            [psb[pb], v_b], [bon_b])
                        dve(lambda en: en.tensor_tensor_scan(out=Lc[:], data0=rmask[:], data1=sg[:], initial=0.0,
                                                             op0=ALU.mult, op1=ALU.add), [par_b, sg_b], [Lc_b])
                        Lc3 = Lc[:, :].rearrange("p (c t) -> p c t", t=64)
                        act(lambda en: en.activation(out=gC[:, fc, ti * 4:(ti + 1) * 4], in_=Lc3[:, :, 63], func=AF.Exp,
                                                     scale=sdec), [Lc_b], [gC_b])
                        act(lambda en: en.activation(out=E1[:], in_=Lc[:], func=AF.Exp, scale=sdec), [Lc_b], [E1_b])
                        dve(lambda en: en.tensor_tensor(out=ar[:, fc, 1, :], in0=r32[:], in1=E1[:], op=ALU.mult),
                            [r_b, E1_b], [ar_b])
                        dve(lambda en: en.tensor_tensor(out=E2[:], in0=Lc[:], in1=sg[:], op=ALU.subtract), [Lc_b, sg_b], [E2_b])
                        act(lambda en: en.activation(out=E2[:], in_=E2[:], func=AF.Exp, scale=sdec), [E2_b], [E2_b])
                        dve(lambda en: en.scalar_tensor_tensor(out=ar[:, fc, 0, :], in0=kk[:], scalar=-1.0, in1=E2[:],
                                                               op0=ALU.mult, op1=ALU.mult), [kk_b, E2_b], [ar_b])
                        act(lambda en: en.activation(out=E1[:], in_=Lc[:], func=AF.Exp, scale=-sdec), [Lc_b], [E1_b])
                        dve(lambda en: en.tensor_tensor(out=bk[:, fc, 0, :], in0=bt[:], in1=E1[:], op=ALU.mult),
                            [bt_b, E1_b], [bk_b])
                        dve(lambda en: en.tensor_tensor(out=bk[:, fc, 1, :], in0=kp[:], in1=E1[:], op=ALU.mult),
                            [kp_b, E1_b], [bk_b])
                        for a_i in range(2):
                            for h2 in range(2):
                                S.op("pool", lambda en: en.tensor_copy(ar_bd[h2 * 64:(h2 + 1) * 64, fc, h2, a_i, :],
                                                                       ar[h2 * 64:(h2 + 1) * 64, fc, a_i, :]),
                                     [ar_b], [bd_b])
                        for h2 in range(2):
                            S.op("pool", lambda en: en.tensor_copy(bt_bd[h2 * 64:(h2 + 1) * 64, fc, h2, :],
                                                                   bk[h2 * 64:(h2 + 1) * 64, fc, 0, :]), [bk_b], [bd_b])
                        E23 = E2[:, :].rearrange("p (c t) -> p c t", t=64)
                        dve(lambda en: en.tensor_tensor(out=E23, in0=Lc3[:, :, 63:64].to_broadcast([128, TW // 64, 64]), in1=Lc3,
                                                        op=ALU.subtract), [Lc_b], [E2_b])
                        act(lambda en: en.activation(out=E2[:], in_=E2[:], func=AF.Exp, scale=sdec), [E2_b], [E2_b])
                        dve(lambda en: en.tensor_tensor(out=bh[:, fc, :], in0=bt[:], in1=E2[:], op=ALU.mult),
                            [bt_b, E2_b], [tok_b])
                        dve(lambda en: en.tensor_tensor(out=kh[:, fc, :], in0=kp[:], in1=E2[:], op=ALU.mult),
                            [kp_b, E2_b], [tok_b])
                        act(lambda en: en.copy(vb[:, fc, :], v32[:]), [v_b], [tok_b])
                    S.barrier()
                with ExitStack() as _es:
                    tokm = _es.enter_context(self.tmp("tokm", [64, 2, 4, 256], BF16))
                    A1 = _es.enter_context(self.tmp("A1", [64, 8, 2, 64], BF16))
                    A2 = _es.enter_context(self.tmp("A2", [64, 8, 2, 64], BF16))
                    Nj = _es.enter_context(self.tmp("Nj", [64, 2, 8, 64], BF16))
                    Mj = _es.enter_context(self.tmp("Mj", [64, 2, 8, 64], BF16))
                    Pj = _es.enter_context(self.tmp("Pj", [64, 2, 8, 64], BF16))
                    AhT = _es.enter_context(self.tmp("AhT", [128, 4, 64], BF16))
                    W2 = _es.enter_context(self.tmp("W2", [64, 8, 64], BF16))
                    Uh = _es.enter_context(self.tmp("Uh", [64, 8, 64], F32))
                    Ub = _es.enter_context(self.tmp("Ub", [64, 8, 64], BF16))
                    Y32 = _es.enter_context(self.tmp("Y32", [64, 8, 64], F32))
                    Ysq = _es.enter_context(self.tmp("Ysq", [64, 8, 64], F32))
                    st8 = _es.enter_context(self.tmp("st8", [64, 2, 4, 4], F32))
                    ynT_g = [Buf(), Buf()]
                    if not hasattr(self, "_Sg_b"):
                        pass

                    def stream(hg):
                        tokm_b, A1_b, A2_b, AhT_b, W2_b, Uh_b, Ub_b, Y_b, Ysq_b, st8_b = [Buf() for _ in range(10)]
                        Nj_b = [Buf(), Buf()]
                        Mj_b = [Buf(), Buf()]
                        Pj_b = [Buf(), Buf()]
                        Sg_b = S_gb[hg]
                        hs = list(range(4 * hg, 4 * hg + 4))
                        fcs = [2 * hg, 2 * hg + 1]
                        H0 = 4 * hg
                        f0 = 2 * hg

                        def hv(t3, *idx):
                            return t3

                        for ci in range(TW // 64):
                            cs = ci * 64
                            cg = ti * 4 + ci
                            srcs = [(vb, tok_b, None), (bh, tok_b, None), (kh, tok_b, None), (ar, ar_b, 0)]
                            pb = self.bank()
                            pv = ps[pb][:, :].bitcast(BF16)
                            for ai in range(4):
                                a_t, a_b_, sub = srcs[ai]
                                for fl in range(2):
                                    fc = f0 + fl
                                    src = a_t[:, fc, cs:cs + 64] if sub is None else a_t[:, fc, sub, cs:cs + 64]
                                    S.op("pe", lambda en: en.transpose(pv[0:64, ai * 256 + fl * 128: ai * 256 + (fl + 1) * 128],
                                                                       src, identb), reads=[a_b_, cstb_], writes=[psb[pb]])
                            self.evac("act" if hg == 0 else "dve", tokm[:, hg, :, :],
                                      pv[0:64, :].rearrange("p (a f) -> p a f", f=256), [psb[pb]], [tokm_b])
                            yield
                            Vt, Bt, Kt, At = tokm[:, hg, 0, :], tokm[:, hg, 1, :], tokm[:, hg, 2, :], tokm[:, hg, 3, :]
                            p1 = self.bank()
                            p2 = self.bank()
                            p3 = self.bank()
                            for fl in range(2):
                                fc = f0 + fl
                                rhs_bd = ar_bd[:, fc, :, :, cs:cs + 64]
                                S.mm(ps[p1][0:64, fl * 256:(fl + 1) * 256], bk[:, fc, 0, cs:cs + 64], rhs_bd, True, True,
                                     reads=[bk_b, bd_b], writes=[psb[p1]])
                                S.mm(ps[p2][0:64, fl * 256:(fl + 1) * 256], bk[:, fc, 1, cs:cs + 64], rhs_bd, True, True,
                                     reads=[bk_b, bd_b], writes=[psb[p2]])
                                S.mm(ps[p3][0:64, fl * 128:(fl + 1) * 128], ar[:, fc, 0, cs:cs + 64], bt_bd[:, fc, :, cs:cs + 64],
                                     True, True, reads=[ar_b, bd_b], writes=[psb[p3]])
                            yield
                            g4 = slice(H0, H0 + 4)
                            dve(lambda en: en.tensor_tensor(out=A1[:, g4, :, :].rearrange("p h a t -> p (h a t)"),
                                                            in0=ps[p1][0:64, :], in1=mask2, op=ALU.mult),
                                [psb[p1], cstb_], [A1_b])
                            dve(lambda en: en.tensor_tensor(out=A2[:, g4, :, :].rearrange("p h a t -> p (h a t)"),
                                                            in0=ps[p2][0:64, :], in1=mask2, op=ALU.mult),
                                [psb[p2], cstb_], [A2_b])
                            dve(lambda en: en.tensor_tensor(out=Nj[:, 0, g4, :].rearrange("p h t -> p (h t)"), in0=ps[p3][0:64, 0:256],
                                                            in1=mask3[:, 0:256], op=ALU.mult), [psb[p3], cstb_], [Nj_b[0]])
                            act(lambda en: en.copy(Mj[:, 0, g4, :], A1[:, g4, 0, :]), [A1_b], [Mj_b[0]])
                            dve(lambda en: en.tensor_tensor(out=Pj[:, 0, g4, :].rearrange("p h t -> p (h t)"),
                                                            in0=Mj[:, 0, g4, :].rearrange("p h t -> p (h t)"), in1=I8[:, 0:256],
                                                            op=ALU.add), [Mj_b[0], cstb_], [Pj_b[0]])
                            yield
                            cur = 0
                            pc = 0
                            for j in range(1, 6):
                                nxt = 1 - cur
                                pn = self.bank()
                                for hl, h in enumerate(hs):
                                    S.mm(ps[pn][0:64, hl * 64:(hl + 1) * 64], Mj[:, cur, h, :], Nj[:, cur, h, :], True, True,
                                         reads=[Mj_b[cur], Nj_b[cur]], writes=[psb[pn]])
                                if j < 5:
                                    pm = self.bank()
                                    for hl, h in enumerate(hs):
                                        S.mm(ps[pm][0:64, hl * 64:(hl + 1) * 64], Nj[:, cur, h, :], Mj[:, cur, h, :], True, True,
                                             reads=[Mj_b[cur], Nj_b[cur]], writes=[psb[pm]])
                                yield
                                self.evac("act", Nj[:, nxt, g4, :].rearrange("p h t -> p (h t)"), ps[pn][0:64, 0:256], [psb[pn]],
                                          [Nj_b[nxt]])
                                if j < 5:
                                    self.evac("dve", Mj[:, nxt, g4, :].rearrange("p h t -> p (h t)"), ps[pm][0:64, 0:256], [psb[pm]],
                                              [Mj_b[nxt]])
                                pp = self.bank()
                                for hl, h in enumerate(hs):
                                    S.mm(ps[pp][0:64, hl * 64:(hl + 1) * 64], Nj[:, nxt, h, :], Pj[:, pc, h, :], True, True,
                                         reads=[Nj_b[nxt], Pj_b[pc]], writes=[psb[pp]])
                                yield
                                dve(lambda en: en.tensor_tensor(out=Pj[:, 1 - pc, g4, :].rearrange("p h t -> p (h t)"),
                                                                in0=ps[pp][0:64, 0:256],
                                                                in1=Pj[:, pc, g4, :].rearrange("p h t -> p (h t)"), op=ALU.add),
                                    [psb[pp], Pj_b[pc]], [Pj_b[1 - pc]])
                                pc = 1 - pc
                                cur = nxt
                            TT = Pj[:, pc, :, :]
                            TT_b = Pj_b[pc]
                            pa = self.bank()
                            pw = self.bank()
                            for hl, h in enumerate(hs):
                                fl, hb = hl // 2, (hl % 2) * 64
                                S.mm(ps[pa][hb:hb + 64, fl * 64:(fl + 1) * 64], At[:, hl * 64:(hl + 1) * 64], TT[:, h, :], True, True,
                                     reads=[tokm_b, TT_b], writes=[psb[pa]])
                                S.mm(ps[pw][0:64, hl * 64:(hl + 1) * 64], A2[:, h, 0, :], Vt[:, hl * 64:(hl + 1) * 64], True, True,
                                     reads=[A2_b, tokm_b], writes=[psb[pw]])
                            yield
                            self.evac("act", AhT[:, f0:f0 + 2, :].rearrange("p f t -> p (f t)"), ps[pa][:, 0:128], [psb[pa]], [AhT_b])
                            self.evac("dve", W2[:, g4, :].rearrange("p h v -> p (h v)"), ps[pw][0:64, 0:256], [psb[pw]], [W2_b])
                            pu = self.bank()
                            for hl, h in enumerate(hs):
                                S.mm(ps[pu][0:64, hl * 64:(hl + 1) * 64], TT[:, h, :], W2[:, h, :], True, True,
                                     reads=[TT_b, W2_b], writes=[psb[pu]])
                            yield
                            self.evac("act", Uh[:, g4, :].rearrange("p h v -> p (h v)"), ps[pu][0:64, 0:256], [psb[pu]], [Uh_b])
                            pu2 = self.bank()
                            for fl in range(2):
                                fc = f0 + fl
                                S.mm(ps[pu2][0:64, fl * 128:(fl + 1) * 128], AhT[:, fc, :], Sbd[:, fc, :, :], True, True,
                                     reads=[AhT_b, Sg_b], writes=[psb[pu2]])
                            yield
                            dve(lambda en: en.tensor_tensor(out=Ub[:, g4, :].rearrange("p h v -> p (h v)"), in0=ps[pu2][0:64, 0:256],
                                                            in1=Uh[:, g4, :].rearrange("p h v -> p (h v)"), op=ALU.add),
                                [psb[pu2], Uh_b], [Ub_b])
                            py = self.bank()
                            pS = self.bank()
                            for fl in range(2):
                                fc = f0 + fl
                                S.mm(ps[py][0:64, fl * 128:(fl + 1) * 128], ar[:, fc, 1, cs:cs + 64], Sbd[:, fc, :, :], True, False,
                                     reads=[ar_b, Sg_b], writes=[psb[py]])
                                for hl in (2 * fl, 2 * fl + 1):
                                    h = H0 + hl
                                    yo = ps[py][0:64, hl * 64:(hl + 1) * 64]
                                    S.mm(yo, A1[:, h, 1, :], Ub[:, h, :], False, False, reads=[A1_b, Ub_b], writes=[psb[py]])
                                    S.mm(yo, A2[:, h, 1, :], Vt[:, hl * 64:(hl + 1) * 64], False, hl == 2 * fl + 1,
                                         reads=[A2_b, tokm_b], writes=[psb[py]])
                            for hl, h in enumerate(hs):
                                fl, hb = hl // 2, (hl % 2) * 64
                                so = ps[pS][hb:hb + 64, fl * 64:(fl + 1) * 64]
                                S.mm(so, Bt[:, hl * 64:(hl + 1) * 64], Ub[:, h, :], True, False, reads=[tokm_b, Ub_b], writes=[psb[pS]])
                                S.mm(so, Kt[:, hl * 64:(hl + 1) * 64], Vt[:, hl * 64:(hl + 1) * 64], False, True, reads=[tokm_b],
                                     writes=[psb[pS]])
                            yield
                            for fl in range(2):
                                fc = f0 + fl
                                dve(lambda en: en.scalar_tensor_tensor(out=S32[:, fc, :], in0=S32[:, fc, :], scalar=gC[:, fc, cg:cg + 1],
                                                                       in1=ps[pS][:, fl * 64:(fl + 1) * 64], op0=ALU.mult, op1=ALU.add),
                                    [psb[pS], gC_b, Sg_b], [Sg_b])
                            for h2 in range(2):
                                act(lambda en: en.copy(Sbd[h2 * 64:(h2 + 1) * 64, f0:f0 + 2, h2, :],
                                                       S32[h2 * 64:(h2 + 1) * 64, f0:f0 + 2, :]), [Sg_b], [Sg_b])
                            Yg = Y32[:, g4, :]
                            Ysg = Ysq[:, g4, :]
                            sg8 = st8[:, hg, :, :]
                            act(lambda en: en.copy(Yg.rearrange("p h v -> p (h v)"), ps[py][0:64, 0:256]), [psb[py]], [Y_b])
                            act(lambda en: en.activation(out=Ysg, in_=Yg, func=AF.Square), [Y_b], [Ysq_b])
                            dve(lambda en: en.tensor_reduce(out=sg8[:, 0, :], in_=Yg, axis=AX.X, op=ALU.add), [Y_b], [st8_b])
                            dve(lambda en: en.tensor_reduce(out=sg8[:, 1, :], in_=Ysg, axis=AX.X, op=ALU.add), [Ysq_b, st8_b], [st8_b])
                            dve(lambda en: en.tensor_scalar(sg8[:, 0, :], sg8[:, 0, :], 1.0 / 64, None, ALU.mult), [st8_b], [st8_b])
                            dve(lambda en: en.tensor_tensor(out=sg8[:, 2, :], in0=sg8[:, 0, :], in1=sg8[:, 0, :], op=ALU.mult),
                                [st8_b], [st8_b])
                            dve(lambda en: en.scalar_tensor_tensor(out=sg8[:, 3, :], in0=sg8[:, 1, :], scalar=1.0 / 64, in1=sg8[:, 2, :],
                                                                   op0=ALU.mult, op1=ALU.subtract), [st8_b], [st8_b])
                            act(lambda en: en.activation(out=sg8[:, 3, :], in_=sg8[:, 3, :], func=AF.Sqrt,
                                                         bias=self.eps_col(RW_GN_EPS)[0:64, :]), [st8_b, cstb_], [st8_b])
                            dve(lambda en: en.reciprocal(sg8[:, 3, :], sg8[:, 3, :]), [st8_b], [st8_b])
                            yield
                            dve(lambda en: en.tensor_tensor(out=Yg, in0=Yg,
                                                            in1=sg8[:, 0, :].unsqueeze(2).to_broadcast([64, 4, 64]), op=ALU.subtract),
                                [Y_b, st8_b], [Y_b])
                            dve(lambda en: en.tensor_tensor(out=Yg, in0=Yg,
                                                            in1=sg8[:, 3, :].unsqueeze(2).to_broadcast([64, 4, 64]), op=ALU.mult),
                                [Y_b, st8_b], [Y_b])
                            pt = self.bank()
                            Yf = Y32[:, :, :].rearrange("p h v -> p (h v)")
                            for fl in range(2):
                                fc = f0 + fl
                                S.op("pe", lambda en: en.transpose(ps[pt][:, fl * 64:(fl + 1) * 64], Yf[:, fc * 128:(fc + 1) * 128],
                                                                   ident32), reads=[Y_b, cstb_], writes=[psb[pt]])
                            yield
                            act(lambda en: en.copy(ynT[:, f0:f0 + 2, cs:cs + 64], ps[pt][:, 0:128].rearrange("p (f t) -> p f t", t=64)),
                                [psb[pt]], [ynT_g[hg]])
                            yield

                    gens = [stream(0), stream(1)]
                    alive = [True, True]
                    while any(alive):
                        for gi in range(2):
                            if alive[gi]:
                                try:
                                    next(gens[gi])
                                except StopIteration:
                                    alive[gi] = False
                    for fc in range(4):
                        dve(lambda en: en.tensor_scalar(ynT[:, fc, :], ynT[:, fc, :], self.col(l, "rw_ln_g", fc),
                                                        self.col(l, "rw_ln_b", fc), ALU.mult, ALU.add),
                            [ynT_b, ynT_g[0], ynT_g[1], cstb_], [ynT_b, ynT_g[0], ynT_g[1]])
                        dve(lambda en: en.tensor_tensor(out=ynT[:, fc, :], in0=ynT[:, fc, :], in1=bon[:, fc, :], op=ALU.add),
                            [ynT_b, bon_b], [ynT_b])
                        dve(lambda en: en.tensor_tensor(out=yrwT[:, fc, t0:t0 + TW], in0=ynT[:, fc, :], in1=gT[:, fc, :], op=ALU.mult),
                            [ynT_b, gT_b], [yrw_b])
                    S.barrier()

    def phase_output(self):
        S = self.S
        nc = self.nc
        ones = self.C("ones")
        ident = self.C("ident", bf=False)
        with ExitStack() as _es:
            oh = _es.enter_context(self.tmp("oh", [128, DC, 512], F32))
            osq = _es.enter_context(self.tmp("osq", [128, DC, 512], BF16))
            orr = _es.enter_context(self.tmp("orr", [128, 512], F32))
            oo = _es.enter_context(self.tmp("oo", [128, 2, D], F32))
            oh_b = Buf()
            osq_b = Buf()
            or_b = Buf()
            oo_b = [Buf(), Buf()]
            out_b = Buf()
            for tt in range(NTT):
                pb = 0
                S.dma("sp", oh[:], self.hT.rearrange("d p t -> p d t")[:, :, tt * 512:(tt + 1) * 512],
                      reads=[self.hT_b[dc][tt] for dc in range(DC)], writes=[oh_b])
                S.op("act", lambda en: en.activation(out=osq[:], in_=oh[:], func=AF.Square), reads=[oh_b], writes=[osq_b])
                for dc in range(DC):
                    S.mm(self.ps[pb][:, :], ones, osq[:, dc, :], dc == 0, dc == DC - 1, reads=[osq_b, self.cst_b],
                         writes=[self.ps_b[pb]])
                S.op("act", lambda en: en.activation(out=orr[:], in_=self.ps[pb][:, :], func=AF.Sqrt, scale=1.0 / D,
                                                     bias=self.eps_col(RMS_EPS)),
                     reads=[self.ps_b[pb], self.cst_b], writes=[or_b])
                S.op("dve", lambda en: en.reciprocal(orr[:], orr[:]), reads=[or_b], writes=[or_b])
                for dc in range(DC):
                    S.op("dve", lambda en, dc=dc: en.scalar_tensor_tensor(
                        out=oh[:, dc, :], in0=oh[:, dc, :], scalar=self.col(0, "norm_out", dc), in1=orr[:],
                        op0=ALU.mult, op1=ALU.mult), reads=[oh_b, or_b, self.cst_b], writes=[oh_b])
                for ts in range(4):
                    k = ts % 2
                    for g in range(4):
                        pb2 = 1 + (ts * 4 + g) % 7
                        for jj in range(4):
                            dc = g * 4 + jj
                            S.op("pe", lambda en, dc=dc, jj=jj, pb2=pb2: en.transpose(
                                self.ps[pb2][:, jj * 128:(jj + 1) * 128], oh[:, dc, ts * 128:(ts + 1) * 128], ident),
                                reads=[oh_b, self.cst_b], writes=[self.ps_b[pb2]])
                        dst = oo[:, k, g * 512:(g + 1) * 512]
                        if g % 2 == 0:
                            S.op("act", lambda en, dst=dst, pb2=pb2: en.copy(dst, self.ps[pb2][:, :]),
                                 reads=[self.ps_b[pb2]], writes=[oo_b[k]])
                        else:
                            S.op("dve", lambda en, dst=dst, pb2=pb2: en.tensor_copy(dst, self.ps[pb2][:, :]),
                                 reads=[self.ps_b[pb2]], writes=[oo_b[k]])
                    r0 = tt * 512 + ts * 128
                    S.dma("sp", self.out[r0:r0 + 128, :], oo[:, k, :], reads=[oo_b[k]], writes=[out_b])
            S.barrier()


def _colpack(inp):
    cp = np.zeros((NL, 128, NCP), np.float32)

    def put(name, vec, l):
        v = np.asarray(vec, np.float32).reshape(-1, 128).T
        cp[l, :, CP[name]:CP[name] + v.shape[1]] = v

    for l in range(NL):
        put("norm_mix", inp["norm_mix"][l], l)
        put("norm_ffn", inp["norm_ffn"][l], l)
        put("gate_b", inp["gate_b"][l].reshape(-1), l)
        put("rw_mu", inp["rw_mu"][l], l)
        for n in ("rw_w0", "rw_a0", "rw_kk", "rw_ka", "rw_ln_g", "rw_ln_b"):
            put(n, inp[n][l], l)
        put("rw_rk", inp["rw_rk"][l].reshape(-1), l)
        put("norm_out", inp["norm_out"], l)
    return cp


def _tile_w(w):
    w = np.asarray(w, dtype=np.float32)
    lead = w.shape[:-2]
    K, N = w.shape[-2:]
    w = w.reshape(lead + (K // 128, 128, N // 128, 128))
    nl = len(lead)
    perm = tuple(range(nl)) + (nl + 2, nl + 1, nl + 0, nl + 3)
    return np.ascontiguousarray(w.transpose(perm)).reshape(lead + (N // 128, 128, K))


def make_in_map(inp, b):
    m = {"x": np.ascontiguousarray(inp["x"][b]), "cst": make_consts(), "cpk": _colpack(inp)}
    for n in ("gm_ln_g", "gm_ln_b", "gm_ws", "rw_w2", "rw_a2", "rw_g2", "router_w", "router_b"):
        m[n] = np.ascontiguousarray(inp[n], dtype=np.float32)
    for n in ("w_in", "p_gm", "p_rw", "p_sb", "w_o", "ffn_w1", "ffn_w3", "ffn_w2", "moe_w1", "moe_w3", "moe_w2"):
        m[n] = _tile_w(inp[n])
    m["gm_bs"] = np.ascontiguousarray(inp["gm_bs"], dtype=np.float32).reshape(NL, 1024)
    return m


_NC_CACHE = {}


def kernel(**inputs):
    inp = {k: np.asarray(v) for k, v in inputs.items()}
    if "nc" not in _NC_CACHE:
        _NC_CACHE["nc"] = Builder().build()
    nc = _NC_CACHE["nc"]
    base = make_in_map(inp, 0)
    in_maps = []
    for b in range(8):
        m = dict(base)
        m["x"] = np.ascontiguousarray(inp["x"][b], dtype=np.float32)
        in_maps.append(m)
    res = run_bass_kernel_spmd(nc, in_maps, core_ids=list(range(8)))
    return np.stack([r["out"] for r in res.results], axis=0).astype(np.float32)
```

```python
import numpy as np
from contextlib import ExitStack
import concourse.bass as bass
import concourse.mybir as mybir
from concourse.bass_utils import run_bass_kernel_spmd

F32 = mybir.dt.float32
BF16 = mybir.dt.bfloat16
AF = mybir.ActivationFunctionType
ALU = mybir.AluOpType
AX = mybir.AxisListType

NL = 2
D = 2048
T = 2048
DC = 16
NTT = 4
N_IN = 12032
GM_OFF, RW_OFF, SB_OFF, GATE_OFF = 0, 1024, 2816, 5888
D_FF = 5632
FC = 44
NE = 8
D_FE = 2816
FCE = 22
RMS_EPS = 1e-6
LN_EPS = 1e-5
RW_GN_EPS = 64e-5

CP = {}
_o = 0
for _n, _w in [("norm_mix", 16), ("norm_ffn", 16), ("gate_b", 48), ("rw_mu", 14), ("rw_w0", 4), ("rw_a0", 4),
               ("rw_kk", 4), ("rw_ka", 4), ("rw_rk", 4), ("rw_ln_g", 4), ("rw_ln_b", 4), ("norm_out", 16),
               ("rw_1mka", 4)]:
    CP[_n] = _o
    _o += _w
NCP = _o

CS = {}
_o = 0
for _n, _w in [("ident", 128), ("ones", 128), ("blockones", 128), ("m_le", 128), ("m_lt", 128), ("m_ge", 128),
               ("m_gt", 128), ("zeros", 128), ("neg_ge", 128), ("neg_ones", 128),
               ("mask2", 512), ("mask3", 512), ("I8", 512)]:
    CS[_n] = _o
    _o += _w
NCST = _o


def make_consts():
    c = np.zeros((128, NCST), np.float32)
    p = np.arange(128)[:, None]
    f = np.arange(128)[None, :]
    c[:, CS["ident"]:CS["ident"] + 128] = (p == f)
    c[:, CS["ones"]:CS["ones"] + 128] = 1.0
    c[:, CS["blockones"]:CS["blockones"] + 128] = ((p // 64) == (f // 64))
    c[:, CS["m_le"]:CS["m_le"] + 128] = (p <= f)
    c[:, CS["m_lt"]:CS["m_lt"] + 128] = (p < f)
    c[:, CS["m_ge"]:CS["m_ge"] + 128] = (p >= f)
    c[:, CS["m_gt"]:CS["m_gt"] + 128] = (p > f)
    c[:, CS["neg_ge"]:CS["neg_ge"] + 128] = -1.0 * (p >= f)
    c[:, CS["neg_ones"]:CS["neg_ones"] + 128] = -1.0
    p64 = np.arange(128)[:, None] % 64
    f64 = np.arange(64)[None, :]
    lt = (p64 < f64).astype(np.float32)
    le = (p64 <= f64).astype(np.float32)
    gt = (p64 > f64).astype(np.float32)
    eq = (p64 == f64).astype(np.float32)
    c[:, CS["mask2"]:CS["mask2"] + 512] = np.tile(np.concatenate([lt, le], axis=1), (1, 4))
    c[:, CS["mask3"]:CS["mask3"] + 512] = np.tile(gt, (1, 8))
    c[:, CS["I8"]:CS["I8"] + 512] = np.tile(eq, (1, 8))
    return c


class Buf:
    __slots__ = ("name", "w", "r")

    def __init__(self, name=""):
        self.name = name
        self.w = []
        self.r = []


def _prune(toks):
    d = {}
    for s, v in toks:
        if d.get(s, 0) < v:
            d[s] = v
    return list(d.items())


class Sched:
    NRING = 20

    def __init__(self, nc, same_eng_sync=True):
        self.nc = nc
        self.engs = {"pe": nc.tensor, "act": nc.scalar, "dve": nc.vector, "pool": nc.gpsimd, "sp": nc.sync}
        self.sems = []
        self.esem = {}
        self.ecnt = {}
        self.known = {}
        for e in self.engs:
            self.esem[e] = self._new_sem("e_" + e)
            self.ecnt[e] = 0
            self.known[e] = {}
        self.ring = {}
        self.dcnt = {}
        self.semval = {}
        for e in ("sp", "act", "pool"):
            self.ring[e] = [self._new_sem("d_%s%d" % (e, i)) for i in range(self.NRING)]
            self.dcnt[e] = 0
            for s in self.ring[e]:
                self.semval[s] = 0
        self.same = same_eng_sync
        self.n_inst = 0
        self.max_pool_inflight = 4
        self.pool_hist = []

    def _new_sem(self, name):
        h = self.nc.semaphore(name).__enter__()
        self.sems.append(h)
        return len(self.sems) - 1

    def _wait(self, e, toks):
        eng = self.engs[e]
        kn = self.known[e]
        own = self.esem[e]
        for s, v in _prune(toks):
            if kn.get(s, 0) >= v:
                continue
            if s == own and (e == "pe" or not self.same):
                continue
            eng.wait_ge(self.sems[s], v)
            kn[s] = v

    def _deps(self, reads, writes):
        toks = []
        for b in reads:
            toks += b.w
        for b in writes:
            toks += b.w
            toks += b.r
        return toks

    def _commit(self, tok, reads, writes):
        for b in writes:
            b.w = [tok]
            b.r = []
        for b in reads:
            if b not in writes:
                b.r = _prune(b.r + [tok])

    def op(self, e, fn, reads=(), writes=()):
        self._wait(e, self._deps(reads, writes))
        inst = fn(self.engs[e])
        self.ecnt[e] += 1
        tok = (self.esem[e], self.ecnt[e])
        inst.then_inc(self.sems[tok[0]], 1)
        self._commit(tok, reads, writes)
        self.n_inst += 1
        return tok

    def mm(self, out, lhsT, rhs, start, stop, reads=(), writes=(), **kw):
        return self.op("pe", lambda en: en.matmul(out, lhsT, rhs, start=start, stop=stop, **kw), reads, writes)

    def dma(self, q, out, in_, reads=(), writes=(), **kw):
        toks = self._deps(reads, writes)
        i = self.dcnt[q] % self.NRING
        self.dcnt[q] += 1
        s = self.ring[q][i]
        prev = self.semval[s]
        if prev > 0:
            toks.append((s, prev))
        if q == "pool" and self.max_pool_inflight:
            hist = self.pool_hist
            if len(hist) >= self.max_pool_inflight:
                toks.append(hist[-self.max_pool_inflight])
        self._wait(q, toks)
        inst = self.engs[q].dma_start(out=out, in_=in_, **kw)
        inst.then_inc(self.sems[s], 16)
        self.semval[s] = prev + 16
        tok = (s, prev + 16)
        if q == "pool":
            self.pool_hist.append(tok)
        self._commit(tok, reads, writes)
        self.n_inst += 1
        return tok

    def all_tokens(self):
        toks = [(self.esem[e], self.ecnt[e]) for e in self.engs if self.ecnt[e] > 0]
        toks += [(s, v) for s, v in self.semval.items() if v > 0]
        return toks

    def barrier(self, engines=None):
        toks = self.all_tokens()
        for e in (engines or self.engs):
            kn = self.known[e]
            eng = self.engs[e]
            for s, v in toks:
                if s == self.esem[e]:
                    continue
                if kn.get(s, 0) >= v:
                    continue
                eng.wait_ge(self.sems[s], v)
                kn[s] = v


class Builder:
    def __init__(self, opts=None):
        self.o = dict(layers=NL, mixer=True, ffn=True, gm=True, rw=True, sb=True, dbg=())
        if opts:
            self.o.update(opts)
        self.nc = bass.Bass("TRN2", target_bir_lowering=False, dynamic_dma_scratch_size=8192)
        self.ctx = []

    def sb(self, name, shape, dt):
        t = self.nc.sbuf_tensor("s_" + name, shape, dt)
        h = t.__enter__()
        return h

    def tmp(self, name, shape, dt):
        self._uid = getattr(self, "_uid", 0) + 1
        return self.nc.sbuf_tensor("%s_%d" % (name, self._uid), shape, dt)

    def din(self, name, shape, dt=F32):
        return self.nc.dram_tensor(name, list(shape), dt, kind="ExternalInput").ap()

    def build(self):
        nc = self.nc
        o = self.o
        S = self.S = Sched(nc)
        I = self.I = {}
        I["x"] = self.din("x", [T, D])
        I["cst"] = self.din("cst", [128, NCST])
        I["cpk"] = self.din("cpk", [NL, 128, NCP])
        I["w_in"] = self.din("w_in", [NL, N_IN // 128, 128, D])
        I["gm_ln_g"] = self.din("gm_ln_g", [NL, 512])
        I["gm_ln_b"] = self.din("gm_ln_b", [NL, 512])
        I["gm_ws"] = self.din("gm_ws", [NL, 8, 128, 128])
        I["gm_bs"] = self.din("gm_bs", [NL, 1024])
        I["rw_w2"] = self.din("rw_w2", [NL, 64, 512])
        I["rw_a2"] = self.din("rw_a2", [NL, 64, 512])
        I["rw_g2"] = self.din("rw_g2", [NL, 128, 512])
        I["p_gm"] = self.din("p_gm", [NL, DC, 128, 512])
        I["p_rw"] = self.din("p_rw", [NL, DC, 128, 512])
        I["p_sb"] = self.din("p_sb", [NL, DC, 128, 1024])
        I["w_o"] = self.din("w_o", [NL, DC, 128, D])
        I["ffn_w1"] = self.din("ffn_w1", [1, FC, 128, D])
        I["ffn_w3"] = self.din("ffn_w3", [1, FC, 128, D])
        I["ffn_w2"] = self.din("ffn_w2", [1, DC, 128, D_FF])
        I["router_w"] = self.din("router_w", [1, D, NE])
        I["router_b"] = self.din("router_b", [1, NE])
        I["moe_w1"] = self.din("moe_w1", [1, NE, FCE, 128, D])
        I["moe_w3"] = self.din("moe_w3", [1, NE, FCE, 128, D])
        I["moe_w2"] = self.din("moe_w2", [1, NE, DC, 128, D_FE])
        self.out = nc.dram_tensor("out", [T, D], F32, kind="ExternalOutput").ap()
        self.hT = nc.dram_tensor("hT_scr", [DC, 128, T], F32, kind="Internal").ap()
        self.mT = nc.dram_tensor("mT_scr", [DC, 128, T], BF16, kind="Internal").ap()
        self.hT_b = [[Buf("hT%d_%d" % (dc, tt)) for tt in range(NTT)] for dc in range(DC)]
        self.mT_b = [Buf("mT%d" % dc) for dc in range(DC)]
        self.dbg_out = {}
        for name, shape, dt in o["dbg"]:
            self.dbg_out[name] = nc.dram_tensor("dbg_" + name, list(shape), dt, kind="ExternalOutput").ap()

        self.cst = self.sb("cst", [128, NCST], F32)
        self.cstb = self.sb("cstb", [128, NCST], BF16)
        self.cpk = self.sb("cpk", [128, NL, NCP], F32)
        self.nT = self.sb("nT", [128, DC, T], BF16)
        self.nT_b = [Buf("nT%d" % tt) for tt in range(NTT)]
        self.wbig = [self.sb("wbig%d" % i, [128, 5632], BF16) for i in range(2)]
        self.wbig_b = [Buf("wb%d" % i) for i in range(2)]
        self.wsml = [self.sb("wsml%d" % i, [128, 2048], BF16) for i in range(6)]
        self.wsml_b = [Buf("wsm%d" % i) for i in range(6)]
        self.wi_big = 0
        self.wi_sml = 0
        self.ps = [nc.psum_tensor("psb%d" % i, [128, 512], F32).__enter__() for i in range(8)]
        self.ps_b = [Buf("ps%d" % i) for i in range(8)]
        self.cst_b = Buf("cst")

        self._eps = {}
        for val in (RMS_EPS, LN_EPS, RW_GN_EPS, 1.0):
            t = self.sb("eps%d" % len(self._eps), [128, 1], F32)
            S.op("pool", lambda en, t=t, val=val: en.memset(t[:], float(val)), writes=[self.cst_b])
            self._eps[val] = t
        S.dma("sp", self.cst[:], I["cst"][:, :], writes=[self.cst_b])
        S.dma("sp", self.cpk[:], I["cpk"].rearrange("l p c -> p l c"), writes=[self.cst_b])
        S.op("dve", lambda en: en.tensor_copy(self.cstb[:], self.cst[:]), reads=[self.cst_b], writes=[self.cst_b])
        S.barrier()

        self.phase_input()
        for l in range(o["layers"]):
            if o["mixer"]:
                self.phase_norm(l, "norm_mix")
                self.phase_mixer(l)
            if o["ffn"] and not (o.get("only_moe") and l % 2 == 0):
                if l % 2 == 0:
                    for _rep in range(o.get("ffn_rep", 1)):
                        self.phase_norm(l, "norm_ffn")
                        self.phase_ffn_dense(l // 2)
                else:
                    self.phase_moe(l, l // 2)
        self.phase_output()
        S.barrier()
        return nc

    def C(self, name, n=128, rows=128, bf=True):
        t = self.cstb if bf else self.cst
        return t[0:rows, CS[name]:CS[name] + n]

    def col(self, l, name, j):
        return self.cpk[:, l, CP[name] + j:CP[name] + j + 1]

    def wload(self, src_ap, kc, ncol, extra_reads=()):
        n = kc * ncol
        if n <= 2048:
            i = self.wi_sml % 6
            self.wi_sml += 1
            slot, b = self.wsml[i], self.wsml_b[i]
        else:
            i = self.wi_big % 2
            self.wi_big += 1
            slot, b = self.wbig[i], self.wbig_b[i]
        nblk = ncol // 128
        if nblk == 1:
            dst = slot[:, 0:n]
            view = slot[:, 0:n].rearrange("p (k c) -> p k c", c=128)
        else:
            dst = slot[:, 0:n].rearrange("p (b x) -> p b x", b=nblk)
            view = slot[:, 0:n].rearrange("p (b k c) -> p k b c", b=nblk, c=128)
        self.S.dma("pool", dst, src_ap, reads=list(extra_reads), writes=[b], max_dma_last_dim=8192)
        return view, b

    @staticmethod
    def wsrc(wr, c0, ncol):
        cb = c0 // 128
        nblk = ncol // 128
        if nblk == 1:
            return wr[cb]
        return wr[cb:cb + nblk].rearrange("b p x -> p b x")

    def phase_input(self):
        S = self.S
        nc = self.nc
        with self.tmp("xin", [128, 2, D], F32) as xin, self.tmp("xst", [128, 2, DC, 128], F32) as xst:
            xin_b = [Buf(), Buf()]
            xst_b = [Buf(), Buf()]
            ident = self.C("ident", bf=False)
            for ti in range(16):
                k = ti % 2
                S.dma("sp", xin[:, k, :], self.I["x"][ti * 128:(ti + 1) * 128, :], writes=[xin_b[k]])
                for g in range(4):
                    pb = (ti * 4 + g) % 8
                    for j in range(4):
                        dc = g * 4 + j
                        S.op("pe", lambda en, dc=dc, j=j, pb=pb: en.transpose(
                            self.ps[pb][:, j * 128:(j + 1) * 128], xin[:, k, dc * 128:(dc + 1) * 128], ident),
                            reads=[xin_b[k], self.cst_b], writes=[self.ps_b[pb]])
                    eng = "act" if g % 2 == 0 else "dve"
                    dst = xst[:, k, g * 4:(g + 1) * 4, :]
                    src = self.ps[pb][:, :].rearrange("p (j t) -> p j t", t=128)
                    if eng == "act":
                        S.op("act", lambda en, dst=dst, src=src: en.copy(dst, src), reads=[self.ps_b[pb]],
                             writes=[xst_b[k]])
                    else:
                        S.op("dve", lambda en, dst=dst, src=src: en.tensor_copy(dst, src), reads=[self.ps_b[pb]],
                             writes=[xst_b[k]])
                tt = ti // 4
                S.dma("sp", self.hT.rearrange("d p t -> p d t")[:, :, ti * 128:(ti + 1) * 128], xst[:, k, :, :],
                      reads=[xst_b[k]], writes=[self.hT_b[dc][tt] for dc in range(DC)])
            S.barrier()

    def phase_norm(self, l, gname, router=None):
        S = self.S
        nc = self.nc
        ones = self.C("ones")
        with self.tmp("nh", [128, 1, DC, 512], F32) as nh, self.tmp("nsq", [128, DC, 512], BF16) as nsq, \
                self.tmp("nr", [128, 2, 512], F32) as nr:
            nh_b = [Buf(), Buf()]
            nsq_b = Buf()
            nr_b = [Buf(), Buf()]
            for tt in range(NTT):
                k = 0
                pb = tt % 2
                S.dma("sp", nh[:, k, :, :], self.hT.rearrange("d p t -> p d t")[:, :, tt * 512:(tt + 1) * 512],
                      reads=[self.hT_b[dc][tt] for dc in range(DC)], writes=[nh_b[k]])
                S.op("act", lambda en: en.activation(out=nsq[:], in_=nh[:, k, :, :], func=AF.Square),
                     reads=[nh_b[k]], writes=[nsq_b])
                for dc in range(DC):
                    S.mm(self.ps[pb][:, :], ones, nsq[:, dc, :], dc == 0, dc == DC - 1,
                         reads=[nsq_b, self.cst_b], writes=[self.ps_b[pb]])
                S.op("act", lambda en: en.activation(out=nr[:, k, :], in_=self.ps[pb][:, :], func=AF.Sqrt,
                                                     scale=1.0 / D, bias=self.eps_col(RMS_EPS)),
                     reads=[self.ps_b[pb], self.cst_b], writes=[nr_b[k]])
                S.op("dve", lambda en: en.reciprocal(nr[:, k, :], nr[:, k, :]), reads=[nr_b[k]], writes=[nr_b[k]])
                for dc in range(DC):
                    if router is None:
                        S.op("dve", lambda en, dc=dc: en.scalar_tensor_tensor(
                            out=self.nT[:, dc, tt * 512:(tt + 1) * 512], in0=nh[:, k, dc, :], scalar=self.col(l, gname, dc),
                            in1=nr[:, k, :], op0=ALU.mult, op1=ALU.mult),
                            reads=[nh_b[k], nr_b[k], self.cst_b], writes=[self.nT_b[tt]])
                    else:
                        S.op("dve", lambda en, dc=dc: en.scalar_tensor_tensor(
                            out=nh[:, k, dc, :], in0=nh[:, k, dc, :], scalar=self.col(l, gname, dc),
                            in1=nr[:, k, :], op0=ALU.mult, op1=ALU.mult),
                            reads=[nh_b[k], nr_b[k], self.cst_b], writes=[nh_b[k]])
                        S.op("act", lambda en, dc=dc: en.copy(self.nT[:, dc, tt * 512:(tt + 1) * 512], nh[:, k, dc, :]),
                             reads=[nh_b[k]], writes=[self.nT_b[tt]])
                if router is not None:
                    rw32, rw_b, lg32, lg_b = router
                    pr = 2 + tt % 2
                    for sub in range(4):
                        for dc in range(DC):
                            S.mm(self.ps[pr][:, sub * 8:(sub + 1) * 8], nh[:, k, dc, sub * 128:(sub + 1) * 128], rw32[:, dc, :],
                                 dc == 0, dc == DC - 1, reads=[nh_b[k], rw_b], writes=[self.ps_b[pr]])
                    S.op("dve", lambda en: en.tensor_copy(lg32[:, tt * 4:(tt + 1) * 4, :],
                                                          self.ps[pr][:, 0:32].rearrange("p (s e) -> p s e", e=8)),
                         reads=[self.ps_b[pr]], writes=[lg_b])
            S.barrier()

    def eps_col(self, val):
        return self._eps[val][:, 0:1]

    def resid_add(self, dc, tt, pb, stage, stage_b, scale_ap=None):
        S = self.S
        hsl = self.hT[dc, :, tt * 512:(tt + 1) * 512]
        S.dma("sp", stage, hsl, reads=[self.hT_b[dc][tt]], writes=[stage_b])
        if scale_ap is None:
            S.op("dve", lambda en: en.tensor_tensor(out=stage, in0=self.ps[pb][:, :], in1=stage, op=ALU.add),
                 reads=[self.ps_b[pb], stage_b], writes=[stage_b])
        else:
            sc_ap, sc_b, tmp, tmp_b = scale_ap
            S.op("dve", lambda en: en.tensor_tensor(out=tmp, in0=self.ps[pb][:, :], in1=sc_ap, op=ALU.mult),
                 reads=[self.ps_b[pb], sc_b], writes=[tmp_b])
            S.op("dve", lambda en: en.tensor_tensor(out=stage, in0=tmp, in1=stage, op=ALU.add),
                 reads=[tmp_b, stage_b], writes=[stage_b])
        S.dma("sp", hsl, stage, reads=[stage_b], writes=[self.hT_b[dc][tt]])

    def swiglu_up(self, w1, w3, nfc, gT, gT_b, tok0, ntok):
        S = self.S
        ntt = ntok // 512
        sa, sa_b = self.sw_sa
        if True:
            cnt = 0
            for fc in range(nfc):
                v1, b1 = self.wload(self.wsrc(w1, fc * 128, 128), 16, 128)
                v3, b3 = self.wload(self.wsrc(w3, fc * 128, 128), 16, 128)
                for t in range(ntt):
                    tg = (tok0 + t * 512) // 512
                    p1 = (cnt * 2) % 8
                    p3 = (cnt * 2 + 1) % 8
                    k = cnt % 2
                    cnt += 1
                    rhs = lambda kc: self.nT[:, kc, tok0 + t * 512: tok0 + (t + 1) * 512]
                    for kc in range(DC):
                        S.mm(self.ps[p1][:, :], v1[:, kc, :], rhs(kc), kc == 0, kc == DC - 1,
                             reads=[b1, self.nT_b[tg]], writes=[self.ps_b[p1]])
                    for kc in range(DC):
                        S.mm(self.ps[p3][:, :], v3[:, kc, :], rhs(kc), kc == 0, kc == DC - 1,
                             reads=[b3, self.nT_b[tg]], writes=[self.ps_b[p3]])
                    S.op("act", lambda en: en.activation(out=sa[:, k, :], in_=self.ps[p1][:, :], func=AF.Silu),
                         reads=[self.ps_b[p1]], writes=[sa_b[k]])
                    S.op("dve", lambda en: en.tensor_tensor(out=gT[:, fc, t * 512:(t + 1) * 512], in0=self.ps[p3][:, :],
                                                            in1=sa[:, k, :], op=ALU.mult),
                         reads=[self.ps_b[p3], sa_b[k]], writes=[gT_b])

    def swiglu_down(self, w2, nfc, gT, gT_b, tok0, ntok, scale=None):
        S = self.S
        ntt = ntok // 512
        st, st_b = self.sw_st
        tmp, tmp_b = self.sw_tmp
        if True:
            cnt = 0
            for dc in range(DC):
                v2, b2 = self.wload(self.wsrc(w2, dc * 128, 128), nfc, 128)
                for t in range(ntt):
                    tg = (tok0 + t * 512) // 512
                    pb = cnt % 8
                    k = cnt % 3
                    k2 = cnt % 2
                    cnt += 1
                    for fc in range(nfc):
                        S.mm(self.ps[pb][:, :], v2[:, fc, :], gT[:, fc, t * 512:(t + 1) * 512], fc == 0, fc == nfc - 1,
                             reads=[b2, gT_b], writes=[self.ps_b[pb]])
                    sc = None
                    if scale is not None:
                        sc = (scale[0][:, tok0 + t * 512: tok0 + (t + 1) * 512], scale[1], tmp[:, k2, :], tmp_b[k2])
                    self.resid_add(dc, tg, pb, st[:, k, :], st_b[k], sc)

    def phase_ffn_dense(self, j):
        S = self.S
        w1 = self.I["ffn_w1"][j]
        w3 = self.I["ffn_w3"][j]
        w2 = self.I["ffn_w2"][j]
        with ExitStack() as es:
            gT = es.enter_context(self.tmp("gT", [128, FC, 512], BF16))
            sa = es.enter_context(self.tmp("sw_a", [128, 2, 512], BF16))
            st = es.enter_context(self.tmp("sw_st", [128, 3, 512], F32))
            tmp = es.enter_context(self.tmp("sw_tmp", [128, 2, 512], F32))
            gT_b = Buf("gT")
            self.sw_sa = (sa, [Buf(), Buf()])
            self.sw_st = (st, [Buf(), Buf(), Buf()])
            self.sw_tmp = (tmp, [Buf(), Buf()])
            for tg in range(4):
                if "ffn_up" not in self.o.get("skip", ()):
                    self.swiglu_up(w1, w3, FC, gT, gT_b, tg * 512, 512)
                if "ffn_down" not in self.o.get("skip", ()):
                    self.swiglu_down(w2, FC, gT, gT_b, tg * 512, 512)
            S.barrier()

    def phase_moe(self, l, j):
        S = self.S
        I = self.I
        ident32 = self.C("ident", bf=False)
        ones32 = self.C("ones", bf=False)
        with ExitStack() as es:
            rw32 = es.enter_context(self.tmp("rw32", [128, DC, NE], F32))
            lg32 = es.enter_context(self.tmp("lg32", [128, 16, NE], F32))
            rb = es.enter_context(self.tmp("rb", [128, NE], F32))
            comb = es.enter_context(self.tmp("comb", [128, 16, NE], F32))
            t8 = es.enter_context(self.tmp("t8", [128, 3, 16, NE], F32))
            m12 = es.enter_context(self.tmp("m12", [128, 4, 16], F32))
            rw_b, lg_b, comb_b, t8_b, m_b = Buf("rw32"), Buf("lg32"), Buf("comb"), Buf("t8"), Buf("m12")
            S.dma("sp", rw32[:], I["router_w"][j].rearrange("(k p) e -> p k e", p=128), writes=[rw_b])
            S.dma("sp", rb[:], I["router_b"][j:j + 1, :].broadcast_to([128, NE]), writes=[rw_b])
            self.phase_norm(l, "norm_ffn", router=(rw32, rw_b, lg32, lg_b))

            def dve(fn, reads, writes):
                return S.op("dve", fn, reads, writes)

            bc8 = lambda ap: ap.unsqueeze(2).to_broadcast([128, 16, NE])
            dve(lambda en: en.tensor_tensor(out=lg32[:], in0=lg32[:], in1=rb[:, :].unsqueeze(1).to_broadcast([128, 16, NE]),
                                            op=ALU.add), [lg_b, rw_b], [lg_b])
            dve(lambda en: en.tensor_reduce(out=m12[:, 0, :], in_=lg32[:], axis=AX.X, op=ALU.max), [lg_b], [m_b])
            dve(lambda en: en.tensor_tensor(out=t8[:, 0, :, :], in0=lg32[:], in1=bc8(m12[:, 0, :]), op=ALU.is_equal),
                [lg_b, m_b], [t8_b])
            dve(lambda en: en.scalar_tensor_tensor(out=t8[:, 0, :, :], in0=t8[:, 0, :, :], scalar=-1e30, in1=lg32[:],
                                                   op0=ALU.mult, op1=ALU.add), [t8_b, lg_b], [t8_b])
            dve(lambda en: en.tensor_reduce(out=m12[:, 1, :], in_=t8[:, 0, :, :], axis=AX.X, op=ALU.max), [t8_b, m_b], [m_b])
            dve(lambda en: en.tensor_tensor(out=t8[:, 1, :, :], in0=lg32[:], in1=bc8(m12[:, 1, :]), op=ALU.is_ge),
                [lg_b, m_b, t8_b], [t8_b])
            dve(lambda en: en.tensor_tensor(out=t8[:, 2, :, :], in0=lg32[:], in1=bc8(m12[:, 0, :]), op=ALU.subtract),
                [lg_b, m_b, t8_b], [t8_b])
            S.op("act", lambda en: en.activation(out=t8[:, 2, :, :], in_=t8[:, 2, :, :], func=AF.Exp), [t8_b], [t8_b])
            dve(lambda en: en.tensor_tensor(out=m12[:, 2, :], in0=m12[:, 1, :], in1=m12[:, 0, :], op=ALU.subtract), [m_b], [m_b])
            S.op("act", lambda en: en.activation(out=m12[:, 2, :], in_=m12[:, 2, :], func=AF.Exp), [m_b], [m_b])
            dve(lambda en: en.tensor_scalar(m12[:, 2, :], m12[:, 2, :], 1.0, None, ALU.add), [m_b], [m_b])
            dve(lambda en: en.reciprocal(m12[:, 3, :], m12[:, 2, :]), [m_b], [m_b])
            dve(lambda en: en.tensor_tensor(out=t8[:, 2, :, :], in0=t8[:, 2, :, :], in1=t8[:, 1, :, :], op=ALU.mult), [t8_b], [t8_b])
            dve(lambda en: en.tensor_tensor(out=comb[:], in0=t8[:, 2, :, :], in1=bc8(m12[:, 3, :]), op=ALU.mult),
                [t8_b, m_b], [comb_b])
            if "comb" in self.dbg_out:
                S.dma("sp", self.dbg_out["comb"], comb[:], reads=[comb_b])

            gT = es.enter_context(self.tmp("gTe", [128, FCE, 1024], BF16))
            sa = es.enter_context(self.tmp("sw_a", [128, 2, 512], BF16))
            st = es.enter_context(self.tmp("sw_st", [128, 3, 512], F32))
            tmp = es.enter_context(self.tmp("sw_tmp", [128, 2, 512], F32))
            combB = es.enter_context(self.tmp("combB", [128, T], F32))
            dg = es.enter_context(self.tmp("dg", [128, 2, 128], F32))
            gT_b = Buf("gTe")
            cB_b = Buf("combB")
            dg_b = [Buf(), Buf()]
            self.sw_sa = (sa, [Buf(), Buf()])
            self.sw_st = (st, [Buf(), Buf(), Buf()])
            self.sw_tmp = (tmp, [Buf(), Buf()])
            for e in range(NE):
                for tt in range(NTT):
                    pb = self.bank()
                    for sub in range(4):
                        ti = tt * 4 + sub
                        k = ti % 2
                        dve(lambda en: en.tensor_scalar(dg[:, k, :], ident32, comb[:, ti, e:e + 1], None, ALU.mult),
                            [comb_b, self.cst_b, dg_b[k]], [dg_b[k]])
                        S.mm(self.ps[pb][:, sub * 128:(sub + 1) * 128], ones32, dg[:, k, :], True, True,
                             reads=[dg_b[k], self.cst_b], writes=[self.ps_b[pb]])
                    S.op("act", lambda en: en.copy(combB[:, tt * 512:(tt + 1) * 512], self.ps[pb][:, :]), [self.ps_b[pb]], [cB_b])
                for th in range(2):
                    self.swiglu_up(I["moe_w1"][j, e], I["moe_w3"][j, e], FCE, gT, gT_b, th * 1024, 1024)
                    self.swiglu_down(I["moe_w2"][j, e], FCE, gT, gT_b, th * 1024, 1024, scale=(combB, cB_b))
            S.barrier()

    def evac(self, eng, dst, src, reads, writes, scale=None):
        S = self.S
        if eng == "act":
            if scale is None:
                S.op("act", lambda en: en.copy(dst, src), reads=reads, writes=writes)
            else:
                S.op("act", lambda en: en.mul(dst, src, scale), reads=reads, writes=writes)
        else:
            if scale is None:
                S.op("dve", lambda en: en.tensor_copy(dst, src), reads=reads, writes=writes)
            else:
                S.op("dve", lambda en: en.tensor_scalar(dst, src, scale, None, ALU.mult), reads=reads, writes=writes)

    def phase_mixer(self, l):
        S = self.S
        o = self.o
        with self.tmp("yrwT", [128, 4, T], BF16) as yrwT:
            yrw_b = Buf("yrw")
            if o["rw"]:
                self.phase_rw(l, yrwT, yrw_b)
            else:
                S.op("pool", lambda en: en.memset(yrwT[:], 0.0), writes=[yrw_b])
            S.barrier()
            with self.tmp("ygmT", [128, 4, T], BF16) as ygmT:
                ygm_b = Buf("ygm")
                if o["gm"]:
                    self.phase_gm(l, ygmT, ygm_b)
                else:
                    S.op("pool", lambda en: en.memset(ygmT[:], 0.0), writes=[ygm_b])
                S.barrier()
                with self.tmp("ysbT", [128, 8, T], BF16) as ysbT:
                    ysb_b = Buf("ysb")
                    if o["sb"]:
                        self.phase_sb(l, ysbT, ysb_b)
                    else:
                        S.op("pool", lambda en: en.memset(ysbT[:], 0.0), writes=[ysb_b])
                    S.barrier()
                    for nm, tns in (("ygm", ygmT), ("yrw", yrwT), ("ysb", ysbT)):
                        if nm in self.dbg_out:
                            S.dma("sp", self.dbg_out[nm].rearrange("c p t -> p c t"), tns[:], reads=[ygm_b, yrw_b, ysb_b])
                    if "merge" not in o.get("skip", ()):
                        self.phase_merge(l, ygmT, ygm_b, yrwT, yrw_b, ysbT, ysb_b)
                    S.barrier()
        if "wo" not in o.get("skip", ()):
            self.phase_wo(l)

    def phase_sb(self, l, ysbT, ysb_b):
        S = self.S
        w = self.I["w_in"][l]
        m_lt = self.C("m_lt")
        neg_ge = self.C("neg_ge")
        neg_ones = self.C("neg_ones")
        zeros64 = self.C("zeros", n=64)
        with self.tmp("v_all", [128, 16, 256], BF16) as v_all, self.tmp("qT", [128, T], BF16) as qT, \
                self.tmp("kT", [128, T], BF16) as kT, \
                self.tmp("sp", [128, 3, 512], BF16) as sp, self.tmp("att", [128, 3, 512], BF16) as att, \
                self.tmp("ssum", [128, 512], BF16) as ssum:
            v_b = Buf("v_all")
            q_b = Buf("qT")
            k_b = Buf("kT")
            ez_b = [Buf(), Buf()]
            sp_b = [Buf(), Buf(), Buf()]
            att_b = [Buf(), Buf(), Buf()]
            ss_b = Buf("ssum")
            cnt = 0
            pi = 0
            for hp in range(8):
                if hp % 2 == 0:
                    wv, wb = self.wload(self.wsrc(w, SB_OFF + 2048 + (hp // 2) * 256, 256), 16, 256)
                    for ti in range(16):
                        pb = 6 + cnt % 2
                        cnt += 1
                        for kc in range(DC):
                            S.mm(self.ps[pb][:, 0:256], self.nT[:, kc, ti * 128:(ti + 1) * 128], wv[:, kc, :], kc == 0,
                                 kc == DC - 1, reads=[wb, self.nT_b[ti // 4]], writes=[self.ps_b[pb]])
                        self.evac("act" if cnt % 2 else "dve", v_all[:, ti, :],
                                  self.ps[pb][:, 0:256], [self.ps_b[pb]], [v_b])
                wq, wqb = self.wload(self.wsrc(w, SB_OFF + hp * 128, 128), 16, 128)
                wk, wkb = self.wload(self.wsrc(w, SB_OFF + 1024 + hp * 128, 128), 16, 128)
                for tt in range(NTT):
                    pb = 6 + (tt % 2)
                    for kc in range(DC):
                        S.mm(self.ps[pb][:, :], wq[:, kc, :], self.nT[:, kc, tt * 512:(tt + 1) * 512], kc == 0, kc == DC - 1,
                             reads=[wqb, self.nT_b[tt]], writes=[self.ps_b[pb]])
                    self.evac("dve", qT[:, tt * 512:(tt + 1) * 512], self.ps[pb][:, :], [self.ps_b[pb]], [q_b], scale=0.125)
                for tt in range(NTT):
                    pb = 6 + (tt % 2)
                    for kc in range(DC):
                        S.mm(self.ps[pb][:, :], wk[:, kc, :], self.nT[:, kc, tt * 512:(tt + 1) * 512], kc == 0, kc == DC - 1,
                             reads=[wkb, self.nT_b[tt]], writes=[self.ps_b[pb]])
                    self.evac("dve", kT[:, tt * 512:(tt + 1) * 512], self.ps[pb][:, :], [self.ps_b[pb]], [k_b])
                for h2 in range(2):
                    hb = h2 * 64
                    h = hp * 2 + h2
                    for qt in range(NTT):
                        ui = getattr(self, "_sb_ui", 0)
                        self._sb_ui = ui + 1
                        po = 4 + (ui % 2)
                        S.mm(self.ps[po][hb:hb + 64, :], zeros64, self.nT[:, 0, 0:512], True, False,
                             reads=[self.cst_b, self.nT_b[0]], writes=[self.ps_b[po]])
                        S.op("pool", lambda en: en.memset(ssum[:], 0.0), writes=[ss_b])
                        def front(kb, pidx):
                            kl = kb - 4 * qt
                            tq0 = max(kl, 0) * 128
                            i = pidx % 3
                            za = (0, 1, 6)[pidx % 3]
                            q_ap = qT[hb:hb + 64, qt * 512 + tq0:(qt + 1) * 512]
                            k_ap = kT[hb:hb + 64, kb * 128:(kb + 1) * 128]
                            S.mm(self.ps[za][:, tq0:512], k_ap, q_ap, True, True, reads=[q_b, k_b], writes=[self.ps_b[za]])
                            S.op("act", lambda en: en.activation(out=self.ps[za][:, tq0:512], in_=self.ps[za][:, tq0:512],
                                                                 func=AF.Exp), reads=[self.ps_b[za]], writes=[self.ps_b[za]])
                            S.op("act", lambda en: en.activation(out=sp[:, i, tq0:512], in_=self.ps[za][:, tq0:512], func=AF.Ln,
                                                                 bias=self.eps_col(1.0)),
                                 reads=[self.ps_b[za], self.cst_b], writes=[sp_b[i]])
                            if kl >= 0:
                                S.op("pool", lambda en: en.tensor_tensor(out=sp[:, i, tq0:tq0 + 128], in0=sp[:, i, tq0:tq0 + 128],
                                                                         in1=m_lt, op=ALU.mult),
                                     reads=[sp_b[i], self.cst_b], writes=[sp_b[i]])

                        def back(kb, pidx, first):
                            kl = kb - 4 * qt
                            tq0 = max(kl, 0) * 128
                            i = pidx % 3
                            zb = (2, 3, 7)[pidx % 3]
                            q_ap = qT[hb:hb + 64, qt * 512 + tq0:(qt + 1) * 512]
                            k_ap = kT[hb:hb + 64, kb * 128:(kb + 1) * 128]
                            S.mm(self.ps[zb][:, tq0:512], k_ap, q_ap, True, False, reads=[q_b, k_b], writes=[self.ps_b[zb]])
                            S.mm(self.ps[zb][:, tq0:512], neg_ge, sp[:, i, tq0:512], False, first,
                                 reads=[sp_b[i], self.cst_b], writes=[self.ps_b[zb]])
                            if not first:
                                S.mm(self.ps[zb][:, tq0:512], neg_ones, ssum[:, tq0:512], False, True,
                                     reads=[ss_b, self.cst_b], writes=[self.ps_b[zb]])
                            S.op("act", lambda en: en.activation(out=att[:, i, tq0:512], in_=self.ps[zb][:, tq0:512],
                                                                 func=AF.Exp), reads=[self.ps_b[zb]], writes=[att_b[i]])
                            if kl >= 0:
                                S.op("pool", lambda en: en.tensor_tensor(out=att[:, i, tq0:tq0 + 128], in0=att[:, i, tq0:tq0 + 128],
                                                                         in1=m_lt, op=ALU.mult),
                                     reads=[att_b[i], self.cst_b], writes=[att_b[i]])
                            S.mm(self.ps[po][hb:hb + 64, tq0:512], v_all[:, kb, (h % 4) * 64:(h % 4 + 1) * 64], att[:, i, tq0:512],
                                 False, kb == 0, reads=[v_b, att_b[i]], writes=[self.ps_b[po]])
                            if kb > 0:
                                S.op("pool", lambda en: en.tensor_tensor(out=ssum[:, tq0:512], in0=ssum[:, tq0:512],
                                                                         in1=sp[:, i, tq0:512], op=ALU.add),
                                     reads=[sp_b[i], ss_b], writes=[ss_b])

                        kbs = list(range(4 * qt + 3, -1, -1))
                        pidx0 = pi
                        front(kbs[0], pidx0)
                        for n_, kb in enumerate(kbs):
                            if n_ + 1 < len(kbs):
                                front(kbs[n_ + 1], pidx0 + n_ + 1)
                            back(kb, pidx0 + n_, n_ == 0)
                        pi = pidx0 + len(kbs)
                        self.evac("dve", ysbT[hb:hb + 64, hp, qt * 512:(qt + 1) * 512], self.ps[po][hb:hb + 64, :],
                                  [self.ps_b[po]], [ysb_b])
            S.barrier()

    def gelu(self, dst, pb, ncol, tmp, tmp_b, writes):
        S = self.S
        src = self.ps[pb][:, 0:ncol]
        S.op("act", lambda en: en.activation(out=tmp, in_=src, func=AF.Square), reads=[self.ps_b[pb]], writes=[tmp_b])
        S.op("dve", lambda en: en.tensor_scalar(tmp, tmp, 0.0713548163, 1.5957691216, ALU.mult, ALU.add),
             reads=[tmp_b], writes=[tmp_b])
        S.op("dve", lambda en: en.tensor_tensor(out=tmp, in0=src, in1=tmp, op=ALU.mult), reads=[tmp_b, self.ps_b[pb]],
             writes=[tmp_b])
        S.op("act", lambda en: en.activation(out=tmp, in_=tmp, func=AF.Sigmoid), reads=[tmp_b], writes=[tmp_b])
        S.op("dve", lambda en: en.tensor_tensor(out=dst, in0=src, in1=tmp, op=ALU.mult), reads=[tmp_b, self.ps_b[pb]],
             writes=writes)

    def phase_gm(self, l, ygmT, ygm_b):
        S = self.S
        w = self.I["w_in"][l]
        I = self.I
        with self.tmp("ug", [128, 4, T], BF16) as ug, self.tmp("vln", [128, 16, 512], BF16) as vln, \
                self.tmp("wsT", [128, 8, 128], BF16) as wsT, self.tmp("lng", [128, 512], F32) as lng, \
                self.tmp("lnb", [128, 512], F32) as lnb, self.tmp("bs32", [1, 1024], F32) as bs32, \
                self.tmp("bsr", [1, 1024], BF16) as bsr, self.tmp("gt", [128, 2, 512], F32) as gt, \
                self.tmp("vg", [128, 2, 512], F32) as vg, self.tmp("ws32", [128, 2, 128], F32) as ws32, \
                self.tmp("st6", [128, 2, 8], F32) as st6:
            ug_b = Buf("ug")
            vln_b = Buf("vln")
            wsT_b = Buf("wsT")
            par_b = Buf("gmpar")
            gt_b = [Buf(), Buf()]
            vg_b = [Buf(), Buf()]
            ws32_b = [Buf(), Buf()]
            st_b = [Buf(), Buf()]
            S.dma("sp", lng[:], I["gm_ln_g"][l:l + 1, :].broadcast_to([128, 512]), writes=[par_b])
            S.dma("sp", lnb[:], I["gm_ln_b"][l:l + 1, :].broadcast_to([128, 512]), writes=[par_b])
            S.dma("sp", bs32[:], I["gm_bs"][l:l + 1, :], writes=[par_b])
            S.op("dve", lambda en: en.tensor_copy(bsr[:], bs32[:]), reads=[par_b], writes=[par_b])
            ident = self.C("ident", bf=False)
            m_le = self.C("m_le", bf=False)
            for h in range(8):
                k = h % 2
                pb = h % 2
                S.dma("sp", ws32[:, k, :], I["gm_ws"][l, h], writes=[ws32_b[k]])
                S.op("pe", lambda en: en.transpose(self.ps[pb][:, 0:128], ws32[:, k, :], ident),
                     reads=[ws32_b[k], self.cst_b], writes=[self.ps_b[pb]])
                S.op("dve", lambda en: en.tensor_tensor(out=wsT[:, h, :], in0=self.ps[pb][:, 0:128], in1=m_le, op=ALU.mult),
                     reads=[self.ps_b[pb], self.cst_b], writes=[wsT_b])
            cnt = 0
            for c in range(4):
                wv, wb = self.wload(self.wsrc(w, GM_OFF + c * 128, 128), 16, 128)
                for tt in range(NTT):
                    pb = 2 + cnt % 6
                    k = cnt % 2
                    cnt += 1
                    for kc in range(DC):
                        S.mm(self.ps[pb][:, :], wv[:, kc, :], self.nT[:, kc, tt * 512:(tt + 1) * 512], kc == 0, kc == DC - 1,
                             reads=[wb, self.nT_b[tt]], writes=[self.ps_b[pb]])
                    self.gelu(ug[:, c, tt * 512:(tt + 1) * 512], pb, 512, gt[:, k, :], gt_b[k], [ug_b])
            wv0, wb0 = self.wload(self.wsrc(w, GM_OFF + 512, 256), 16, 256)
            wv1, wb1 = self.wload(self.wsrc(w, GM_OFF + 768, 256), 16, 256)
            for ti in range(16):
                pb = 2 + cnt % 6
                k = cnt % 2
                cnt += 1
                for cb, (wv, wb) in enumerate(((wv0, wb0), (wv1, wb1))):
                    for kc in range(DC):
                        S.mm(self.ps[pb][:, cb * 256:(cb + 1) * 256], self.nT[:, kc, ti * 128:(ti + 1) * 128], wv[:, kc, :],
                             kc == 0, kc == DC - 1, reads=[wb, self.nT_b[ti // 4]], writes=[self.ps_b[pb]])
                self.gelu(vg[:, k, :], pb, 512, gt[:, k, :], gt_b[k], [vg_b[k]])
                S.op("dve", lambda en: en.bn_stats(st6[:, k, 0:6], vg[:, k, :]), reads=[vg_b[k]], writes=[st_b[k]])
                S.op("dve", lambda en: en.bn_aggr(st6[:, k, 6:8], st6[:, k, 0:6]), reads=[st_b[k]], writes=[st_b[k]])
                S.op("act", lambda en: en.activation(out=st6[:, k, 7:8], in_=st6[:, k, 7:8], func=AF.Sqrt,
                                                     bias=self.eps_col(LN_EPS)), reads=[st_b[k], self.cst_b], writes=[st_b[k]])
                S.op("dve", lambda en: en.reciprocal(st6[:, k, 7:8], st6[:, k, 7:8]), reads=[st_b[k]], writes=[st_b[k]])
                S.op("dve", lambda en: en.tensor_scalar(vg[:, k, :], vg[:, k, :], st6[:, k, 6:7], st6[:, k, 7:8],
                                                        ALU.subtract, ALU.mult), reads=[vg_b[k], st_b[k]], writes=[vg_b[k]])
                S.op("dve", lambda en: en.tensor_tensor(out=vg[:, k, :], in0=vg[:, k, :], in1=lng[:], op=ALU.mult),
                     reads=[vg_b[k], par_b], writes=[vg_b[k]])
                S.op("dve", lambda en: en.tensor_tensor(out=vln[:, ti, :], in0=vg[:, k, :], in1=lnb[:], op=ALU.add),
                     reads=[vg_b[k], par_b], writes=[vln_b])
            ones_row = self.cstb[0:1, CS["ones"]:CS["ones"] + 64]
            for cg in range(4):
                for hp in range(4):
                    pb = 2 + cnt % 6
                    cnt += 1
                    for cl in range(4):
                        c = cg * 4 + cl
                        for h2 in range(2):
                            h = hp * 2 + h2
                            outp = self.ps[pb][h2 * 64:(h2 + 1) * 64, cl * 128:(cl + 1) * 128]
                            S.mm(outp, vln[:, c, h * 64:(h + 1) * 64], wsT[:, h, :], True, False,
                                 reads=[vln_b, wsT_b], writes=[self.ps_b[pb]])
                            S.mm(outp, ones_row, bsr[0:1, h * 128:(h + 1) * 128], False, True,
                                 reads=[par_b, self.cst_b], writes=[self.ps_b[pb]])
                    S.op("dve", lambda en: en.tensor_tensor(out=ygmT[:, hp, cg * 512:(cg + 1) * 512], in0=self.ps[pb][:, :],
                                                            in1=ug[:, hp, cg * 512:(cg + 1) * 512], op=ALU.mult),
                         reads=[self.ps_b[pb], ug_b], writes=[ygm_b])
            S.barrier()

    def phase_merge(self, l, ygmT, ygm_b, yrwT, yrw_b, ysbT, ysb_b):
        S = self.S
        I = self.I
        w = I["w_in"][l]
        with self.tmp("gate", [128, 2, 3, 512], BF16) as gate, self.tmp("mst", [128, 2, T], BF16) as mst, \
                self.tmp("mt", [128, 2, 2, 512], F32) as mt:
            gate_b = [[Buf() for _ in range(3)] for _ in range(2)]
            mst_b = [Buf(), Buf()]
            mt_b = [[Buf(), Buf()], [Buf(), Buf()]]
            cnt = 0
            gi = 0
            for dc in range(DC):
                wg = [self.wload(self.wsrc(w, GATE_OFF + b * 2048 + dc * 128, 128), 16, 128) for b in range(3)]
                wp = [self.wload(self.wsrc(I["p_gm"][l], dc * 128, 128), 4, 128),
                      self.wload(self.wsrc(I["p_rw"][l], dc * 128, 128), 4, 128),
                      self.wload(self.wsrc(I["p_sb"][l], dc * 128, 128), 8, 128)]
                ys = [(ygmT, ygm_b, 4), (yrwT, yrw_b, 4), (ysbT, ysb_b, 8)]
                km = dc % 2
                for tt in range(NTT):
                    kg = gi % 2
                    gi += 1
                    for b in range(3):
                        pb = cnt % 8
                        cnt += 1
                        for kc in range(DC):
                            S.mm(self.ps[pb][:, :], wg[b][0][:, kc, :], self.nT[:, kc, tt * 512:(tt + 1) * 512], kc == 0,
                                 kc == DC - 1, reads=[wg[b][1], self.nT_b[tt]], writes=[self.ps_b[pb]])
                        S.op("act", lambda en: en.activation(out=gate[:, kg, b, :], in_=self.ps[pb][:, :], func=AF.Sigmoid,
                                                             bias=self.col(l, "gate_b", b * 16 + dc)),
                             reads=[self.ps_b[pb], self.cst_b], writes=[gate_b[kg][b]])
                    pbs = []
                    for b in range(3):
                        pb = cnt % 8
                        cnt += 1
                        pbs.append(pb)
                        yt, yb, nk = ys[b]
                        for kc in range(nk):
                            S.mm(self.ps[pb][:, :], wp[b][0][:, kc, :], yt[:, kc, tt * 512:(tt + 1) * 512], kc == 0, kc == nk - 1,
                                 reads=[wp[b][1], yb], writes=[self.ps_b[pb]])
                    t0 = mt[:, kg, 0, :]
                    t1 = mt[:, kg, 1, :]
                    b0, b1 = mt_b[kg]
                    S.op("dve", lambda en: en.tensor_tensor(out=t0, in0=self.ps[pbs[0]][:, :], in1=gate[:, kg, 0, :], op=ALU.mult),
                         reads=[self.ps_b[pbs[0]], gate_b[kg][0]], writes=[b0])
                    S.op("dve", lambda en: en.tensor_tensor(out=t1, in0=self.ps[pbs[1]][:, :], in1=gate[:, kg, 1, :], op=ALU.mult),
                         reads=[self.ps_b[pbs[1]], gate_b[kg][1]], writes=[b1])
                    S.op("dve", lambda en: en.tensor_tensor(out=t0, in0=t0, in1=t1, op=ALU.add), reads=[b0, b1], writes=[b0])
                    S.op("dve", lambda en: en.tensor_tensor(out=t1, in0=self.ps[pbs[2]][:, :], in1=gate[:, kg, 2, :], op=ALU.mult),
                         reads=[self.ps_b[pbs[2]], gate_b[kg][2]], writes=[b1])
                    S.op("dve", lambda en: en.tensor_tensor(out=mst[:, km, tt * 512:(tt + 1) * 512], in0=t0, in1=t1, op=ALU.add),
                         reads=[b0, b1], writes=[mst_b[km]])
                S.dma("sp", self.mT[dc], mst[:, km, :], reads=[mst_b[km]], writes=[self.mT_b[dc]])
            S.barrier()

    def phase_wo(self, l):
        S = self.S
        wo = self.I["w_o"][l]
        with self.tmp("wo_st", [128, 3, 512], F32) as st:
            st_b = [Buf(), Buf(), Buf()]
            for dc in range(DC):
                S.dma("sp", self.nT[:, dc, :], self.mT[dc], reads=[self.mT_b[dc]], writes=self.nT_b)
            cnt = 0
            for dc in range(DC):
                wv, wb = self.wload(self.wsrc(wo, dc * 128, 128), 16, 128)
                for tt in range(NTT):
                    pb = cnt % 8
                    k = cnt % 3
                    cnt += 1
                    for kc in range(DC):
                        S.mm(self.ps[pb][:, :], wv[:, kc, :], self.nT[:, kc, tt * 512:(tt + 1) * 512], kc == 0, kc == DC - 1,
                             reads=[wb, self.nT_b[tt]], writes=[self.ps_b[pb]])
                    self.resid_add(dc, tt, pb, st[:, k, :], st_b[k])
            S.barrier()

    def bank(self):
        self.pbi = getattr(self, "pbi", 0) + 1
        return self.pbi % 8

    def phase_rw(self, l, yrwT, yrw_b):
        S = self.S
        I = self.I
        w = I["w_in"][l]
        TW = 256
        NTW = T // TW
        sdec = -0.6065306597126334
        ps = self.ps
        psb = self.ps_b
        cstb_ = self.cst_b
        blockones = self.C("blockones")
        identb = self.C("ident")
        ident32 = self.cst[0:64, CS["ident"]:CS["ident"] + 64]
        mask2 = self.cst[0:64, CS["mask2"]:CS["mask2"] + 512]
        mask3 = self.cst[0:64, CS["mask3"]:CS["mask3"] + 512]
        I8 = self.cstb[0:64, CS["I8"]:CS["I8"] + 512]

        def dve(fn, reads, writes):
            return S.op("dve", fn, reads, writes)

        def act(fn, reads, writes):
            return S.op("act", fn, reads, writes)

        with ExitStack() as _es:
            lwa = _es.enter_context(self.tmp("lwa", [128, 512], BF16))
            lg = _es.enter_context(self.tmp("lg", [128, 512], BF16))
            l32 = _es.enter_context(self.tmp("l32", [128, 2, 512], F32))
            rmask = _es.enter_context(self.tmp("rmask", [128, TW], F32))
            prevcol = _es.enter_context(self.tmp("prevcol", [128, 16], F32))
            S32 = _es.enter_context(self.tmp("S32", [128, 4, 64], F32))
            Sbf = _es.enter_context(self.tmp("Sbf", [128, 4, 64], BF16))
            gC = _es.enter_context(self.tmp("gC", [128, 4, 32], F32))
            ar = _es.enter_context(self.tmp("ar", [128, 4, 2, TW], BF16))
            bk = _es.enter_context(self.tmp("bk", [128, 4, 2, TW], BF16))
            vb = _es.enter_context(self.tmp("vb", [128, 4, TW], BF16))
            bh = _es.enter_context(self.tmp("bh", [128, 4, TW], BF16))
            kh = _es.enter_context(self.tmp("kh", [128, 4, TW], BF16))
            gT = _es.enter_context(self.tmp("gT", [128, 4, TW], BF16))
            bon = _es.enter_context(self.tmp("bon", [128, 4, TW], BF16))
            ynT = _es.enter_context(self.tmp("ynT", [128, 4, TW], F32))
            ar_bd = _es.enter_context(self.tmp("ar_bd", [128, 4, 2, 2, TW], BF16))
            bt_bd = _es.enter_context(self.tmp("bt_bd", [128, 4, 2, TW], BF16))
            Sbd = _es.enter_context(self.tmp("Sbd", [128, 4, 2, 64], BF16))
            bd_b = Buf("bd")
            par_b = Buf("rwpar")
            pc_b = Buf("prevcol")
            S_b = Buf("S")
            S_gb = [Buf("S0"), Buf("S1")]
            gC_b = Buf("gC")
            ar_b = Buf("ar")
            bk_b = Buf("bk")
            tok_b = Buf("vbbhkh")
            gT_b = Buf("gT")
            bon_b = Buf("bon")
            ynT_b = Buf("ynT")
            S.dma("sp", l32[0:64, 0, :], I["rw_w2"][l], writes=[par_b])
            S.dma("sp", l32[64:128, 0, :], I["rw_a2"][l], writes=[par_b])
            S.dma("sp", l32[:, 1, :], I["rw_g2"][l], writes=[par_b])
            dve(lambda en: en.tensor_copy(lwa[:], l32[:, 0, :]), [par_b], [par_b])
            dve(lambda en: en.tensor_copy(lg[:], l32[:, 1, :]), [par_b], [par_b])
            S.op("pool", lambda en: en.memset(rmask[:], 1.0), writes=[par_b])
            S.op("pool", lambda en: en.memset(rmask[:, :].rearrange("p (c t) -> p c t", t=64)[:, :, 0:1], 0.0), writes=[par_b])
            S.op("pool", lambda en: en.memset(S32[:], 0.0), writes=[S_b, S_gb[0], S_gb[1]])
            S.op("pool", lambda en: en.memset(Sbf[:], 0.0), writes=[S_b, S_gb[0], S_gb[1]])
            S.op("pool", lambda en: en.memset(Sbd[:], 0.0), writes=[S_b, S_gb[0], S_gb[1]])
            S.op("pool", lambda en: en.memset(ar_bd[:], 0.0), writes=[bd_b])
            S.op("pool", lambda en: en.memset(bt_bd[:], 0.0), writes=[bd_b])
            S.op("pool", lambda en: en.memset(prevcol[:], 0.0), writes=[pc_b])

            for ti in range(NTW):
                t0 = ti * TW
                tg = t0 // 512
                with ExitStack() as _es:
                    p32 = _es.enter_context(self.tmp("p32", [128, 2, TW + 1], F32))
                    dd = _es.enter_context(self.tmp("dd", [128, 2, TW], F32))
                    twl = _es.enter_context(self.tmp("twl", [128, TW], BF16))
                    sgl = _es.enter_context(self.tmp("sgl", [128, TW], BF16))
                    r32 = _es.enter_context(self.tmp("r32", [128, TW], F32))
                    k32 = _es.enter_context(self.tmp("k32", [128, TW], F32))
                    v32 = _es.enter_context(self.tmp("v32", [128, TW], F32))
                    sg = _es.enter_context(self.tmp("sg", [128, TW], F32))
                    a32 = _es.enter_context(self.tmp("a32", [128, TW], F32))
                    kk = _es.enter_context(self.tmp("kk", [128, TW], F32))
                    kp = _es.enter_context(self.tmp("kp", [128, TW], F32))
                    bt = _es.enter_context(self.tmp("bt", [128, TW], F32))
                    Lc = _es.enter_context(self.tmp("Lc", [128, TW], F32))
                    E1 = _es.enter_context(self.tmp("E1", [128, TW], F32))
                    E2 = _es.enter_context(self.tmp("E2", [128, TW], F32))
                    x16 = _es.enter_context(self.tmp("x16", [128, TW], BF16))
                    p32_b = [Buf(), Buf()]
                    dd_b = [Buf(), Buf()]
                    lo_b = Buf("lora_in")
                    r_b, k_b, v_b, sg_b, a_b, kk_b, kp_b, bt_b, Lc_b, E1_b, E2_b, x16_b = [Buf() for _ in range(12)]
                    self._shi = 0

                    def shifted(j, dst, dst_b, func=None, rows=None):
                        wv, wb = self.wload(self.wsrc(w, RW_OFF + j * 128, 128), 16, 128)
                        pb = self.bank()
                        q = self._shi % 2
                        self._shi += 1
                        for kc in range(DC):
                            S.mm(ps[pb][:, 0:TW], wv[:, kc, :], self.nT[:, kc, t0:t0 + TW], kc == 0, kc == DC - 1,
                                 reads=[wb, self.nT_b[tg]], writes=[psb[pb]])
                        act(lambda en: en.copy(p32[:, q, 1:TW + 1], ps[pb][:, 0:TW]), [psb[pb]], [p32_b[q]])
                        act(lambda en: en.copy(p32[:, q, 0:1], prevcol[:, j:j + 1]), [pc_b], [p32_b[q]])
                        act(lambda en: en.copy(prevcol[:, j:j + 1], p32[:, q, TW:TW + 1]), [p32_b[q]], [pc_b])
                        dve(lambda en: en.tensor_tensor(out=dd[:, q, :], in0=p32[:, q, 0:TW], in1=p32[:, q, 1:TW + 1],
                                                        op=ALU.subtract), [p32_b[q]], [dd_b[q]])
                        if func is None:
                            dve(lambda en: en.scalar_tensor_tensor(out=dst, in0=dd[:, q, :], scalar=self.col(l, "rw_mu", j),
                                                                   in1=p32[:, q, 1:TW + 1], op0=ALU.mult, op1=ALU.add),
                                [dd_b[q], p32_b[q], cstb_], [dst_b])
                        else:
                            dve(lambda en: en.scalar_tensor_tensor(out=dd[:, q, :], in0=dd[:, q, :], scalar=self.col(l, "rw_mu", j),
                                                                   in1=p32[:, q, 1:TW + 1], op0=ALU.mult, op1=ALU.add),
                                [dd_b[q], p32_b[q], cstb_], [dd_b[q]])
                            for (r0, r1, f) in func:
                                if f is None:
                                    act(lambda en: en.copy(dst[r0:r1, :], dd[r0:r1, q, :]), [dd_b[q]], [dst_b])
                                else:
                                    act(lambda en: en.activation(out=dst[r0:r1, :], in_=dd[r0:r1, q, :], func=f),
                                        [dd_b[q]], [dst_b])

                    shifted(12, twl, lo_b, func=[(0, 64, AF.Tanh), (64, 128, None)])
                    shifted(13, sgl, lo_b, func=[(0, 128, AF.Sigmoid)])
                    for fc in range(4):
                        shifted(fc, r32[:], r_b)
                        shifted(4 + fc, k32[:], k_b)
                        shifted(8 + fc, v32[:], v_b)
                        cs128 = slice(fc * 128, (fc + 1) * 128)
                        pb = self.bank()
                        S.mm(ps[pb][:, 0:TW], lwa[0:64, cs128], twl[0:64, :], True, True, reads=[par_b, lo_b], writes=[psb[pb]])
                        act(lambda en: en.activation(out=sg[:], in_=ps[pb][:, 0:TW], func=AF.Sigmoid,
                                                     bias=self.col(l, "rw_w0", fc)), [psb[pb], cstb_], [sg_b])
                        pb = self.bank()
                        S.mm(ps[pb][:, 0:TW], lwa[64:128, cs128], twl[64:128, :], True, True, reads=[par_b, lo_b], writes=[psb[pb]])
                        act(lambda en: en.activation(out=a32[:], in_=ps[pb][:, 0:TW], func=AF.Sigmoid,
                                                     bias=self.col(l, "rw_a0", fc)), [psb[pb], cstb_], [a_b])
                        pb = self.bank()
                        S.mm(ps[pb][:, 0:TW], lg[:, cs128], sgl[:, :], True, True, reads=[par_b, lo_b], writes=[psb[pb]])
                        act(lambda en: en.copy(gT[:, fc, :], ps[pb][:, 0:TW]), [psb[pb]], [gT_b])
                        dve(lambda en: en.tensor_scalar(kk[:], k32[:], self.col(l, "rw_kk", fc), None, ALU.mult),
                            [k_b, cstb_], [kk_b])
                        act(lambda en: en.activation(out=x16[:], in_=kk[:], func=AF.Square), [kk_b], [x16_b])
                        pb = self.bank()
                        S.mm(ps[pb][:, 0:TW], blockones, x16[:], True, True, reads=[x16_b, cstb_], writes=[psb[pb]])
                        act(lambda en: en.activation(out=E1[:], in_=ps[pb][:, 0:TW], func=AF.Sqrt), [psb[pb]], [E1_b])
                        dve(lambda en: en.tensor_scalar(E1[:], E1[:], 1e-12, None, ALU.max), [E1_b], [E1_b])
                        dve(lambda en: en.reciprocal(E1[:], E1[:]), [E1_b], [E1_b])
                        dve(lambda en: en.tensor_tensor(out=kk[:], in0=kk[:], in1=E1[:], op=ALU.mult), [kk_b, E1_b], [kk_b])
                        dve(lambda en: en.tensor_scalar(kp[:], a32[:], -1.0, self.col(l, "rw_ka", fc), ALU.add, ALU.mult),
                            [a_b, cstb_], [kp_b])
                        dve(lambda en: en.scalar_tensor_tensor(out=kp[:], in0=kp[:], scalar=1.0, in1=k32[:], op0=ALU.add,
                                                               op1=ALU.mult), [kp_b, k_b], [kp_b])
                        dve(lambda en: en.tensor_tensor(out=bt[:], in0=kk[:], in1=a32[:], op=ALU.mult), [kk_b, a_b], [bt_b])
                        dve(lambda en: en.scalar_tensor_tensor(out=x16[:], in0=r32[:], scalar=self.col(l, "rw_rk", fc), in1=kp[:],
                                                               op0=ALU.mult, op1=ALU.mult), [r_b, kp_b, cstb_], [x16_b])
                        pb = self.bank()
                        S.mm(ps[pb][:, 0:TW], blockones, x16[:], True, True, reads=[x16_b, cstb_], writes=[psb[pb]])
                        dve(lambda en: en.tensor_tensor(out=bon[:, fc, :], in0=ps[pb][:, 0:TW], in1=v32[:], op=ALU.mult),
                            [psb[pb], v_b], [bon_b])
                        dve(lambda en: en.tensor_tensor_scan(out=Lc[:], data0=rmask[:], data1=sg[:], initial=0.0,
                                                             op0=ALU.mult, op1=ALU.add), [par_b, sg_b], [Lc_b])
                        Lc3 = Lc[:, :].rearrange("p (c t) -> p c t", t=64)
                        act(lambda en: en.activation(out=gC[:, fc, ti * 4:(ti + 1) * 4], in_=Lc3[:, :, 63], func=AF.Exp,
                                                     scale=sdec), [Lc_b], [gC_b])
                        act(lambda en: en.activation(out=E1[:], in_=Lc[:], func=AF.Exp, scale=sdec), [Lc_b], [E1_b])
                        dve(lambda en: en.tensor_tensor(out=ar[:, fc, 1, :], in0=r32[:], in1=E1[:], op=ALU.mult),
                            [r_b, E1_b], [ar_b])
                        dve(lambda en: en.tensor_tensor(out=E2[:], in0=Lc[:], in1=sg[:], op=ALU.subtract), [Lc_b, sg_b], [E2_b])
                        act(lambda en: en.activation(out=E2[:], in_=E2[:], func=AF.Exp, scale=sdec), [E2_b], [E2_b])
                        dve(lambda en: en.scalar_tensor_tensor(out=ar[:, fc, 0, :], in0=kk[:], scalar=-1.0, in1=E2[:],
                                                               op0=ALU.mult, op1=ALU.mult), [kk_b, E2_b], [ar_b])
                        act(lambda en: en.activation(out=E1[:], in_=Lc[:], func=AF.Exp, scale=-sdec), [Lc_b], [E1_b])
                        dve(lambda en: en.tensor_tensor(out=bk[:, fc, 0, :], in0=bt[:], in1=E1[:], op=ALU.mult),
                            [bt_b, E1_b], [bk_b])
                        dve(lambda en: en.tensor_tensor(out=bk[:, fc, 1, :], in0=kp[:], in1=E1[:], op=ALU.mult),
                            [kp_b, E1_b], [bk_b])
                        for a_i in range(2):
                            for h2 in range(2):
                                S.op("pool", lambda en: en.tensor_copy(ar_bd[h2 * 64:(h2 + 1) * 64, fc, h2, a_i, :],
                                                                       ar[h2 * 64:(h2 + 1) * 64, fc, a_i, :]),
                                     [ar_b], [bd_b])
                        for h2 in range(2):
                            S.op("pool", lambda en: en.tensor_copy(bt_bd[h2 * 64:(h2 + 1) * 64, fc, h2, :],
                                                                   bk[h2 * 64:(h2 + 1) * 64, fc, 0, :]), [bk_b], [bd_b])
                        E23 = E2[:, :].rearrange("p (c t) -> p c t", t=64)
                        dve(lambda en: en.tensor_tensor(out=E23, in0=Lc3[:, :, 63:64].to_broadcast([128, TW // 64, 64]), in1=Lc3,
                                                        op=ALU.subtract), [Lc_b], [E2_b])
                        act(lambda en: en.activation(out=E2[:], in_=E2[:], func=AF.Exp, scale=sdec), [E2_b], [E2_b])
                        dve(lambda en: en.tensor_tensor(out=bh[:, fc, :], in0=bt[:], in1=E2[:], op=ALU.mult),
                            [bt_b, E2_b], [tok_b])
                        dve(lambda en: en.tensor_tensor(out=kh[:, fc, :], in0=kp[:], in1=E2[:], op=ALU.mult),
                            [kp_b, E2_b], [tok_b])
                        act(lambda en: en.copy(vb[:, fc, :], v32[:]), [v_b], [tok_b])
                    S.barrier()
                with ExitStack() as _es:
                    tokm = _es.enter_context(self.tmp("tokm", [64, 2, 4, 256], BF16))
                    A1 = _es.enter_context(self.tmp("A1", [64, 8, 2, 64], BF16))
                    A2 = _es.enter_context(self.tmp("A2", [64, 8, 2, 64], BF16))
                    Nj = _es.enter_context(self.tmp("Nj", [64, 2, 8, 64], BF16))
                    Mj = _es.enter_context(self.tmp("Mj", [64, 2, 8, 64], BF16))
                    Pj = _es.enter_context(self.tmp("Pj", [64, 2, 8, 64], BF16))
                    AhT = _es.enter_context(self.tmp("AhT", [128, 4, 64], BF16))
                    W2 = _es.enter_context(self.tmp("W2", [64, 8, 64], BF16))
                    Uh = _es.enter_context(self.tmp("Uh", [64, 8, 64], F32))
                    Ub = _es.enter_context(self.tmp("Ub", [64, 8, 64], BF16))
                    Y32 = _es.enter_context(self.tmp("Y32", [64, 8, 64], F32))
                    Ysq = _es.enter_context(self.tmp("Ysq", [64, 8, 64], F32))
                    st8 = _es.enter_context(self.tmp("st8", [64, 2, 4, 4], F32))
                    ynT_g = [Buf(), Buf()]
                    if not hasattr(self, "_Sg_b"):
                        pass

                    def stream(hg):
                        tokm_b, A1_b, A2_b, AhT_b, W2_b, Uh_b, Ub_b, Y_b, Ysq_b, st8_b = [Buf() for _ in range(10)]
                        Nj_b = [Buf(), Buf()]
                        Mj_b = [Buf(), Buf()]
                        Pj_b = [Buf(), Buf()]
                        Sg_b = S_gb[hg]
                        hs = list(range(4 * hg, 4 * hg + 4))
                        fcs = [2 * hg, 2 * hg + 1]
                        H0 = 4 * hg
                        f0 = 2 * hg

                        def hv(t3, *idx):
                            return t3

                        for ci in range(TW // 64):
                            cs = ci * 64
                            cg = ti * 4 + ci
                            srcs = [(vb, tok_b, None), (bh, tok_b, None), (kh, tok_b, None), (ar, ar_b, 0)]
                            pb = self.bank()
                            pv = ps[pb][:, :].bitcast(BF16)
                            for ai in range(4):
                                a_t, a_b_, sub = srcs[ai]
                                for fl in range(2):
                                    fc = f0 + fl
                                    src = a_t[:, fc, cs:cs + 64] if sub is None else a_t[:, fc, sub, cs:cs + 64]
                                    S.op("pe", lambda en: en.transpose(pv[0:64, ai * 256 + fl * 128: ai * 256 + (fl + 1) * 128],
                                                                       src, identb), reads=[a_b_, cstb_], writes=[psb[pb]])
                            self.evac("act" if hg == 0 else "dve", tokm[:, hg, :, :],
                                      pv[0:64, :].rearrange("p (a f) -> p a f", f=256), [psb[pb]], [tokm_b])
                            yield
                            Vt, Bt, Kt, At = tokm[:, hg, 0, :], tokm[:, hg, 1, :], tokm[:, hg, 2, :], tokm[:, hg, 3, :]
                            p1 = self.bank()
                            p2 = self.bank()
                            p3 = self.bank()
                            for fl in range(2):
                                fc = f0 + fl
                                rhs_bd = ar_bd[:, fc, :, :, cs:cs + 64]
                                S.mm(ps[p1][0:64, fl * 256:(fl + 1) * 256], bk[:, fc, 0, cs:cs + 64], rhs_bd, True, True,
                                     reads=[bk_b, bd_b], writes=[psb[p1]])
                                S.mm(ps[p2][0:64, fl * 256:(fl + 1) * 256], bk[:, fc, 1, cs:cs + 64], rhs_bd, True, True,
                                     reads=[bk_b, bd_b], writes=[psb[p2]])
                                S.mm(ps[p3][0:64, fl * 128:(fl + 1) * 128], ar[:, fc, 0, cs:cs + 64], bt_bd[:, fc, :, cs:cs + 64],
                                     True, True, reads=[ar_b, bd_b], writes=[psb[p3]])
                            yield
                            g4 = slice(H0, H0 + 4)
                            dve(lambda en: en.tensor_tensor(out=A1[:, g4, :, :].rearrange("p h a t -> p (h a t)"),
                                                            in0=ps[p1][0:64, :], in1=mask2, op=ALU.mult),
                                [psb[p1], cstb_], [A1_b])
                            dve(lambda en: en.tensor_tensor(out=A2[:, g4, :, :].rearrange("p h a t -> p (h a t)"),
                                                            in0=ps[p2][0:64, :], in1=mask2, op=ALU.mult),
                                [psb[p2], cstb_], [A2_b])
                            dve(lambda en: en.tensor_tensor(out=Nj[:, 0, g4, :].rearrange("p h t -> p (h t)"), in0=ps[p3][0:64, 0:256],
                                                            in1=mask3[:, 0:256], op=ALU.mult), [psb[p3], cstb_], [Nj_b[0]])
                            act(lambda en: en.copy(Mj[:, 0, g4, :], A1[:, g4, 0, :]), [A1_b], [Mj_b[0]])
                            dve(lambda en: en.tensor_tensor(out=Pj[:, 0, g4, :].rearrange("p h t -> p (h t)"),
                                                            in0=Mj[:, 0, g4, :].rearrange("p h t -> p (h t)"), in1=I8[:, 0:256],
                                                            op=ALU.add), [Mj_b[0], cstb_], [Pj_b[0]])
                            yield
                            cur = 0
                            pc = 0
                            for j in range(1, 6):
                                nxt = 1 - cur
                                pn = self.bank()
                                for hl, h in enumerate(hs):
                                    S.mm(ps[pn][0:64, hl * 64:(hl + 1) * 64], Mj[:, cur, h, :], Nj[:, cur, h, :], True, True,
                                         reads=[Mj_b[cur], Nj_b[cur]], writes=[psb[pn]])
                                if j < 5:
                                    pm = self.bank()
                                    for hl, h in enumerate(hs):
                                        S.mm(ps[pm][0:64, hl * 64:(hl + 1) * 64], Nj[:, cur, h, :], Mj[:, cur, h, :], True, True,
                                             reads=[Mj_b[cur], Nj_b[cur]], writes=[psb[pm]])
                                yield
                                self.evac("act", Nj[:, nxt, g4, :].rearrange("p h t -> p (h t)"), ps[pn][0:64, 0:256], [psb[pn]],
                                          [Nj_b[nxt]])
                                if j < 5:
                                    self.evac("dve", Mj[:, nxt, g4, :].rearrange("p h t -> p (h t)"), ps[pm][0:64, 0:256], [psb[pm]],
                                              [Mj_b[nxt]])
                                pp = self.bank()
                                for hl, h in enumerate(hs):
                                    S.mm(ps[pp][0:64, hl * 64:(hl + 1) * 64], Nj[:, nxt, h, :], Pj[:, pc, h, :], True, True,
                                         reads=[Nj_b[nxt], Pj_b[pc]], writes=[psb[pp]])
                                yield
                                dve(lambda en: en.tensor_tensor(out=Pj[:, 1 - pc, g4, :].rearrange("p h t -> p (h t)"),
                                                                in0=ps[pp][0:64, 0:256],
                                                                in1=Pj[:, pc, g4, :].rearrange("p h t -> p (h t)"), op=ALU.add),
                                    [psb[pp], Pj_b[pc]], [Pj_b[1 - pc]])
                                pc = 1 - pc
                                cur = nxt
                            TT = Pj[:, pc, :, :]
                            TT_b = Pj_b[pc]
                            pa = self.bank()
                            pw = self.bank()
                            for hl, h in enumerate(hs):
                                fl, hb = hl // 2, (hl % 2) * 64
                                S.mm(ps[pa][hb:hb + 64, fl * 64:(fl + 1) * 64], At[:, hl * 64:(hl + 1) * 64], TT[:, h, :], True, True,
                                     reads=[tokm_b, TT_b], writes=[psb[pa]])
                                S.mm(ps[pw][0:64, hl * 64:(hl + 1) * 64], A2[:, h, 0, :], Vt[:, hl * 64:(hl + 1) * 64], True, True,
                                     reads=[A2_b, tokm_b], writes=[psb[pw]])
                            yield
                            self.evac("act", AhT[:, f0:f0 + 2, :].rearrange("p f t -> p (f t)"), ps[pa][:, 0:128], [psb[pa]], [AhT_b])
                            self.evac("dve", W2[:, g4, :].rearrange("p h v -> p (h v)"), ps[pw][0:64, 0:256], [psb[pw]], [W2_b])
                            pu = self.bank()
                            for hl, h in enumerate(hs):
                                S.mm(ps[pu][0:64, hl * 64:(hl + 1) * 64], TT[:, h, :], W2[:, h, :], True, True,
                                     reads=[TT_b, W2_b], writes=[psb[pu]])
                            yield
                            self.evac("act", Uh[:, g4, :].rearrange("p h v -> p (h v)"), ps[pu][0:64, 0:256], [psb[pu]], [Uh_b])
                            pu2 = self.bank()
                            for fl in range(2):
                                fc = f0 + fl
                                S.mm(ps[pu2][0:64, fl * 128:(fl + 1) * 128], AhT[:, fc, :], Sbd[:, fc, :, :], True, True,
                                     reads=[AhT_b, Sg_b], writes=[psb[pu2]])
                            yield
                            dve(lambda en: en.tensor_tensor(out=Ub[:, g4, :].rearrange("p h v -> p (h v)"), in0=ps[pu2][0:64, 0:256],
                                                            in1=Uh[:, g4, :].rearrange("p h v -> p (h v)"), op=ALU.add),
                                [psb[pu2], Uh_b], [Ub_b])
                            py = self.bank()
                            pS = self.bank()
                            for fl in range(2):
                                fc = f0 + fl
                                S.mm(ps[py][0:64, fl * 128:(fl + 1) * 128], ar[:, fc, 1, cs:cs + 64], Sbd[:, fc, :, :], True, False,
                                     reads=[ar_b, Sg_b], writes=[psb[py]])
                                for hl in (2 * fl, 2 * fl + 1):
                                    h = H0 + hl
                                    yo = ps[py][0:64, hl * 64:(hl + 1) * 64]
                                    S.mm(yo, A1[:, h, 1, :], Ub[:, h, :], False, False, reads=[A1_b, Ub_b], writes=[psb[py]])
                                    S.mm(yo, A2[:, h, 1, :], Vt[:, hl * 64:(hl + 1) * 64], False, hl == 2 * fl + 1,
                                         reads=[A2_b, tokm_b], writes=[psb[py]])
                            for hl, h in enumerate(hs):
                                fl, hb = hl // 2, (hl % 2) * 64
                                so = ps[pS][hb:hb + 64, fl * 64:(fl + 1) * 64]
                                S.mm(so, Bt[:, hl * 64:(hl + 1) * 64], Ub[:, h, :], True, False, reads=[tokm_b, Ub_b], writes=[psb[pS]])
                                S.mm(so, Kt[:, hl * 64:(hl + 1) * 64], Vt[:, hl * 64:(hl + 1) * 64], False, True, reads=[tokm_b],
                                     writes=[psb[pS]])
                            yield
                            for fl in range(2):
                                fc = f0 + fl
                                dve(lambda en: en.scalar_tensor_tensor(out=S32[:, fc, :], in0=S32[:, fc, :], scalar=gC[:, fc, cg:cg + 1],
                                                                       in1=ps[pS][:, fl * 64:(fl + 1) * 64], op0=ALU.mult, op1=ALU.add),
                                    [psb[pS], gC_b, Sg_b], [Sg_b])
                            for h2 in range(2):
                                act(lambda en: en.copy(Sbd[h2 * 64:(h2 + 1) * 64, f0:f0 + 2, h2, :],
                                                       S32[h2 * 64:(h2 + 1) * 64, f0:f0 + 2, :]), [Sg_b], [Sg_b])
                            Yg = Y32[:, g4, :]
                            Ysg = Ysq[:, g4, :]
                            sg8 = st8[:, hg, :, :]
                            act(lambda en: en.copy(Yg.rearrange("p h v -> p (h v)"), ps[py][0:64, 0:256]), [psb[py]], [Y_b])
                            act(lambda en: en.activation(out=Ysg, in_=Yg, func=AF.Square), [Y_b], [Ysq_b])
                            dve(lambda en: en.tensor_reduce(out=sg8[:, 0, :], in_=Yg, axis=AX.X, op=ALU.add), [Y_b], [st8_b])
                            dve(lambda en: en.tensor_reduce(out=sg8[:, 1, :], in_=Ysg, axis=AX.X, op=ALU.add), [Ysq_b, st8_b], [st8_b])
                            dve(lambda en: en.tensor_scalar(sg8[:, 0, :], sg8[:, 0, :], 1.0 / 64, None, ALU.mult), [st8_b], [st8_b])
                            dve(lambda en: en.tensor_tensor(out=sg8[:, 2, :], in0=sg8[:, 0, :], in1=sg8[:, 0, :], op=ALU.mult),
                                [st8_b], [st8_b])
                            dve(lambda en: en.scalar_tensor_tensor(out=sg8[:, 3, :], in0=sg8[:, 1, :], scalar=1.0 / 64, in1=sg8[:, 2, :],
                                                                   op0=ALU.mult, op1=ALU.subtract), [st8_b], [st8_b])
                            act(lambda en: en.activation(out=sg8[:, 3, :], in_=sg8[:, 3, :], func=AF.Sqrt,
                                                         bias=self.eps_col(RW_GN_EPS)[0:64, :]), [st8_b, cstb_], [st8_b])
                            dve(lambda en: en.reciprocal(sg8[:, 3, :], sg8[:, 3, :]), [st8_b], [st8_b])
                            yield
                            dve(lambda en: en.tensor_tensor(out=Yg, in0=Yg,
                                                            in1=sg8[:, 0, :].unsqueeze(2).to_broadcast([64, 4, 64]), op=ALU.subtract),
                                [Y_b, st8_b], [Y_b])
                            dve(lambda en: en.tensor_tensor(out=Yg, in0=Yg,
                                                            in1=sg8[:, 3, :].unsqueeze(2).to_broadcast([64, 4, 64]), op=ALU.mult),
                                [Y_b, st8_b], [Y_b])
                            pt = self.bank()
                            Yf = Y32[:, :, :].rearrange("p h v -> p (h v)")
                            for fl in range(2):
                                fc = f0 + fl
                                S.op("pe", lambda en: en.transpose(ps[pt][:, fl * 64:(fl + 1) * 64], Yf[:, fc * 128:(fc + 1) * 128],
                                                                   ident32), reads=[Y_b, cstb_], writes=[psb[pt]])
                            yield
                            act(lambda en: en.copy(ynT[:, f0:f0 + 2, cs:cs + 64], ps[pt][:, 0:128].rearrange("p (f t) -> p f t", t=64)),
                                [psb[pt]], [ynT_g[hg]])
                            yield

                    gens = [stream(0), stream(1)]
                    alive = [True, True]
                    while any(alive):
                        for gi in range(2):
                            if alive[gi]:
                                try:
                                    next(gens[gi])
                                except StopIteration:
                                    alive[gi] = False
                    for fc in range(4):
                        dve(lambda en: en.tensor_scalar(ynT[:, fc, :], ynT[:, fc, :], self.col(l, "rw_ln_g", fc),
                                                        self.col(l, "rw_ln_b", fc), ALU.mult, ALU.add),
                            [ynT_b, ynT_g[0], ynT_g[1], cstb_], [ynT_b, ynT_g[0], ynT_g[1]])
                        dve(lambda en: en.tensor_tensor(out=ynT[:, fc, :], in0=ynT[:, fc, :], in1=bon[:, fc, :], op=ALU.add),
                            [ynT_b, bon_b], [ynT_b])
                        dve(lambda en: en.tensor_tensor(out=yrwT[:, fc, t0:t0 + TW], in0=ynT[:, fc, :], in1=gT[:, fc, :], op=ALU.mult),
                            [ynT_b, gT_b], [yrw_b])
                    S.barrier()

    def phase_output(self):
        S = self.S
        nc = self.nc
        ones = self.C("ones")
        ident = self.C("ident", bf=False)
        with ExitStack() as _es:
            oh = _es.enter_context(self.tmp("oh", [128, DC, 512], F32))
            osq = _es.enter_context(self.tmp("osq", [128, DC, 512], BF16))
            orr = _es.enter_context(self.tmp("orr", [128, 512], F32))
            oo = _es.enter_context(self.tmp("oo", [128, 2, D], F32))
            oh_b = Buf()
            osq_b = Buf()
            or_b = Buf()
            oo_b = [Buf(), Buf()]
            out_b = Buf()
            for tt in range(NTT):
                pb = 0
                S.dma("sp", oh[:], self.hT.rearrange("d p t -> p d t")[:, :, tt * 512:(tt + 1) * 512],
                      reads=[self.hT_b[dc][tt] for dc in range(DC)], writes=[oh_b])
                S.op("act", lambda en: en.activation(out=osq[:], in_=oh[:], func=AF.Square), reads=[oh_b], writes=[osq_b])
                for dc in range(DC):
                    S.mm(self.ps[pb][:, :], ones, osq[:, dc, :], dc == 0, dc == DC - 1, reads=[osq_b, self.cst_b],
                         writes=[self.ps_b[pb]])
                S.op("act", lambda en: en.activation(out=orr[:], in_=self.ps[pb][:, :], func=AF.Sqrt, scale=1.0 / D,
                                                     bias=self.eps_col(RMS_EPS)),
                     reads=[self.ps_b[pb], self.cst_b], writes=[or_b])
                S.op("dve", lambda en: en.reciprocal(orr[:], orr[:]), reads=[or_b], writes=[or_b])
                for dc in range(DC):
                    S.op("dve", lambda en, dc=dc: en.scalar_tensor_tensor(
                        out=oh[:, dc, :], in0=oh[:, dc, :], scalar=self.col(0, "norm_out", dc), in1=orr[:],
                        op0=ALU.mult, op1=ALU.mult), reads=[oh_b, or_b, self.cst_b], writes=[oh_b])
                for ts in range(4):
                    k = ts % 2
                    for g in range(4):
                        pb2 = 1 + (ts * 4 + g) % 7
                        for jj in range(4):
                            dc = g * 4 + jj
                            S.op("pe", lambda en, dc=dc, jj=jj, pb2=pb2: en.transpose(
                                self.ps[pb2][:, jj * 128:(jj + 1) * 128], oh[:, dc, ts * 128:(ts + 1) * 128], ident),
                                reads=[oh_b, self.cst_b], writes=[self.ps_b[pb2]])
                        dst = oo[:, k, g * 512:(g + 1) * 512]
                        if g % 2 == 0:
                            S.op("act", lambda en, dst=dst, pb2=pb2: en.copy(dst, self.ps[pb2][:, :]),
                                 reads=[self.ps_b[pb2]], writes=[oo_b[k]])
                        else:
                            S.op("dve", lambda en, dst=dst, pb2=pb2: en.tensor_copy(dst, self.ps[pb2][:, :]),
                                 reads=[self.ps_b[pb2]], writes=[oo_b[k]])
                    r0 = tt * 512 + ts * 128
                    S.dma("sp", self.out[r0:r0 + 128, :], oo[:, k, :], reads=[oo_b[k]], writes=[out_b])
            S.barrier()


def _colpack(inp):
    cp = np.zeros((NL, 128, NCP), np.float32)

    def put(name, vec, l):
        v = np.asarray(vec, np.float32).reshape(-1, 128).T
        cp[l, :, CP[name]:CP[name] + v.shape[1]] = v

    for l in range(NL):
        put("norm_mix", inp["norm_mix"][l], l)
        put("norm_ffn", inp["norm_ffn"][l], l)
        put("gate_b", inp["gate_b"][l].reshape(-1), l)
        put("rw_mu", inp["rw_mu"][l], l)
        for n in ("rw_w0", "rw_a0", "rw_kk", "rw_ka", "rw_ln_g", "rw_ln_b"):
            put(n, inp[n][l], l)
        put("rw_rk", inp["rw_rk"][l].reshape(-1), l)
        put("norm_out", inp["norm_out"], l)
    return cp


def _tile_w(w):
    w = np.asarray(w, dtype=np.float32)
    lead = w.shape[:-2]
    K, N = w.shape[-2:]
    w = w.reshape(lead + (K // 128, 128, N // 128, 128))
    nl = len(lead)
    perm = tuple(range(nl)) + (nl + 2, nl + 1, nl + 0, nl + 3)
    return np.ascontiguousarray(w.transpose(perm)).reshape(lead + (N // 128, 128, K))


def make_in_map(inp, b):
    m = {"x": np.ascontiguousarray(inp["x"][b]), "cst": make_consts(), "cpk": _colpack(inp)}
    for n in ("gm_ln_g", "gm_ln_b", "gm_ws", "rw_w2", "rw_a2", "rw_g2", "router_w", "router_b"):
        m[n] = np.ascontiguousarray(inp[n], dtype=np.float32)
    for n in ("w_in", "p_gm", "p_rw", "p_sb", "w_o", "ffn_w1", "ffn_w3", "ffn_w2", "moe_w1", "moe_w3", "moe_w2"):
        m[n] = _tile_w(inp[n])
    m["gm_bs"] = np.ascontiguousarray(inp["gm_bs"], dtype=np.float32).reshape(NL, 1024)
    return m


_NC_CACHE = {}


def kernel(**inputs):
    inp = {k: np.asarray(v) for k, v in inputs.items()}
    if "nc" not in _NC_CACHE:
        _NC_CACHE["nc"] = Builder().build()
    nc = _NC_CACHE["nc"]
    base = make_in_map(inp, 0)
    in_maps = []
    for b in range(8):
        m = dict(base)
        m["x"] = np.ascontiguousarray(inp["x"][b], dtype=np.float32)
        in_maps.append(m)
    res = run_bass_kernel_spmd(nc, in_maps, core_ids=list(range(8)))
    return np.stack([r["out"] for r in res.results], axis=0).astype(np.float32)
```

```python
import numpy as np
from contextlib import ExitStack
import concourse.bass as bass
import concourse.mybir as mybir
from concourse.bass_utils import run_bass_kernel_spmd

F32 = mybir.dt.float32
BF16 = mybir.dt.bfloat16
AF = mybir.ActivationFunctionType
ALU = mybir.AluOpType
AX = mybir.AxisListType

NL = 2
D = 2048
T = 2048
DC = 16
NTT = 4
N_IN = 12032
GM_OFF, RW_OFF, SB_OFF, GATE_OFF = 0, 1024, 2816, 5888
D_FF = 5632
FC = 44
NE = 8
D_FE = 2816
FCE = 22
RMS_EPS = 1e-6
LN_EPS = 1e-5
RW_GN_EPS = 64e-5

CP = {}
_o = 0
for _n, _w in [("norm_mix", 16), ("norm_ffn", 16), ("gate_b", 48), ("rw_mu", 14), ("rw_w0", 4), ("rw_a0", 4),
               ("rw_kk", 4), ("rw_ka", 4), ("rw_rk", 4), ("rw_ln_g", 4), ("rw_ln_b", 4), ("norm_out", 16),
               ("rw_1mka", 4)]:
    CP[_n] = _o
    _o += _w
NCP = _o

CS = {}
_o = 0
for _n, _w in [("ident", 128), ("ones", 128), ("blockones", 128), ("m_le", 128), ("m_lt", 128), ("m_ge", 128),
               ("m_gt", 128), ("zeros", 128), ("neg_ge", 128), ("neg_ones", 128),
               ("mask2", 512), ("mask3", 512), ("I8", 512)]:
    CS[_n] = _o
    _o += _w
NCST = _o


def make_consts():
    c = np.zeros((128, NCST), np.float32)
    p = np.arange(128)[:, None]
    f = np.arange(128)[None, :]
    c[:, CS["ident"]:CS["ident"] + 128] = (p == f)
    c[:, CS["ones"]:CS["ones"] + 128] = 1.0
    c[:, CS["blockones"]:CS["blockones"] + 128] = ((p // 64) == (f // 64))
    c[:, CS["m_le"]:CS["m_le"] + 128] = (p <= f)
    c[:, CS["m_lt"]:CS["m_lt"] + 128] = (p < f)
    c[:, CS["m_ge"]:CS["m_ge"] + 128] = (p >= f)
    c[:, CS["m_gt"]:CS["m_gt"] + 128] = (p > f)
    c[:, CS["neg_ge"]:CS["neg_ge"] + 128] = -1.0 * (p >= f)
    c[:, CS["neg_ones"]:CS["neg_ones"] + 128] = -1.0
    p64 = np.arange(128)[:, None] % 64
    f64 = np.arange(64)[None, :]
    lt = (p64 < f64).astype(np.float32)
    le = (p64 <= f64).astype(np.float32)
    gt = (p64 > f64).astype(np.float32)
    eq = (p64 == f64).astype(np.float32)
    c[:, CS["mask2"]:CS["mask2"] + 512] = np.tile(np.concatenate([lt, le], axis=1), (1, 4))
    c[:, CS["mask3"]:CS["mask3"] + 512] = np.tile(gt, (1, 8))
    c[:, CS["I8"]:CS["I8"] + 512] = np.tile(eq, (1, 8))
    return c


class Buf:
    __slots__ = ("name", "w", "r")

    def __init__(self, name=""):
        self.name = name
        self.w = []
        self.r = []


def _prune(toks):
    d = {}
    for s, v in toks:
        if d.get(s, 0) < v:
            d[s] = v
    return list(d.items())


class Sched:
    NRING = 20

    def __init__(self, nc, same_eng_sync=True):
        self.nc = nc
        self.engs = {"pe": nc.tensor, "act": nc.scalar, "dve": nc.vector, "pool": nc.gpsimd, "sp": nc.sync}
        self.sems = []
        self.esem = {}
        self.ecnt = {}
        self.known = {}
        for e in self.engs:
            self.esem[e] = self._new_sem("e_" + e)
            self.ecnt[e] = 0
            self.known[e] = {}
        self.ring = {}
        self.dcnt = {}
        self.semval = {}
        for e in ("sp", "act", "pool"):
            self.ring[e] = [self._new_sem("d_%s%d" % (e, i)) for i in range(self.NRING)]
            self.dcnt[e] = 0
            for s in self.ring[e]:
                self.semval[s] = 0
        self.same = same_eng_sync
        self.n_inst = 0
        self.max_pool_inflight = 4
        self.pool_hist = []

    def _new_sem(self, name):
        h = self.nc.semaphore(name).__enter__()
        self.sems.append(h)
        return len(self.sems) - 1

    def _wait(self, e, toks):
        eng = self.engs[e]
        kn = self.known[e]
        own = self.esem[e]
        for s, v in _prune(toks):
            if kn.get(s, 0) >= v:
                continue
            if s == own and (e == "pe" or not self.same):
                continue
            eng.wait_ge(self.sems[s], v)
            kn[s] = v

    def _deps(self, reads, writes):
        toks = []
        for b in reads:
            toks += b.w
        for b in writes:
            toks += b.w
            toks += b.r
        return toks

    def _commit(self, tok, reads, writes):
        for b in writes:
            b.w = [tok]
            b.r = []
        for b in reads:
            if b not in writes:
                b.r = _prune(b.r + [tok])

    def op(self, e, fn, reads=(), writes=()):
        self._wait(e, self._deps(reads, writes))
        inst = fn(self.engs[e])
        self.ecnt[e] += 1
        tok = (self.esem[e], self.ecnt[e])
        inst.then_inc(self.sems[tok[0]], 1)
        self._commit(tok, reads, writes)
        self.n_inst += 1
        return tok

    def mm(self, out, lhsT, rhs, start, stop, reads=(), writes=(), **kw):
        return self.op("pe", lambda en: en.matmul(out, lhsT, rhs, start=start, stop=stop, **kw), reads, writes)

    def dma(self, q, out, in_, reads=(), writes=(), **kw):
        toks = self._deps(reads, writes)
        i = self.dcnt[q] % self.NRING
        self.dcnt[q] += 1
        s = self.ring[q][i]
        prev = self.semval[s]
        if prev > 0:
            toks.append((s, prev))
        if q == "pool" and self.max_pool_inflight:
            hist = self.pool_hist
            if len(hist) >= self.max_pool_inflight:
                toks.append(hist[-self.max_pool_inflight])
        self._wait(q, toks)
        inst = self.engs[q].dma_start(out=out, in_=in_, **kw)
        inst.then_inc(self.sems[s], 16)
        self.semval[s] = prev + 16
        tok = (s, prev + 16)
        if q == "pool":
            self.pool_hist.append(tok)
        self._commit(tok, reads, writes)
        self.n_inst += 1
        return tok

    def all_tokens(self):
        toks = [(self.esem[e], self.ecnt[e]) for e in self.engs if self.ecnt[e] > 0]
        toks += [(s, v) for s, v in self.semval.items() if v > 0]
        return toks

    def barrier(self, engines=None):
        toks = self.all_tokens()
        for e in (engines or self.engs):
            kn = self.known[e]
            eng = self.engs[e]
            for s, v in toks:
                if s == self.esem[e]:
                    continue
                if kn.get(s, 0) >= v:
                    continue
                eng.wait_ge(self.sems[s], v)
                kn[s] = v


class Builder:
    def __init__(self, opts=None):
        self.o = dict(layers=NL, mixer=True, ffn=True, gm=True, rw=True, sb=True, dbg=())
        if opts:
            self.o.update(opts)
        self.nc = bass.Bass("TRN2", target_bir_lowering=False, dynamic_dma_scratch_size=8192)
        self.ctx = []

    def sb(self, name, shape, dt):
        t = self.nc.sbuf_tensor("s_" + name, shape, dt)
        h = t.__enter__()
        return h

    def tmp(self, name, shape, dt):
        self._uid = getattr(self, "_uid", 0) + 1
        return self.nc.sbuf_tensor("%s_%d" % (name, self._uid), shape, dt)

    def din(self, name, shape, dt=F32):
        return self.nc.dram_tensor(name, list(shape), dt, kind="ExternalInput").ap()

    def build(self):
        nc = self.nc
        o = self.o
        S = self.S = Sched(nc)
        I = self.I = {}
        I["x"] = self.din("x", [T, D])
        I["cst"] = self.din("cst", [128, NCST])
        I["cpk"] = self.din("cpk", [NL, 128, NCP])
        I["w_in"] = self.din("w_in", [NL, N_IN // 128, 128, D])
        I["gm_ln_g"] = self.din("gm_ln_g", [NL, 512])
        I["gm_ln_b"] = self.din("gm_ln_b", [NL, 512])
        I["gm_ws"] = self.din("gm_ws", [NL, 8, 128, 128])
        I["gm_bs"] = self.din("gm_bs", [NL, 1024])
        I["rw_w2"] = self.din("rw_w2", [NL, 64, 512])
        I["rw_a2"] = self.din("rw_a2", [NL, 64, 512])
        I["rw_g2"] = self.din("rw_g2", [NL, 128, 512])
        I["p_gm"] = self.din("p_gm", [NL, DC, 128, 512])
        I["p_rw"] = self.din("p_rw", [NL, DC, 128, 512])
        I["p_sb"] = self.din("p_sb", [NL, DC, 128, 1024])
        I["w_o"] = self.din("w_o", [NL, DC, 128, D])
        I["ffn_w1"] = self.din("ffn_w1", [1, FC, 128, D])
        I["ffn_w3"] = self.din("ffn_w3", [1, FC, 128, D])
        I["ffn_w2"] = self.din("ffn_w2", [1, DC, 128, D_FF])
        I["router_w"] = self.din("router_w", [1, D, NE])
        I["router_b"] = self.din("router_b", [1, NE])
        I["moe_w1"] = self.din("moe_w1", [1, NE, FCE, 128, D])
        I["moe_w3"] = self.din("moe_w3", [1, NE, FCE, 128, D])
        I["moe_w2"] = self.din("moe_w2", [1, NE, DC, 128, D_FE])
        self.out = nc.dram_tensor("out", [T, D], F32, kind="ExternalOutput").ap()
        self.hT = nc.dram_tensor("hT_scr", [DC, 128, T], F32, kind="Internal").ap()
        self.mT = nc.dram_tensor("mT_scr", [DC, 128, T], BF16, kind="Internal").ap()
        self.hT_b = [[Buf("hT%d_%d" % (dc, tt)) for tt in range(NTT)] for dc in range(DC)]
        self.mT_b = [Buf("mT%d" % dc) for dc in range(DC)]
        self.dbg_out = {}
        for name, shape, dt in o["dbg"]:
            self.dbg_out[name] = nc.dram_tensor("dbg_" + name, list(shape), dt, kind="ExternalOutput").ap()

        self.cst = self.sb("cst", [128, NCST], F32)
        self.cstb = self.sb("cstb", [128, NCST], BF16)
        self.cpk = self.sb("cpk", [128, NL, NCP], F32)
        self.nT = self.sb("nT", [128, DC, T], BF16)
        self.nT_b = [Buf("nT%d" % tt) for tt in range(NTT)]
        self.wbig = [self.sb("wbig%d" % i, [128, 5632], BF16) for i in range(2)]
        self.wbig_b = [Buf("wb%d" % i) for i in range(2)]
        self.wsml = [self.sb("wsml%d" % i, [128, 2048], BF16) for i in range(6)]
        self.wsml_b = [Buf("wsm%d" % i) for i in range(6)]
        self.wi_big = 0
        self.wi_sml = 0
        self.ps = [nc.psum_tensor("psb%d" % i, [128, 512], F32).__enter__() for i in range(8)]
        self.ps_b = [Buf("ps%d" % i) for i in range(8)]
        self.cst_b = Buf("cst")

        self._eps = {}
        for val in (RMS_EPS, LN_EPS, RW_GN_EPS, 1.0):
            t = self.sb("eps%d" % len(self._eps), [128, 1], F32)
            S.op("pool", lambda en, t=t, val=val: en.memset(t[:], float(val)), writes=[self.cst_b])
            self._eps[val] = t
        S.dma("sp", self.cst[:], I["cst"][:, :], writes=[self.cst_b])
        S.dma("sp", self.cpk[:], I["cpk"].rearrange("l p c -> p l c"), writes=[self.cst_b])
        S.op("dve", lambda en: en.tensor_copy(self.cstb[:], self.cst[:]), reads=[self.cst_b], writes=[self.cst_b])
        S.barrier()

        self.phase_input()
        for l in range(o["layers"]):
            if o["mixer"]:
                self.phase_norm(l, "norm_mix")
                self.phase_mixer(l)
            if o["ffn"] and not (o.get("only_moe") and l % 2 == 0):
                if l % 2 == 0:
                    for _rep in range(o.get("ffn_rep", 1)):
                        self.phase_norm(l, "norm_ffn")
                        self.phase_ffn_dense(l // 2)
                else:
                    self.phase_moe(l, l // 2)
        self.phase_output()
        S.barrier()
        return nc

    def C(self, name, n=128, rows=128, bf=True):
        t = self.cstb if bf else self.cst
        return t[0:rows, CS[name]:CS[name] + n]

    def col(self, l, name, j):
        return self.cpk[:, l, CP[name] + j:CP[name] + j + 1]

    def wload(self, src_ap, kc, ncol, extra_reads=()):
        n = kc * ncol
        if n <= 2048:
            i = self.wi_sml % 6
            self.wi_sml += 1
            slot, b = self.wsml[i], self.wsml_b[i]
        else:
            i = self.wi_big % 2
            self.wi_big += 1
            slot, b = self.wbig[i], self.wbig_b[i]
        nblk = ncol // 128
        if nblk == 1:
            dst = slot[:, 0:n]
            view = slot[:, 0:n].rearrange("p (k c) -> p k c", c=128)
        else:
            dst = slot[:, 0:n].rearrange("p (b x) -> p b x", b=nblk)
            view = slot[:, 0:n].rearrange("p (b k c) -> p k b c", b=nblk, c=128)
        self.S.dma("pool", dst, src_ap, reads=list(extra_reads), writes=[b], max_dma_last_dim=8192)
        return view, b

    @staticmethod
    def wsrc(wr, c0, ncol):
        cb = c0 // 128
        nblk = ncol // 128
        if nblk == 1:
            return wr[cb]
        return wr[cb:cb + nblk].rearrange("b p x -> p b x")

    def phase_input(self):
        S = self.S
        nc = self.nc
        with self.tmp("xin", [128, 2, D], F32) as xin, self.tmp("xst", [128, 2, DC, 128], F32) as xst:
            xin_b = [Buf(), Buf()]
            xst_b = [Buf(), Buf()]
            ident = self.C("ident", bf=False)
            for ti in range(16):
                k = ti % 2
                S.dma("sp", xin[:, k, :], self.I["x"][ti * 128:(ti + 1) * 128, :], writes=[xin_b[k]])
                for g in range(4):
                    pb = (ti * 4 + g) % 8
                    for j in range(4):
                        dc = g * 4 + j
                        S.op("pe", lambda en, dc=dc, j=j, pb=pb: en.transpose(
                            self.ps[pb][:, j * 128:(j + 1) * 128], xin[:, k, dc * 128:(dc + 1) * 128], ident),
                            reads=[xin_b[k], self.cst_b], writes=[self.ps_b[pb]])
                    eng = "act" if g % 2 == 0 else "dve"
                    dst = xst[:, k, g * 4:(g + 1) * 4, :]
                    src = self.ps[pb][:, :].rearrange("p (j t) -> p j t", t=128)
                    if eng == "act":
                        S.op("act", lambda en, dst=dst, src=src: en.copy(dst, src), reads=[self.ps_b[pb]],
                             writes=[xst_b[k]])
                    else:
                        S.op("dve", lambda en, dst=dst, src=src: en.tensor_copy(dst, src), reads=[self.ps_b[pb]],
                             writes=[xst_b[k]])
                tt = ti // 4
                S.dma("sp", self.hT.rearrange("d p t -> p d t")[:, :, ti * 128:(ti + 1) * 128], xst[:, k, :, :],
                      reads=[xst_b[k]], writes=[self.hT_b[dc][tt] for dc in range(DC)])
            S.barrier()

    def phase_norm(self, l, gname, router=None):
        S = self.S
        nc = self.nc
        ones = self.C("ones")
        with self.tmp("nh", [128, 1, DC, 512], F32) as nh, self.tmp("nsq", [128, DC, 512], BF16) as nsq, \
                self.tmp("nr", [128, 2, 512], F32) as nr:
            nh_b = [Buf(), Buf()]
            nsq_b = Buf()
            nr_b = [Buf(), Buf()]
            for tt in range(NTT):
                k = 0
                pb = tt % 2
                S.dma("sp", nh[:, k, :, :], self.hT.rearrange("d p t -> p d t")[:, :, tt * 512:(tt + 1) * 512],
                      reads=[self.hT_b[dc][tt] for dc in range(DC)], writes=[nh_b[k]])
                S.op("act", lambda en: en.activation(out=nsq[:], in_=nh[:, k, :, :], func=AF.Square),
                     reads=[nh_b[k]], writes=[nsq_b])
                for dc in range(DC):
                    S.mm(self.ps[pb][:, :], ones, nsq[:, dc, :], dc == 0, dc == DC - 1,
                         reads=[nsq_b, self.cst_b], writes=[self.ps_b[pb]])
                S.op("act", lambda en: en.activation(out=nr[:, k, :], in_=self.ps[pb][:, :], func=AF.Sqrt,
                                                     scale=1.0 / D, bias=self.eps_col(RMS_EPS)),
                     reads=[self.ps_b[pb], self.cst_b], writes=[nr_b[k]])
                S.op("dve", lambda en: en.reciprocal(nr[:, k, :], nr[:, k, :]), reads=[nr_b[k]], writes=[nr_b[k]])
                for dc in range(DC):
                    if router is None:
                        S.op("dve", lambda en, dc=dc: en.scalar_tensor_tensor(
                            out=self.nT[:, dc, tt * 512:(tt + 1) * 512], in0=nh[:, k, dc, :], scalar=self.col(l, gname, dc),
                            in1=nr[:, k, :], op0=ALU.mult, op1=ALU.mult),
                            reads=[nh_b[k], nr_b[k], self.cst_b], writes=[self.nT_b[tt]])
                    else:
                        S.op("dve", lambda en, dc=dc: en.scalar_tensor_tensor(
                            out=nh[:, k, dc, :], in0=nh[:, k, dc, :], scalar=self.col(l, gname, dc),
                            in1=nr[:, k, :], op0=ALU.mult, op1=ALU.mult),
                            reads=[nh_b[k], nr_b[k], self.cst_b], writes=[nh_b[k]])
                        S.op("act", lambda en, dc=dc: en.copy(self.nT[:, dc, tt * 512:(tt + 1) * 512], nh[:, k, dc, :]),
                             reads=[nh_b[k]], writes=[self.nT_b[tt]])
                if router is not None:
                    rw32, rw_b, lg32, lg_b = router
                    pr = 2 + tt % 2
                    for sub in range(4):
                        for dc in range(DC):
                            S.mm(self.ps[pr][:, sub * 8:(sub + 1) * 8], nh[:, k, dc, sub * 128:(sub + 1) * 128], rw32[:, dc, :],
                                 dc == 0, dc == DC - 1, reads=[nh_b[k], rw_b], writes=[self.ps_b[pr]])
                    S.op("dve", lambda en: en.tensor_copy(lg32[:, tt * 4:(tt + 1) * 4, :],
                                                          self.ps[pr][:, 0:32].rearrange("p (s e) -> p s e", e=8)),
                         reads=[self.ps_b[pr]], writes=[lg_b])
            S.barrier()

    def eps_col(self, val):
        return self._eps[val][:, 0:1]

    def resid_add(self, dc, tt, pb, stage, stage_b, scale_ap=None):
        S = self.S
        hsl = self.hT[dc, :, tt * 512:(tt + 1) * 512]
        S.dma("sp", stage, hsl, reads=[self.hT_b[dc][tt]], writes=[stage_b])
        if scale_ap is None:
            S.op("dve", lambda en: en.tensor_tensor(out=stage, in0=self.ps[pb][:, :], in1=stage, op=ALU.add),
                 reads=[self.ps_b[pb], stage_b], writes=[stage_b])
        else:
            sc_ap, sc_b, tmp, tmp_b = scale_ap
            S.op("dve", lambda en: en.tensor_tensor(out=tmp, in0=self.ps[pb][:, :], in1=sc_ap, op=ALU.mult),
                 reads=[self.ps_b[pb], sc_b], writes=[tmp_b])
            S.op("dve", lambda en: en.tensor_tensor(out=stage, in0=tmp, in1=stage, op=ALU.add),
                 reads=[tmp_b, stage_b], writes=[stage_b])
        S.dma("sp", hsl, stage, reads=[stage_b], writes=[self.hT_b[dc][tt]])

    def swiglu_up(self, w1, w3, nfc, gT, gT_b, tok0, ntok):
        S = self.S
        ntt = ntok // 512
        sa, sa_b = self.sw_sa
        if True:
            cnt = 0
            for fc in range(nfc):
                v1, b1 = self.wload(self.wsrc(w1, fc * 128, 128), 16, 128)
                v3, b3 = self.wload(self.wsrc(w3, fc * 128, 128), 16, 128)
                for t in range(ntt):
                    tg = (tok0 + t * 512) // 512
                    p1 = (cnt * 2) % 8
                    p3 = (cnt * 2 + 1) % 8
                    k = cnt % 2
                    cnt += 1
                    rhs = lambda kc: self.nT[:, kc, tok0 + t * 512: tok0 + (t + 1) * 512]
                    for kc in range(DC):
                        S.mm(self.ps[p1][:, :], v1[:, kc, :], rhs(kc), kc == 0, kc == DC - 1,
                             reads=[b1, self.nT_b[tg]], writes=[self.ps_b[p1]])
                    for kc in range(DC):
                        S.mm(self.ps[p3][:, :], v3[:, kc, :], rhs(kc), kc == 0, kc == DC - 1,
                             reads=[b3, self.nT_b[tg]], writes=[self.ps_b[p3]])
                    S.op("act", lambda en: en.activation(out=sa[:, k, :], in_=self.ps[p1][:, :], func=AF.Silu),
                         reads=[self.ps_b[p1]], writes=[sa_b[k]])
                    S.op("dve", lambda en: en.tensor_tensor(out=gT[:, fc, t * 512:(t + 1) * 512], in0=self.ps[p3][:, :],
                                                            in1=sa[:, k, :], op=ALU.mult),
                         reads=[self.ps_b[p3], sa_b[k]], writes=[gT_b])

    def swiglu_down(self, w2, nfc, gT, gT_b, tok0, ntok, scale=None):
        S = self.S
        ntt = ntok // 512
        st, st_b = self.sw_st
        tmp, tmp_b = self.sw_tmp
        if True:
            cnt = 0
            for dc in range(DC):
                v2, b2 = self.wload(self.wsrc(w2, dc * 128, 128), nfc, 128)
                for t in range(ntt):
                    tg = (tok0 + t * 512) // 512
                    pb = cnt % 8
                    k = cnt % 3
                    k2 = cnt % 2
                    cnt += 1
                    for fc in range(nfc):
                        S.mm(self.ps[pb][:, :], v2[:, fc, :], gT[:, fc, t * 512:(t + 1) * 512], fc == 0, fc == nfc - 1,
                             reads=[b2, gT_b], writes=[self.ps_b[pb]])
                    sc = None
                    if scale is not None:
                        sc = (scale[0][:, tok0 + t * 512: tok0 + (t + 1) * 512], scale[1], tmp[:, k2, :], tmp_b[k2])
                    self.resid_add(dc, tg, pb, st[:, k, :], st_b[k], sc)

    def phase_ffn_dense(self, j):
        S = self.S
        w1 = self.I["ffn_w1"][j]
        w3 = self.I["ffn_w3"][j]
        w2 = self.I["ffn_w2"][j]
        with ExitStack() as es:
            gT = es.enter_context(self.tmp("gT", [128, FC, 512], BF16))
            sa = es.enter_context(self.tmp("sw_a", [128, 2, 512], BF16))
            st = es.enter_context(self.tmp("sw_st", [128, 3, 512], F32))
            tmp = es.enter_context(self.tmp("sw_tmp", [128, 2, 512], F32))
            gT_b = Buf("gT")
            self.sw_sa = (sa, [Buf(), Buf()])
            self.sw_st = (st, [Buf(), Buf(), Buf()])
            self.sw_tmp = (tmp, [Buf(), Buf()])
            for tg in range(4):
                if "ffn_up" not in self.o.get("skip", ()):
                    self.swiglu_up(w1, w3, FC, gT, gT_b, tg * 512, 512)
                if "ffn_down" not in self.o.get("skip", ()):
                    self.swiglu_down(w2, FC, gT, gT_b, tg * 512, 512)
            S.barrier()

    def phase_moe(self, l, j):
        S = self.S
        I = self.I
        ident32 = self.C("ident", bf=False)
        ones32 = self.C("ones", bf=False)
        with ExitStack() as es:
            rw32 = es.enter_context(self.tmp("rw32", [128, DC, NE], F32))
            lg32 = es.enter_context(self.tmp("lg32", [128, 16, NE], F32))
            rb = es.enter_context(self.tmp("rb", [128, NE], F32))
            comb = es.enter_context(self.tmp("comb", [128, 16, NE], F32))
            t8 = es.enter_context(self.tmp("t8", [128, 3, 16, NE], F32))
            m12 = es.enter_context(self.tmp("m12", [128, 4, 16], F32))
            rw_b, lg_b, comb_b, t8_b, m_b = Buf("rw32"), Buf("lg32"), Buf("comb"), Buf("t8"), Buf("m12")
            S.dma("sp", rw32[:], I["router_w"][j].rearrange("(k p) e -> p k e", p=128), writes=[rw_b])
            S.dma("sp", rb[:], I["router_b"][j:j + 1, :].broadcast_to([128, NE]), writes=[rw_b])
            self.phase_norm(l, "norm_ffn", router=(rw32, rw_b, lg32, lg_b))

            def dve(fn, reads, writes):
                return S.op("dve", fn, reads, writes)

            bc8 = lambda ap: ap.unsqueeze(2).to_broadcast([128, 16, NE])
            dve(lambda en: en.tensor_tensor(out=lg32[:], in0=lg32[:], in1=rb[:, :].unsqueeze(1).to_broadcast([128, 16, NE]),
                                            op=ALU.add), [lg_b, rw_b], [lg_b])
            dve(lambda en: en.tensor_reduce(out=m12[:, 0, :], in_=lg32[:], axis=AX.X, op=ALU.max), [lg_b], [m_b])
            dve(lambda en: en.tensor_tensor(out=t8[:, 0, :, :], in0=lg32[:], in1=bc8(m12[:, 0, :]), op=ALU.is_equal),
                [lg_b, m_b], [t8_b])
            dve(lambda en: en.scalar_tensor_tensor(out=t8[:, 0, :, :], in0=t8[:, 0, :, :], scalar=-1e30, in1=lg32[:],
                                                   op0=ALU.mult, op1=ALU.add), [t8_b, lg_b], [t8_b])
            dve(lambda en: en.tensor_reduce(out=m12[:, 1, :], in_=t8[:, 0, :, :], axis=AX.X, op=ALU.max), [t8_b, m_b], [m_b])
            dve(lambda en: en.tensor_tensor(out=t8[:, 1, :, :], in0=lg32[:], in1=bc8(m12[:, 1, :]), op=ALU.is_ge),
                [lg_b, m_b, t8_b], [t8_b])
            dve(lambda en: en.tensor_tensor(out=t8[:, 2, :, :], in0=lg32[:], in1=bc8(m12[:, 0, :]), op=ALU.subtract),
                [lg_b, m_b, t8_b], [t8_b])
            S.op("act", lambda en: en.activation(out=t8[:, 2, :, :], in_=t8[:, 2, :, :], func=AF.Exp), [t8_b], [t8_b])
            dve(lambda en: en.tensor_tensor(out=m12[:, 2, :], in0=m12[:, 1, :], in1=m12[:, 0, :], op=ALU.subtract), [m_b], [m_b])
            S.op("act", lambda en: en.activation(out=m12[:, 2, :], in_=m12[:, 2, :], func=AF.Exp), [m_b], [m_b])
            dve(lambda en: en.tensor_scalar(m12[:, 2, :], m12[:, 2, :], 1.0, None, ALU.add), [m_b], [m_b])
            dve(lambda en: en.reciprocal(m12[:, 3, :], m12[:, 2, :]), [m_b], [m_b])
            dve(lambda en: en.tensor_tensor(out=t8[:, 2, :, :], in0=t8[:, 2, :, :], in1=t8[:, 1, :, :], op=ALU.mult), [t8_b], [t8_b])
            dve(lambda en: en.tensor_tensor(out=comb[:], in0=t8[:, 2, :, :], in1=bc8(m12[:, 3, :]), op=ALU.mult),
                [t8_b, m_b], [comb_b])
            if "comb" in self.dbg_out:
                S.dma("sp", self.dbg_out["comb"], comb[:], reads=[comb_b])

            gT = es.enter_context(self.tmp("gTe", [128, FCE, 1024], BF16))
            sa = es.enter_context(self.tmp("sw_a", [128, 2, 512], BF16))
            st = es.enter_context(self.tmp("sw_st", [128, 3, 512], F32))
            tmp = es.enter_context(self.tmp("sw_tmp", [128, 2, 512], F32))
            combB = es.enter_context(self.tmp("combB", [128, T], F32))
            dg = es.enter_context(self.tmp("dg", [128, 2, 128], F32))
            gT_b = Buf("gTe")
            cB_b = Buf("combB")
            dg_b = [Buf(), Buf()]
            self.sw_sa = (sa, [Buf(), Buf()])
            self.sw_st = (st, [Buf(), Buf(), Buf()])
            self.sw_tmp = (tmp, [Buf(), Buf()])
            for e in range(NE):
                for tt in range(NTT):
                    pb = self.bank()
                    for sub in range(4):
                        ti = tt * 4 + sub
                        k = ti % 2
                        dve(lambda en: en.tensor_scalar(dg[:, k, :], ident32, comb[:, ti, e:e + 1], None, ALU.mult),
                            [comb_b, self.cst_b, dg_b[k]], [dg_b[k]])
                        S.mm(self.ps[pb][:, sub * 128:(sub + 1) * 128], ones32, dg[:, k, :], True, True,
                             reads=[dg_b[k], self.cst_b], writes=[self.ps_b[pb]])
                    S.op("act", lambda en: en.copy(combB[:, tt * 512:(tt + 1) * 512], self.ps[pb][:, :]), [self.ps_b[pb]], [cB_b])
                for th in range(2):
                    self.swiglu_up(I["moe_w1"][j, e], I["moe_w3"][j, e], FCE, gT, gT_b, th * 1024, 1024)
                    self.swiglu_down(I["moe_w2"][j, e], FCE, gT, gT_b, th * 1024, 1024, scale=(combB, cB_b))
            S.barrier()

    def evac(self, eng, dst, src, reads, writes, scale=None):
        S = self.S
        if eng == "act":
            if scale is None:
                S.op("act", lambda en: en.copy(dst, src), reads=reads, writes=writes)
            else:
                S.op("act", lambda en: en.mul(dst, src, scale), reads=reads, writes=writes)
        else:
            if scale is None:
                S.op("dve", lambda en: en.tensor_copy(dst, src), reads=reads, writes=writes)
            else:
                S.op("dve", lambda en: en.tensor_scalar(dst, src, scale, None, ALU.mult), reads=reads, writes=writes)

    def phase_mixer(self, l):
        S = self.S
        o = self.o
        with self.tmp("yrwT", [128, 4, T], BF16) as yrwT:
            yrw_b = Buf("yrw")
            if o["rw"]:
                self.phase_rw(l, yrwT, yrw_b)
            else:
                S.op("pool", lambda en: en.memset(yrwT[:], 0.0), writes=[yrw_b])
            S.barrier()
            with self.tmp("ygmT", [128, 4, T], BF16) as ygmT:
                ygm_b = Buf("ygm")
                if o["gm"]:
                    self.phase_gm(l, ygmT, ygm_b)
                else:
                    S.op("pool", lambda en: en.memset(ygmT[:], 0.0), writes=[ygm_b])
                S.barrier()
                with self.tmp("ysbT", [128, 8, T], BF16) as ysbT:
                    ysb_b = Buf("ysb")
                    if o["sb"]:
                        self.phase_sb(l, ysbT, ysb_b)
                    else:
                        S.op("pool", lambda en: en.memset(ysbT[:], 0.0), writes=[ysb_b])
                    S.barrier()
                    for nm, tns in (("ygm", ygmT), ("yrw", yrwT), ("ysb", ysbT)):
                        if nm in self.dbg_out:
                            S.dma("sp", self.dbg_out[nm].rearrange("c p t -> p c t"), tns[:], reads=[ygm_b, yrw_b, ysb_b])
                    if "merge" not in o.get("skip", ()):
                        self.phase_merge(l, ygmT, ygm_b, yrwT, yrw_b, ysbT, ysb_b)
                    S.barrier()
        if "wo" not in o.get("skip", ()):
            self.phase_wo(l)

    def phase_sb(self, l, ysbT, ysb_b):
        S = self.S
        w = self.I["w_in"][l]
        m_lt = self.C("m_lt")
        neg_ge = self.C("neg_ge")
        neg_ones = self.C("neg_ones")
        zeros64 = self.C("zeros", n=64)
        with self.tmp("v_all", [128, 16, 256], BF16) as v_all, self.tmp("qT", [128, T], BF16) as qT, \
                self.tmp("kT", [128, T], BF16) as kT, \
                self.tmp("sp", [128, 3, 512], BF16) as sp, self.tmp("att", [128, 3, 512], BF16) as att, \
                self.tmp("ssum", [128, 512], BF16) as ssum:
            v_b = Buf("v_all")
            q_b = Buf("qT")
            k_b = Buf("kT")
            ez_b = [Buf(), Buf()]
            sp_b = [Buf(), Buf(), Buf()]
            att_b = [Buf(), Buf(), Buf()]
            ss_b = Buf("ssum")
            cnt = 0
            pi = 0
            for hp in range(8):
                if hp % 2 == 0:
                    wv, wb = self.wload(self.wsrc(w, SB_OFF + 2048 + (hp // 2) * 256, 256), 16, 256)
                    for ti in range(16):
                        pb = 6 + cnt % 2
                        cnt += 1
                        for kc in range(DC):
                            S.mm(self.ps[pb][:, 0:256], self.nT[:, kc, ti * 128:(ti + 1) * 128], wv[:, kc, :], kc == 0,
                                 kc == DC - 1, reads=[wb, self.nT_b[ti // 4]], writes=[self.ps_b[pb]])
                        self.evac("act" if cnt % 2 else "dve", v_all[:, ti, :],
                                  self.ps[pb][:, 0:256], [self.ps_b[pb]], [v_b])
                wq, wqb = self.wload(self.wsrc(w, SB_OFF + hp * 128, 128), 16, 128)
                wk, wkb = self.wload(self.wsrc(w, SB_OFF + 1024 + hp * 128, 128), 16, 128)
                for tt in range(NTT):
                    pb = 6 + (tt % 2)
                    for kc in range(DC):
                        S.mm(self.ps[pb][:, :], wq[:, kc, :], self.nT[:, kc, tt * 512:(tt + 1) * 512], kc == 0, kc == DC - 1,
                             reads=[wqb, self.nT_b[tt]], writes=[self.ps_b[pb]])
                    self.evac("dve", qT[:, tt * 512:(tt + 1) * 512], self.ps[pb][:, :], [self.ps_b[pb]], [q_b], scale=0.125)
                for tt in range(NTT):
                    pb = 6 + (tt % 2)
                    for kc in range(DC):
                        S.mm(self.ps[pb][:, :], wk[:, kc, :], self.nT[:, kc, tt * 512:(tt + 1) * 512], kc == 0, kc == DC - 1,
                             reads=[wkb, self.nT_b[tt]], writes=[self.ps_b[pb]])
                    self.evac("dve", kT[:, tt * 512:(tt + 1) * 512], self.ps[pb][:, :], [self.ps_b[pb]], [k_b])
                for h2 in range(2):
                    hb = h2 * 64
                    h = hp * 2 + h2
                    for qt in range(NTT):
                        ui = getattr(self, "_sb_ui", 0)
                        self._sb_ui = ui + 1
                        po = 4 + (ui % 2)
                        S.mm(self.ps[po][hb:hb + 64, :], zeros64, self.nT[:, 0, 0:512], True, False,
                             reads=[self.cst_b, self.nT_b[0]], writes=[self.ps_b[po]])
                        S.op("pool", lambda en: en.memset(ssum[:], 0.0), writes=[ss_b])
                        def front(kb, pidx):
                            kl = kb - 4 * qt
                            tq0 = max(kl, 0) * 128
                            i = pidx % 3
                            za = (0, 1, 6)[pidx % 3]
                            q_ap = qT[hb:hb + 64, qt * 512 + tq0:(qt + 1) * 512]
                            k_ap = kT[hb:hb + 64, kb * 128:(kb + 1) * 128]
                            S.mm(self.ps[za][:, tq0:512], k_ap, q_ap, True, True, reads=[q_b, k_b], writes=[self.ps_b[za]])
                            S.op("act", lambda en: en.activation(out=self.ps[za][:, tq0:512], in_=self.ps[za][:, tq0:512],
                                                                 func=AF.Exp), reads=[self.ps_b[za]], writes=[self.ps_b[za]])
                            S.op("act", lambda en: en.activation(out=sp[:, i, tq0:512], in_=self.ps[za][:, tq0:512], func=AF.Ln,
                                                                 bias=self.eps_col(1.0)),
                                 reads=[self.ps_b[za], self.cst_b], writes=[sp_b[i]])
                            if kl >= 0:
                                S.op("pool", lambda en: en.tensor_tensor(out=sp[:, i, tq0:tq0 + 128], in0=sp[:, i, tq0:tq0 + 128],
                                                                         in1=m_lt, op=ALU.mult),
                                     reads=[sp_b[i], self.cst_b], writes=[sp_b[i]])

                        def back(kb, pidx, first):
                            kl = kb - 4 * qt
                            tq0 = max(kl, 0) * 128
                            i = pidx % 3
                            zb = (2, 3, 7)[pidx % 3]
                            q_ap = qT[hb:hb + 64, qt * 512 + tq0:(qt + 1) * 512]
                            k_ap = kT[hb:hb + 64, kb * 128:(kb + 1) * 128]
                            S.mm(self.ps[zb][:, tq0:512], k_ap, q_ap, True, False, reads=[q_b, k_b], writes=[self.ps_b[zb]])
                            S.mm(self.ps[zb][:, tq0:512], neg_ge, sp[:, i, tq0:512], False, first,
                                 reads=[sp_b[i], self.cst_b], writes=[self.ps_b[zb]])
                            if not first:
                                S.mm(self.ps[zb][:, tq0:512], neg_ones, ssum[:, tq0:512], False, True,
                                     reads=[ss_b, self.cst_b], writes=[self.ps_b[zb]])
                            S.op("act", lambda en: en.activation(out=att[:, i, tq0:512], in_=self.ps[zb][:, tq0:512],
                                                                 func=AF.Exp), reads=[self.ps_b[zb]], writes=[att_b[i]])
                            if kl >= 0:
                                S.op("pool", lambda en: en.tensor_tensor(out=att[:, i, tq0:tq0 + 128], in0=att[:, i, tq0:tq0 + 128],
                                                                         in1=m_lt, op=ALU.mult),
                                     reads=[att_b[i], self.cst_b], writes=[att_b[i]])
                            S.mm(self.ps[po][hb:hb + 64, tq0:512], v_all[:, kb, (h % 4) * 64:(h % 4 + 1) * 64], att[:, i, tq0:512],
                                 False, kb == 0, reads=[v_b, att_b[i]], writes=[self.ps_b[po]])
                            if kb > 0:
                                S.op("pool", lambda en: en.tensor_tensor(out=ssum[:, tq0:512], in0=ssum[:, tq0:512],
                                                                         in1=sp[:, i, tq0:512], op=ALU.add),
                                     reads=[sp_b[i], ss_b], writes=[ss_b])

                        kbs = list(range(4 * qt + 3, -1, -1))
                        pidx0 = pi
                        front(kbs[0], pidx0)
                        for n_, kb in enumerate(kbs):
                            if n_ + 1 < len(kbs):
                                front(kbs[n_ + 1], pidx0 + n_ + 1)
                            back(kb, pidx0 + n_, n_ == 0)
                        pi = pidx0 + len(kbs)
                        self.evac("dve", ysbT[hb:hb + 64, hp, qt * 512:(qt + 1) * 512], self.ps[po][hb:hb + 64, :],
                                  [self.ps_b[po]], [ysb_b])
            S.barrier()

    def gelu(self, dst, pb, ncol, tmp, tmp_b, writes):
        S = self.S
        src = self.ps[pb][:, 0:ncol]
        S.op("act", lambda en: en.activation(out=tmp, in_=src, func=AF.Square), reads=[self.ps_b[pb]], writes=[tmp_b])
        S.op("dve", lambda en: en.tensor_scalar(tmp, tmp, 0.0713548163, 1.5957691216, ALU.mult, ALU.add),
             reads=[tmp_b], writes=[tmp_b])
        S.op("dve", lambda en: en.tensor_tensor(out=tmp, in0=src, in1=tmp, op=ALU.mult), reads=[tmp_b, self.ps_b[pb]],
             writes=[tmp_b])
        S.op("act", lambda en: en.activation(out=tmp, in_=tmp, func=AF.Sigmoid), reads=[tmp_b], writes=[tmp_b])
        S.op("dve", lambda en: en.tensor_tensor(out=dst, in0=src, in1=tmp, op=ALU.mult), reads=[tmp_b, self.ps_b[pb]],
             writes=writes)

    def phase_gm(self, l, ygmT, ygm_b):
        S = self.S
        w = self.I["w_in"][l]
        I = self.I
        with self.tmp("ug", [128, 4, T], BF16) as ug, self.tmp("vln", [128, 16, 512], BF16) as vln, \
                self.tmp("wsT", [128, 8, 128], BF16) as wsT, self.tmp("lng", [128, 512], F32) as lng, \
                self.tmp("lnb", [128, 512], F32) as lnb, self.tmp("bs32", [1, 1024], F32) as bs32, \
                self.tmp("bsr", [1, 1024], BF16) as bsr, self.tmp("gt", [128, 2, 512], F32) as gt, \
                self.tmp("vg", [128, 2, 512], F32) as vg, self.tmp("ws32", [128, 2, 128], F32) as ws32, \
                self.tmp("st6", [128, 2, 8], F32) as st6:
            ug_b = Buf("ug")
            vln_b = Buf("vln")
            wsT_b = Buf("wsT")
            par_b = Buf("gmpar")
            gt_b = [Buf(), Buf()]
            vg_b = [Buf(), Buf()]
            ws32_b = [Buf(), Buf()]
            st_b = [Buf(), Buf()]
            S.dma("sp", lng[:], I["gm_ln_g"][l:l + 1, :].broadcast_to([128, 512]), writes=[par_b])
            S.dma("sp", lnb[:], I["gm_ln_b"][l:l + 1, :].broadcast_to([128, 512]), writes=[par_b])
            S.dma("sp", bs32[:], I["gm_bs"][l:l + 1, :], writes=[par_b])
            S.op("dve", lambda en: en.tensor_copy(bsr[:], bs32[:]), reads=[par_b], writes=[par_b])
            ident = self.C("ident", bf=False)
            m_le = self.C("m_le", bf=False)
            for h in range(8):
                k = h % 2
                pb = h % 2
                S.dma("sp", ws32[:, k, :], I["gm_ws"][l, h], writes=[ws32_b[k]])
                S.op("pe", lambda en: en.transpose(self.ps[pb][:, 0:128], ws32[:, k, :], ident),
                     reads=[ws32_b[k], self.cst_b], writes=[self.ps_b[pb]])
                S.op("dve", lambda en: en.tensor_tensor(out=wsT[:, h, :], in0=self.ps[pb][:, 0:128], in1=m_le, op=ALU.mult),
                     reads=[self.ps_b[pb], self.cst_b], writes=[wsT_b])
            cnt = 0
            for c in range(4):
                wv, wb = self.wload(self.wsrc(w, GM_OFF + c * 128, 128), 16, 128)
                for tt in range(NTT):
                    pb = 2 + cnt % 6
                    k = cnt % 2
                    cnt += 1
                    for kc in range(DC):
                        S.mm(self.ps[pb][:, :], wv[:, kc, :], self.nT[:, kc, tt * 512:(tt + 1) * 512], kc == 0, kc == DC - 1,
                             reads=[wb, self.nT_b[tt]], writes=[self.ps_b[pb]])
                    self.gelu(ug[:, c, tt * 512:(tt + 1) * 512], pb, 512, gt[:, k, :], gt_b[k], [ug_b])
            wv0, wb0 = self.wload(self.wsrc(w, GM_OFF + 512, 256), 16, 256)
            wv1, wb1 = self.wload(self.wsrc(w, GM_OFF + 768, 256), 16, 256)
            for ti in range(16):
                pb = 2 + cnt % 6
                k = cnt % 2
                cnt += 1
                for cb, (wv, wb) in enumerate(((wv0, wb0), (wv1, wb1))):
                    for kc in range(DC):
                        S.mm(self.ps[pb][:, cb * 256:(cb + 1) * 256], self.nT[:, kc, ti * 128:(ti + 1) * 128], wv[:, kc, :],
                             kc == 0, kc == DC - 1, reads=[wb, self.nT_b[ti // 4]], writes=[self.ps_b[pb]])
                self.gelu(vg[:, k, :], pb, 512, gt[:, k, :], gt_b[k], [vg_b[k]])
                S.op("dve", lambda en: en.bn_stats(st6[:, k, 0:6], vg[:, k, :]), reads=[vg_b[k]], writes=[st_b[k]])
                S.op("dve", lambda en: en.bn_aggr(st6[:, k, 6:8], st6[:, k, 0:6]), reads=[st_b[k]], writes=[st_b[k]])
                S.op("act", lambda en: en.activation(out=st6[:, k, 7:8], in_=st6[:, k, 7:8], func=AF.Sqrt,
                                                     bias=self.eps_col(LN_EPS)), reads=[st_b[k], self.cst_b], writes=[st_b[k]])
                S.op("dve", lambda en: en.reciprocal(st6[:, k, 7:8], st6[:, k, 7:8]), reads=[st_b[k]], writes=[st_b[k]])
                S.op("dve", lambda en: en.tensor_scalar(vg[:, k, :], vg[:, k, :], st6[:, k, 6:7], st6[:, k, 7:8],
                                                        ALU.subtract, ALU.mult), reads=[vg_b[k], st_b[k]], writes=[vg_b[k]])
                S.op("dve", lambda en: en.tensor_tensor(out=vg[:, k, :], in0=vg[:, k, :], in1=lng[:], op=ALU.mult),
                     reads=[vg_b[k], par_b], writes=[vg_b[k]])
                S.op("dve", lambda en: en.tensor_tensor(out=vln[:, ti, :], in0=vg[:, k, :], in1=lnb[:], op=ALU.add),
                     reads=[vg_b[k], par_b], writes=[vln_b])
            ones_row = self.cstb[0:1, CS["ones"]:CS["ones"] + 64]
            for cg in range(4):
                for hp in range(4):
                    pb = 2 + cnt % 6
                    cnt += 1
                    for cl in range(4):
                        c = cg * 4 + cl
                        for h2 in range(2):
                            h = hp * 2 + h2
                            outp = self.ps[pb][h2 * 64:(h2 + 1) * 64, cl * 128:(cl + 1) * 128]
                            S.mm(outp, vln[:, c, h * 64:(h + 1) * 64], wsT[:, h, :], True, False,
                                 reads=[vln_b, wsT_b], writes=[self.ps_b[pb]])
                            S.mm(outp, ones_row, bsr[0:1, h * 128:(h + 1) * 128], False, True,
                                 reads=[par_b, self.cst_b], writes=[self.ps_b[pb]])
                    S.op("dve", lambda en: en.tensor_tensor(out=ygmT[:, hp, cg * 512:(cg + 1) * 512], in0=self.ps[pb][:, :],
                                                            in1=ug[:, hp, cg * 512:(cg + 1) * 512], op=ALU.mult),
                         reads=[self.ps_b[pb], ug_b], writes=[ygm_b])
            S.barrier()

    def phase_merge(self, l, ygmT, ygm_b, yrwT, yrw_b, ysbT, ysb_b):
        S = self.S
        I = self.I
        w = I["w_in"][l]
        with self.tmp("gate", [128, 2, 3, 512], BF16) as gate, self.tmp("mst", [128, 2, T], BF16) as mst, \
                self.tmp("mt", [128, 2, 2, 512], F32) as mt:
            gate_b = [[Buf() for _ in range(3)] for _ in range(2)]
            mst_b = [Buf(), Buf()]
            mt_b = [[Buf(), Buf()], [Buf(), Buf()]]
            cnt = 0
            gi = 0
            for dc in range(DC):
                wg = [self.wload(self.wsrc(w, GATE_OFF + b * 2048 + dc * 128, 128), 16, 128) for b in range(3)]
                wp = [self.wload(self.wsrc(I["p_gm"][l], dc * 128, 128), 4, 128),
                      self.wload(self.wsrc(I["p_rw"][l], dc * 128, 128), 4, 128),
                      self.wload(self.wsrc(I["p_sb"][l], dc * 128, 128), 8, 128)]
                ys = [(ygmT, ygm_b, 4), (yrwT, yrw_b, 4), (ysbT, ysb_b, 8)]
                km = dc % 2
                for tt in range(NTT):
                    kg = gi % 2
                    gi += 1
                    for b in range(3):
                        pb = cnt % 8
                        cnt += 1
                        for kc in range(DC):
                            S.mm(self.ps[pb][:, :], wg[b][0][:, kc, :], self.nT[:, kc, tt * 512:(tt + 1) * 512], kc == 0,
                                 kc == DC - 1, reads=[wg[b][1], self.nT_b[tt]], writes=[self.ps_b[pb]])
                        S.op("act", lambda en: en.activation(out=gate[:, kg, b, :], in_=self.ps[pb][:, :], func=AF.Sigmoid,
                                                             bias=self.col(l, "gate_b", b * 16 + dc)),
                             reads=[self.ps_b[pb], self.cst_b], writes=[gate_b[kg][b]])
                    pbs = []
                    for b in range(3):
                        pb = cnt % 8
                        cnt += 1
                        pbs.append(pb)
                        yt, yb, nk = ys[b]
                        for kc in range(nk):
                            S.mm(self.ps[pb][:, :], wp[b][0][:, kc, :], yt[:, kc, tt * 512:(tt + 1) * 512], kc == 0, kc == nk - 1,
                                 reads=[wp[b][1], yb], writes=[self.ps_b[pb]])
                    t0 = mt[:, kg, 0, :]
                    t1 = mt[:, kg, 1, :]
                    b0, b1 = mt_b[kg]
                    S.op("dve", lambda en: en.tensor_tensor(out=t0, in0=self.ps[pbs[0]][:, :], in1=gate[:, kg, 0, :], op=ALU.mult),
                         reads=[self.ps_b[pbs[0]], gate_b[kg][0]], writes=[b0])
                    S.op("dve", lambda en: en.tensor_tensor(out=t1, in0=self.ps[pbs[1]][:, :], in1=gate[:, kg, 1, :], op=ALU.mult),
                         reads=[self.ps_b[pbs[1]], gate_b[kg][1]], writes=[b1])
                    S.op("dve", lambda en: en.tensor_tensor(out=t0, in0=t0, in1=t1, op=ALU.add), reads=[b0, b1], writes=[b0])
                    S.op("dve", lambda en: en.tensor_tensor(out=t1, in0=self.ps[pbs[2]][:, :], in1=gate[:, kg, 2, :], op=ALU.mult),
                         reads=[self.ps_b[pbs[2]], gate_b[kg][2]], writes=[b1])
                    S.op("dve", lambda en: en.tensor_tensor(out=mst[:, km, tt * 512:(tt + 1) * 512], in0=t0, in1=t1, op=ALU.add),
                         reads=[b0, b1], writes=[mst_b[km]])
                S.dma("sp", self.mT[dc], mst[:, km, :], reads=[mst_b[km]], writes=[self.mT_b[dc]])
            S.barrier()

    def phase_wo(self, l):
        S = self.S
        wo = self.I["w_o"][l]
        with self.tmp("wo_st", [128, 3, 512], F32) as st:
            st_b = [Buf(), Buf(), Buf()]
            for dc in range(DC):
                S.dma("sp", self.nT[:, dc, :], self.mT[dc], reads=[self.mT_b[dc]], writes=self.nT_b)
            cnt = 0
            for dc in range(DC):
                wv, wb = self.wload(self.wsrc(wo, dc * 128, 128), 16, 128)
                for tt in range(NTT):
                    pb = cnt % 8
                    k = cnt % 3
                    cnt += 1
                    for kc in range(DC):
                        S.mm(self.ps[pb][:, :], wv[:, kc, :], self.nT[:, kc, tt * 512:(tt + 1) * 512], kc == 0, kc == DC - 1,
                             reads=[wb, self.nT_b[tt]], writes=[self.ps_b[pb]])
                    self.resid_add(dc, tt, pb, st[:, k, :], st_b[k])
            S.barrier()

    def bank(self):
        self.pbi = getattr(self, "pbi", 0) + 1
        return self.pbi % 8

    def phase_rw(self, l, yrwT, yrw_b):
        S = self.S
        I = self.I
        w = I["w_in"][l]
        TW = 256
        NTW = T // TW
        sdec = -0.6065306597126334
        ps = self.ps
        psb = self.ps_b
        cstb_ = self.cst_b
        blockones = self.C("blockones")
        identb = self.C("ident")
        ident32 = self.cst[0:64, CS["ident"]:CS["ident"] + 64]
        mask2 = self.cst[0:64, CS["mask2"]:CS["mask2"] + 512]
        mask3 = self.cst[0:64, CS["mask3"]:CS["mask3"] + 512]
        I8 = self.cstb[0:64, CS["I8"]:CS["I8"] + 512]

        def dve(fn, reads, writes):
            return S.op("dve", fn, reads, writes)

        def act(fn, reads, writes):
            return S.op("act", fn, reads, writes)

        with ExitStack() as _es:
            lwa = _es.enter_context(self.tmp("lwa", [128, 512], BF16))
            lg = _es.enter_context(self.tmp("lg", [128, 512], BF16))
            l32 = _es.enter_context(self.tmp("l32", [128, 2, 512], F32))
            rmask = _es.enter_context(self.tmp("rmask", [128, TW], F32))
            prevcol = _es.enter_context(self.tmp("prevcol", [128, 16], F32))
            S32 = _es.enter_context(self.tmp("S32", [128, 4, 64], F32))
            Sbf = _es.enter_context(self.tmp("Sbf", [128, 4, 64], BF16))
            gC = _es.enter_context(self.tmp("gC", [128, 4, 32], F32))
            ar = _es.enter_context(self.tmp("ar", [128, 4, 2, TW], BF16))
            bk = _es.enter_context(self.tmp("bk", [128, 4, 2, TW], BF16))
            vb = _es.enter_context(self.tmp("vb", [128, 4, TW], BF16))
            bh = _es.enter_context(self.tmp("bh", [128, 4, TW], BF16))
            kh = _es.enter_context(self.tmp("kh", [128, 4, TW], BF16))
            gT = _es.enter_context(self.tmp("gT", [128, 4, TW], BF16))
            bon = _es.enter_context(self.tmp("bon", [128, 4, TW], BF16))
            ynT = _es.enter_context(self.tmp("ynT", [128, 4, TW], F32))
            ar_bd = _es.enter_context(self.tmp("ar_bd", [128, 4, 2, 2, TW], BF16))
            bt_bd = _es.enter_context(self.tmp("bt_bd", [128, 4, 2, TW], BF16))
            Sbd = _es.enter_context(self.tmp("Sbd", [128, 4, 2, 64], BF16))
            bd_b = Buf("bd")
            par_b = Buf("rwpar")
            pc_b = Buf("prevcol")
            S_b = Buf("S")
            S_gb = [Buf("S0"), Buf("S1")]
            gC_b = Buf("gC")
            ar_b = Buf("ar")
            bk_b = Buf("bk")
            tok_b = Buf("vbbhkh")
            gT_b = Buf("gT")
            bon_b = Buf("bon")
            ynT_b = Buf("ynT")
            S.dma("sp", l32[0:64, 0, :], I["rw_w2"][l], writes=[par_b])
            S.dma("sp", l32[64:128, 0, :], I["rw_a2"][l], writes=[par_b])
            S.dma("sp", l32[:, 1, :], I["rw_g2"][l], writes=[par_b])
            dve(lambda en: en.tensor_copy(lwa[:], l32[:, 0, :]), [par_b], [par_b])
            dve(lambda en: en.tensor_copy(lg[:], l32[:, 1, :]), [par_b], [par_b])
            S.op("pool", lambda en: en.memset(rmask[:], 1.0), writes=[par_b])
            S.op("pool", lambda en: en.memset(rmask[:, :].rearrange("p (c t) -> p c t", t=64)[:, :, 0:1], 0.0), writes=[par_b])
            S.op("pool", lambda en: en.memset(S32[:], 0.0), writes=[S_b, S_gb[0], S_gb[1]])
            S.op("pool", lambda en: en.memset(Sbf[:], 0.0), writes=[S_b, S_gb[0], S_gb[1]])
            S.op("pool", lambda en: en.memset(Sbd[:], 0.0), writes=[S_b, S_gb[0], S_gb[1]])
            S.op("pool", lambda en: en.memset(ar_bd[:], 0.0), writes=[bd_b])
            S.op("pool", lambda en: en.memset(bt_bd[:], 0.0), writes=[bd_b])
            S.op("pool", lambda en: en.memset(prevcol[:], 0.0), writes=[pc_b])

            for ti in range(NTW):
                t0 = ti * TW
                tg = t0 // 512
                with ExitStack() as _es:
                    p32 = _es.enter_context(self.tmp("p32", [128, 2, TW + 1], F32))
                    dd = _es.enter_context(self.tmp("dd", [128, 2, TW], F32))
                    twl = _es.enter_context(self.tmp("twl", [128, TW], BF16))
                    sgl = _es.enter_context(self.tmp("sgl", [128, TW], BF16))
                    r32 = _es.enter_context(self.tmp("r32", [128, TW], F32))
                    k32 = _es.enter_context(self.tmp("k32", [128, TW], F32))
                    v32 = _es.enter_context(self.tmp("v32", [128, TW], F32))
                    sg = _es.enter_context(self.tmp("sg", [128, TW], F32))
                    a32 = _es.enter_context(self.tmp("a32", [128, TW], F32))
                    kk = _es.enter_context(self.tmp("kk", [128, TW], F32))
                    kp = _es.enter_context(self.tmp("kp", [128, TW], F32))
                    bt = _es.enter_context(self.tmp("bt", [128, TW], F32))
                    Lc = _es.enter_context(self.tmp("Lc", [128, TW], F32))
                    E1 = _es.enter_context(self.tmp("E1", [128, TW], F32))
                    E2 = _es.enter_context(self.tmp("E2", [128, TW], F32))
                    x16 = _es.enter_context(self.tmp("x16", [128, TW], BF16))
                    p32_b = [Buf(), Buf()]
                    dd_b = [Buf(), Buf()]
                    lo_b = Buf("lora_in")
                    r_b, k_b, v_b, sg_b, a_b, kk_b, kp_b, bt_b, Lc_b, E1_b, E2_b, x16_b = [Buf() for _ in range(12)]
                    self._shi = 0

                    def shifted(j, dst, dst_b, func=None, rows=None):
                        wv, wb = self.wload(self.wsrc(w, RW_OFF + j * 128, 128), 16, 128)
                        pb = self.bank()
                        q = self._shi % 2
                        self._shi += 1
                        for kc in range(DC):
                            S.mm(ps[pb][:, 0:TW], wv[:, kc, :], self.nT[:, kc, t0:t0 + TW], kc == 0, kc == DC - 1,
                                 reads=[wb, self.nT_b[tg]], writes=[psb[pb]])
                        act(lambda en: en.copy(p32[:, q, 1:TW + 1], ps[pb][:, 0:TW]), [psb[pb]], [p32_b[q]])
                        act(lambda en: en.copy(p32[:, q, 0:1], prevcol[:, j:j + 1]), [pc_b], [p32_b[q]])
                        act(lambda en: en.copy(prevcol[:, j:j + 1], p32[:, q, TW:TW + 1]), [p32_b[q]], [pc_b])
                        dve(lambda en: en.tensor_tensor(out=dd[:, q, :], in0=p32[:, q, 0:TW], in1=p32[:, q, 1:TW + 1],
                                                        op=ALU.subtract), [p32_b[q]], [dd_b[q]])
                        if func is None:
                            dve(lambda en: en.scalar_tensor_tensor(out=dst, in0=dd[:, q, :], scalar=self.col(l, "rw_mu", j),
                                                                   in1=p32[:, q, 1:TW + 1], op0=ALU.mult, op1=ALU.add),
                                [dd_b[q], p32_b[q], cstb_], [dst_b])
                        else:
                            dve(lambda en: en.scalar_tensor_tensor(out=dd[:, q, :], in0=dd[:, q, :], scalar=self.col(l, "rw_mu", j),
                                                                   in1=p32[:, q, 1:TW + 1], op0=ALU.mult, op1=ALU.add),
                                [dd_b[q], p32_b[q], cstb_], [dd_b[q]])
                            for (r0, r1, f) in func:
                                if f is None:
                                    act(lambda en: en.copy(dst[r0:r1, :], dd[r0:r1, q, :]), [dd_b[q]], [dst_b])
                                else:
                                    act(lambda en: en.activation(out=dst[r0:r1, :], in_=dd[r0:r1, q, :], func=f),
                                        [dd_b[q]], [dst_b])

                    shifted(12, twl, lo_b, func=[(0, 64, AF.Tanh), (64, 128, None)])
                    shifted(13, sgl, lo_b, func=[(0, 128, AF.Sigmoid)])
                    for fc in range(4):
                        shifted(fc, r32[:], r_b)
                        shifted(4 + fc, k32[:], k_b)
                        shifted(8 + fc, v32[:], v_b)
                        cs128 = slice(fc * 128, (fc + 1) * 128)
                        pb = self.bank()
                        S.mm(ps[pb][:, 0:TW], lwa[0:64, cs128], twl[0:64, :], True, True, reads=[par_b, lo_b], writes=[psb[pb]])
                        act(lambda en: en.activation(out=sg[:], in_=ps[pb][:, 0:TW], func=AF.Sigmoid,
                                                     bias=self.col(l, "rw_w0", fc)), [psb[pb], cstb_], [sg_b])
                        pb = self.bank()
                        S.mm(ps[pb][:, 0:TW], lwa[64:128, cs128], twl[64:128, :], True, True, reads=[par_b, lo_b], writes=[psb[pb]])
                        act(lambda en: en.activation(out=a32[:], in_=ps[pb][:, 0:TW], func=AF.Sigmoid,
                                                     bias=self.col(l, "rw_a0", fc)), [psb[pb], cstb_], [a_b])
                        pb = self.bank()
                        S.mm(ps[pb][:, 0:TW], lg[:, cs128], sgl[:, :], True, True, reads=[par_b, lo_b], writes=[psb[pb]])
                        act(lambda en: en.copy(gT[:, fc, :], ps[pb][:, 0:TW]), [psb[pb]], [gT_b])
                        dve(lambda en: en.tensor_scalar(kk[:], k32[:], self.col(l, "rw_kk", fc), None, ALU.mult),
                            [k_b, cstb_], [kk_b])
                        act(lambda en: en.activation(out=x16[:], in_=kk[:], func=AF.Square), [kk_b], [x16_b])
                        pb = self.bank()
                        S.mm(ps[pb][:, 0:TW], blockones, x16[:], True, True, reads=[x16_b, cstb_], writes=[psb[pb]])
                        act(lambda en: en.activation(out=E1[:], in_=ps[pb][:, 0:TW], func=AF.Sqrt), [psb[pb]], [E1_b])
                        dve(lambda en: en.tensor_scalar(E1[:], E1[:], 1e-12, None, ALU.max), [E1_b], [E1_b])
                        dve(lambda en: en.reciprocal(E1[:], E1[:]), [E1_b], [E1_b])
                        dve(lambda en: en.tensor_tensor(out=kk[:], in0=kk[:], in1=E1[:], op=ALU.mult), [kk_b, E1_b], [kk_b])
                        dve(lambda en: en.tensor_scalar(kp[:], a32[:], -1.0, self.col(l, "rw_ka", fc), ALU.add, ALU.mult),
                            [a_b, cstb_], [kp_b])
                        dve(lambda en: en.scalar_tensor_tensor(out=kp[:], in0=kp[:], scalar=1.0, in1=k32[:], op0=ALU.add,
                                                               op1=ALU.mult), [kp_b, k_b], [kp_b])
                        dve(lambda en: en.tensor_tensor(out=bt[:], in0=kk[:], in1=a32[:], op=ALU.mult), [kk_b, a_b], [bt_b])
                        dve(lambda en: en.scalar_tensor_tensor(out=x16[:], in0=r32[:], scalar=self.col(l, "rw_rk", fc), in1=kp[:],
                                                               op0=ALU.mult, op1=ALU.mult), [r_b, kp_b, cstb_], [x16_b])
                        pb = self.bank()
                        S.mm(ps[pb][:, 0:TW], blockones, x16[:], True, True, reads=[x16_b, cstb_], writes=[psb[pb]])
                        dve(lambda en: en.tensor_tensor(out=bon[:, fc, :], in0=ps[pb][:, 0:TW], in1=v32[:], op=ALU.mult),
                            [psb[pb], v_b], [bon_b])
                        dve(lambda en: en.tensor_tensor_scan(out=Lc[:], data0=rmask[:], data1=sg[:], initial=0.0,
                                                             op0=ALU.mult, op1=ALU.add), [par_b, sg_b], [Lc_b])
                        Lc3 = Lc[:, :].rearrange("p (c t) -> p c t", t=64)
                        act(lambda en: en.activation(out=gC[:, fc, ti * 4:(ti + 1) * 4], in_=Lc3[:, :, 63], func=AF.Exp,
                                                     scale=sdec), [Lc_b], [gC_b])
                        act(lambda en: en.activation(out=E1[:], in_=Lc[:], func=AF.Exp, scale=sdec), [Lc_b], [E1_b])
                        dve(lambda en: en.tensor_tensor(out=ar[:, fc, 1, :], in0=r32[:], in1=E1[:], op=ALU.mult),
                            [r_b, E1_b], [ar_b])
                        dve(lambda en: en.tensor_tensor(out=E2[:], in0=Lc[:], in1=sg[:], op=ALU.subtract), [Lc_b, sg_b], [E2_b])
                        act(lambda en: en.activation(out=E2[:], in_=E2[:], func=AF.Exp, scale=sdec), [E2_b], [E2_b])
                        dve(lambda en: en.scalar_tensor_tensor(out=ar[:, fc, 0, :], in0=kk[:], scalar=-1.0, in1=E2[:],
                                                               op0=ALU.mult, op1=ALU.mult), [kk_b, E2_b], [ar_b])
                        act(lambda en: en.activation(out=E1[:], in_=Lc[:], func=AF.Exp, scale=-sdec), [Lc_b], [E1_b])
                        dve(lambda en: en.tensor_tensor(out=bk[:, fc, 0, :], in0=bt[:], in1=E1[:], op=ALU.mult),
                            [bt_b, E1_b], [bk_b])
                        dve(lambda en: en.tensor_tensor(out=bk[:, fc, 1, :], in0=kp[:], in1=E1[:], op=ALU.mult),
                            [kp_b, E1_b], [bk_b])
                        for a_i in range(2):
                            for h2 in range(2):
                                S.op("pool", lambda en: en.tensor_copy(ar_bd[h2 * 64:(h2 + 1) * 64, fc, h2, a_i, :],
                                                                       ar[h2 * 64:(h2 + 1) * 64, fc, a_i, :]),
                                     [ar_b], [bd_b])
                        for h2 in range(2):
                            S.op("pool", lambda en: en.tensor_copy(bt_bd[h2 * 64:(h2 + 1) * 64, fc, h2, :],
                                                                   bk[h2 * 64:(h2 + 1) * 64, fc, 0, :]), [bk_b], [bd_b])
                        E23 = E2[:, :].rearrange("p (c t) -> p c t", t=64)
                        dve(lambda en: en.tensor_tensor(out=E23, in0=Lc3[:, :, 63:64].to_broadcast([128, TW // 64, 64]), in1=Lc3,
                                                        op=ALU.subtract), [Lc_b], [E2_b])
                        act(lambda en: en.activation(out=E2[:], in_=E2[:], func=AF.Exp, scale=sdec), [E2_b], [E2_b])
                        dve(lambda en: en.tensor_tensor(out=bh[:, fc, :], in0=bt[:], in1=E2[:], op=ALU.mult),
                            [bt_b, E2_b], [tok_b])
                        dve(lambda en: en.tensor_tensor(out=kh[:, fc, :], in0=kp[:], in1=E2[:], op=ALU.mult),
                            [kp_b, E2_b], [tok_b])
                        act(lambda en: en.copy(vb[:, fc, :], v32[:]), [v_b], [tok_b])
                    S.barrier()
                ynT_g = [Buf(), Buf()]
                with ExitStack() as _es:
                    tokm = _es.enter_context(self.tmp("tokm", [64, 4, 512], BF16))
                    A1 = _es.enter_context(self.tmp("A1", [64, 8, 2, 64], BF16))
                    A2 = _es.enter_context(self.tmp("A2", [64, 8, 2, 64], BF16))
                    Nj = _es.enter_context(self.tmp("Nj", [64, 2, 8, 64], BF16))
                    Mj = _es.enter_context(self.tmp("Mj", [64, 2, 8, 64], BF16))
                    Pj = _es.enter_context(self.tmp("Pj", [64, 2, 8, 64], BF16))
                    AhT = _es.enter_context(self.tmp("AhT", [128, 4, 64], BF16))
                    W2 = _es.enter_context(self.tmp("W2", [64, 8, 64], BF16))
                    Uh = _es.enter_context(self.tmp("Uh", [64, 8, 64], F32))
                    Ub = _es.enter_context(self.tmp("Ub", [64, 8, 64], BF16))
                    Y32 = _es.enter_context(self.tmp("Y32", [64, 8, 64], F32))
                    Ysq = _es.enter_context(self.tmp("Ysq", [64, 8, 64], F32))
                    st8 = _es.enter_context(self.tmp("st8", [64, 4, 8], F32))
                    tokm_b, A1_b, A2_b, AhT_b, W2_b, Uh_b, Ub_b, Y_b, Ysq_b, st8_b = [Buf() for _ in range(10)]
                    Nj_b = [Buf(), Buf()]
                    Mj_b = [Buf(), Buf()]
                    Pj_b = [Buf(), Buf()]
                    for ci in range(TW // 64):
                        cs = ci * 64
                        cg = ti * 4 + ci
                        srcs = [(vb, tok_b, None), (bh, tok_b, None), (kh, tok_b, None), (ar, ar_b, 0)]
                        for half in range(2):
                            pb = self.bank()
                            pv = ps[pb][:, :].bitcast(BF16)
                            for ai in range(2):
                                a_t, a_b_, sub = srcs[half * 2 + ai]
                                for fc in range(4):
                                    src = a_t[:, fc, cs:cs + 64] if sub is None else a_t[:, fc, sub, cs:cs + 64]
                                    S.op("pe", lambda en: en.transpose(pv[0:64, ai * 512 + fc * 128: ai * 512 + (fc + 1) * 128],
                                                                       src, identb), reads=[a_b_, cstb_], writes=[psb[pb]])
                            self.evac("act" if half == 0 else "dve", tokm[:, half * 2:half * 2 + 2, :],
                                      pv[0:64, :].rearrange("p (a f) -> p a f", f=512), [psb[pb]], [tokm_b])
                        Vt, Bt, Kt, At = tokm[:, 0, :], tokm[:, 1, :], tokm[:, 2, :], tokm[:, 3, :]
                        p1 = [self.bank(), self.bank()]
                        p2 = [self.bank(), self.bank()]
                        p3 = self.bank()
                        for fc in range(4):
                            rhs_bd = ar_bd[:, fc, :, :, cs:cs + 64]
                            co = (fc % 2) * 256
                            S.mm(ps[p1[fc // 2]][0:64, co:co + 256], bk[:, fc, 0, cs:cs + 64], rhs_bd, True, True,
                                 reads=[bk_b, bd_b], writes=[psb[p1[fc // 2]]])
                            S.mm(ps[p2[fc // 2]][0:64, co:co + 256], bk[:, fc, 1, cs:cs + 64], rhs_bd, True, True,
                                 reads=[bk_b, bd_b], writes=[psb[p2[fc // 2]]])
                            S.mm(ps[p3][0:64, fc * 128:(fc + 1) * 128], ar[:, fc, 0, cs:cs + 64], bt_bd[:, fc, :, cs:cs + 64],
                                 True, True, reads=[ar_b, bd_b], writes=[psb[p3]])
                        for hf in range(2):
                            dve(lambda en: en.tensor_tensor(out=A1[:, hf * 4:(hf + 1) * 4, :, :].rearrange("p h a t -> p (h a t)"),
                                                            in0=ps[p1[hf]][0:64, :], in1=mask2, op=ALU.mult),
                                [psb[p1[hf]], cstb_], [A1_b])
                            dve(lambda en: en.tensor_tensor(out=A2[:, hf * 4:(hf + 1) * 4, :, :].rearrange("p h a t -> p (h a t)"),
                                                            in0=ps[p2[hf]][0:64, :], in1=mask2, op=ALU.mult),
                                [psb[p2[hf]], cstb_], [A2_b])
                        dve(lambda en: en.tensor_tensor(out=Nj[:, 0, :, :].rearrange("p h t -> p (h t)"), in0=ps[p3][0:64, :],
                                                        in1=mask3, op=ALU.mult), [psb[p3], cstb_], [Nj_b[0]])
                        act(lambda en: en.copy(Mj[:, 0, :, :], A1[:, :, 0, :]), [A1_b], [Mj_b[0]])
                        dve(lambda en: en.tensor_tensor(out=Pj[:, 0, :, :].rearrange("p h t -> p (h t)"),
                                                        in0=Mj[:, 0, :, :].rearrange("p h t -> p (h t)"), in1=I8, op=ALU.add),
                            [Mj_b[0], cstb_], [Pj_b[0]])
                        cur = 0
                        pc = 0
                        for j in range(1, 6):
                            nxt = 1 - cur
                            pn = self.bank()
                            for h in range(8):
                                S.mm(ps[pn][0:64, h * 64:(h + 1) * 64], Mj[:, cur, h, :], Nj[:, cur, h, :], True, True,
                                     reads=[Mj_b[cur], Nj_b[cur]], writes=[psb[pn]])
                            if j < 5:
                                pm = self.bank()
                                for h in range(8):
                                    S.mm(ps[pm][0:64, h * 64:(h + 1) * 64], Nj[:, cur, h, :], Mj[:, cur, h, :], True, True,
                                         reads=[Mj_b[cur], Nj_b[cur]], writes=[psb[pm]])
                            self.evac("act", Nj[:, nxt, :, :].rearrange("p h t -> p (h t)"), ps[pn][0:64, :], [psb[pn]], [Nj_b[nxt]])
                            if j < 5:
                                self.evac("dve", Mj[:, nxt, :, :].rearrange("p h t -> p (h t)"), ps[pm][0:64, :], [psb[pm]],
                                          [Mj_b[nxt]])
                            pp = self.bank()
                            for h in range(8):
                                S.mm(ps[pp][0:64, h * 64:(h + 1) * 64], Nj[:, nxt, h, :], Pj[:, pc, h, :], True, True,
                                     reads=[Nj_b[nxt], Pj_b[pc]], writes=[psb[pp]])
                            dve(lambda en: en.tensor_tensor(out=Pj[:, 1 - pc, :, :].rearrange("p h t -> p (h t)"),
                                                            in0=ps[pp][0:64, :],
                                                            in1=Pj[:, pc, :, :].rearrange("p h t -> p (h t)"), op=ALU.add),
                                [psb[pp], Pj_b[pc]], [Pj_b[1 - pc]])
                            pc = 1 - pc
                            cur = nxt
                        TT = Pj[:, pc, :, :]
                        TT_b = Pj_b[pc]
                        pa = self.bank()
                        pw = self.bank()
                        for h in range(8):
                            fc, hb = h // 2, (h % 2) * 64
                            S.mm(ps[pa][hb:hb + 64, fc * 64:(fc + 1) * 64], At[:, h * 64:(h + 1) * 64], TT[:, h, :], True, True,
                                 reads=[tokm_b, TT_b], writes=[psb[pa]])
                            S.mm(ps[pw][0:64, h * 64:(h + 1) * 64], A2[:, h, 0, :], Vt[:, h * 64:(h + 1) * 64], True, True,
                                 reads=[A2_b, tokm_b], writes=[psb[pw]])
                        self.evac("act", AhT[:, :, :].rearrange("p f t -> p (f t)"), ps[pa][:, 0:256], [psb[pa]], [AhT_b])
                        self.evac("dve", W2[:, :, :].rearrange("p h v -> p (h v)"), ps[pw][0:64, :], [psb[pw]], [W2_b])
                        pu = self.bank()
                        for h in range(8):
                            S.mm(ps[pu][0:64, h * 64:(h + 1) * 64], TT[:, h, :], W2[:, h, :], True, True,
                                 reads=[TT_b, W2_b], writes=[psb[pu]])
                        self.evac("act", Uh[:, :, :].rearrange("p h v -> p (h v)"), ps[pu][0:64, :], [psb[pu]], [Uh_b])
                        pu2 = self.bank()
                        for fc in range(4):
                            S.mm(ps[pu2][0:64, fc * 128:(fc + 1) * 128], AhT[:, fc, :], Sbd[:, fc, :, :], True, True,
                                 reads=[AhT_b, S_b], writes=[psb[pu2]])
                        dve(lambda en: en.tensor_tensor(out=Ub[:, :, :].rearrange("p h v -> p (h v)"), in0=ps[pu2][0:64, :],
                                                        in1=Uh[:, :, :].rearrange("p h v -> p (h v)"), op=ALU.add),
                            [psb[pu2], Uh_b], [Ub_b])
                        py = self.bank()
                        pS = self.bank()
                        for fc in range(4):
                            S.mm(ps[py][0:64, fc * 128:(fc + 1) * 128], ar[:, fc, 1, cs:cs + 64], Sbd[:, fc, :, :], True, False,
                                 reads=[ar_b, S_b], writes=[psb[py]])
                            for h in (2 * fc, 2 * fc + 1):
                                yo = ps[py][0:64, h * 64:(h + 1) * 64]
                                S.mm(yo, A1[:, h, 1, :], Ub[:, h, :], False, False, reads=[A1_b, Ub_b], writes=[psb[py]])
                                S.mm(yo, A2[:, h, 1, :], Vt[:, h * 64:(h + 1) * 64], False, h == 2 * fc + 1,
                                     reads=[A2_b, tokm_b], writes=[psb[py]])
                        for h in range(8):
                            fc, hb = h // 2, (h % 2) * 64
                            so = ps[pS][hb:hb + 64, fc * 64:(fc + 1) * 64]
                            S.mm(so, Bt[:, h * 64:(h + 1) * 64], Ub[:, h, :], True, False, reads=[tokm_b, Ub_b], writes=[psb[pS]])
                            S.mm(so, Kt[:, h * 64:(h + 1) * 64], Vt[:, h * 64:(h + 1) * 64], False, True, reads=[tokm_b],
                                 writes=[psb[pS]])
                        for fc in range(4):
                            dve(lambda en: en.scalar_tensor_tensor(out=S32[:, fc, :], in0=S32[:, fc, :], scalar=gC[:, fc, cg:cg + 1],
                                                                   in1=ps[pS][:, fc * 64:(fc + 1) * 64], op0=ALU.mult, op1=ALU.add),
                                [psb[pS], gC_b, S_b], [S_b])
                        for h2 in range(2):
                            act(lambda en: en.copy(Sbd[h2 * 64:(h2 + 1) * 64, :, h2, :], S32[h2 * 64:(h2 + 1) * 64, :, :]),
                                [S_b], [S_b])
                        act(lambda en: en.copy(Y32[:, :, :].rearrange("p h v -> p (h v)"), ps[py][0:64, :]), [psb[py]], [Y_b])
                        act(lambda en: en.activation(out=Ysq[:], in_=Y32[:], func=AF.Square), [Y_b], [Ysq_b])
                        dve(lambda en: en.tensor_reduce(out=st8[:, 0, :], in_=Y32[:], axis=AX.X, op=ALU.add), [Y_b], [st8_b])
                        dve(lambda en: en.tensor_reduce(out=st8[:, 1, :], in_=Ysq[:], axis=AX.X, op=ALU.add), [Ysq_b, st8_b], [st8_b])
                        dve(lambda en: en.tensor_scalar(st8[:, 0, :], st8[:, 0, :], 1.0 / 64, None, ALU.mult), [st8_b], [st8_b])
                        dve(lambda en: en.tensor_tensor(out=st8[:, 2, :], in0=st8[:, 0, :], in1=st8[:, 0, :], op=ALU.mult),
                            [st8_b], [st8_b])
                        dve(lambda en: en.scalar_tensor_tensor(out=st8[:, 3, :], in0=st8[:, 1, :], scalar=1.0 / 64, in1=st8[:, 2, :],
                                                               op0=ALU.mult, op1=ALU.subtract), [st8_b], [st8_b])
                        act(lambda en: en.activation(out=st8[:, 3, :], in_=st8[:, 3, :], func=AF.Sqrt,
                                                     bias=self.eps_col(RW_GN_EPS)[0:64, :]), [st8_b, cstb_], [st8_b])
                        dve(lambda en: en.reciprocal(st8[:, 3, :], st8[:, 3, :]), [st8_b], [st8_b])
                        dve(lambda en: en.tensor_tensor(out=Y32[:], in0=Y32[:],
                                                        in1=st8[:, 0, :].unsqueeze(2).to_broadcast([64, 8, 64]), op=ALU.subtract),
                            [Y_b, st8_b], [Y_b])
                        dve(lambda en: en.tensor_tensor(out=Y32[:], in0=Y32[:],
                                                        in1=st8[:, 3, :].unsqueeze(2).to_broadcast([64, 8, 64]), op=ALU.mult),
                            [Y_b, st8_b], [Y_b])
                        pt = self.bank()
                        Yf = Y32[:, :, :].rearrange("p h v -> p (h v)")
                        for fc in range(4):
                            S.op("pe", lambda en: en.transpose(ps[pt][:, fc * 64:(fc + 1) * 64], Yf[:, fc * 128:(fc + 1) * 128],
                                                               ident32), reads=[Y_b, cstb_], writes=[psb[pt]])
                        act(lambda en: en.copy(ynT[:, :, cs:cs + 64], ps[pt][:, 0:256].rearrange("p (f t) -> p f t", t=64)),
                            [psb[pt]], [ynT_b])
                    for fc in range(4):
                        dve(lambda en: en.tensor_scalar(ynT[:, fc, :], ynT[:, fc, :], self.col(l, "rw_ln_g", fc),
                                                        self.col(l, "rw_ln_b", fc), ALU.mult, ALU.add),
                            [ynT_b, ynT_g[0], ynT_g[1], cstb_], [ynT_b, ynT_g[0], ynT_g[1]])
                        dve(lambda en: en.tensor_tensor(out=ynT[:, fc, :], in0=ynT[:, fc, :], in1=bon[:, fc, :], op=ALU.add),
                            [ynT_b, bon_b], [ynT_b])
                        dve(lambda en: en.tensor_tensor(out=yrwT[:, fc, t0:t0 + TW], in0=ynT[:, fc, :], in1=gT[:, fc, :], op=ALU.mult),
                            [ynT_b, gT_b], [yrw_b])
                    S.barrier()

    def phase_output(self):
        S = self.S
        nc = self.nc
        ones = self.C("ones")
        ident = self.C("ident", bf=False)
        with ExitStack() as _es:
            oh = _es.enter_context(self.tmp("oh", [128, DC, 512], F32))
            osq = _es.enter_context(self.tmp("osq", [128, DC, 512], BF16))
            orr = _es.enter_context(self.tmp("orr", [128, 512], F32))
            oo = _es.enter_context(self.tmp("oo", [128, 2, D], F32))
            oh_b = Buf()
            osq_b = Buf()
            or_b = Buf()
            oo_b = [Buf(), Buf()]
            out_b = Buf()
            for tt in range(NTT):
                pb = 0
                S.dma("sp", oh[:], self.hT.rearrange("d p t -> p d t")[:, :, tt * 512:(tt + 1) * 512],
                      reads=[self.hT_b[dc][tt] for dc in range(DC)], writes=[oh_b])
                S.op("act", lambda en: en.activation(out=osq[:], in_=oh[:], func=AF.Square), reads=[oh_b], writes=[osq_b])
                for dc in range(DC):
                    S.mm(self.ps[pb][:, :], ones, osq[:, dc, :], dc == 0, dc == DC - 1, reads=[osq_b, self.cst_b],
                         writes=[self.ps_b[pb]])
                S.op("act", lambda en: en.activation(out=orr[:], in_=self.ps[pb][:, :], func=AF.Sqrt, scale=1.0 / D,
                                                     bias=self.eps_col(RMS_EPS)),
                     reads=[self.ps_b[pb], self.cst_b], writes=[or_b])
                S.op("dve", lambda en: en.reciprocal(orr[:], orr[:]), reads=[or_b], writes=[or_b])
                for dc in range(DC):
                    S.op("dve", lambda en, dc=dc: en.scalar_tensor_tensor(
                        out=oh[:, dc, :], in0=oh[:, dc, :], scalar=self.col(0, "norm_out", dc), in1=orr[:],
                        op0=ALU.mult, op1=ALU.mult), reads=[oh_b, or_b, self.cst_b], writes=[oh_b])
                for ts in range(4):
                    k = ts % 2
                    for g in range(4):
                        pb2 = 1 + (ts * 4 + g) % 7
                        for jj in range(4):
                            dc = g * 4 + jj
                            S.op("pe", lambda en, dc=dc, jj=jj, pb2=pb2: en.transpose(
                                self.ps[pb2][:, jj * 128:(jj + 1) * 128], oh[:, dc, ts * 128:(ts + 1) * 128], ident),
                                reads=[oh_b, self.cst_b], writes=[self.ps_b[pb2]])
                        dst = oo[:, k, g * 512:(g + 1) * 512]
                        if g % 2 == 0:
                            S.op("act", lambda en, dst=dst, pb2=pb2: en.copy(dst, self.ps[pb2][:, :]),
                                 reads=[self.ps_b[pb2]], writes=[oo_b[k]])
                        else:
                            S.op("dve", lambda en, dst=dst, pb2=pb2: en.tensor_copy(dst, self.ps[pb2][:, :]),
                                 reads=[self.ps_b[pb2]], writes=[oo_b[k]])
                    r0 = tt * 512 + ts * 128
                    S.dma("sp", self.out[r0:r0 + 128, :], oo[:, k, :], reads=[oo_b[k]], writes=[out_b])
            S.barrier()


def _colpack(inp):
    cp = np.zeros((NL, 128, NCP), np.float32)

    def put(name, vec, l):
        v = np.asarray(vec, np.float32).reshape(-1, 128).T
        cp[l, :, CP[name]:CP[name] + v.shape[1]] = v

    for l in range(NL):
        put("norm_mix", inp["norm_mix"][l], l)
        put("norm_ffn", inp["norm_ffn"][l], l)
        put("gate_b", inp["gate_b"][l].reshape(-1), l)
        put("rw_mu", inp["rw_mu"][l], l)
        for n in ("rw_w0", "rw_a0", "rw_kk", "rw_ka", "rw_ln_g", "rw_ln_b"):
            put(n, inp[n][l], l)
        put("rw_rk", inp["rw_rk"][l].reshape(-1), l)
        put("norm_out", inp["norm_out"], l)
    return cp


def _tile_w(w):
    w = np.asarray(w, dtype=np.float32)
    lead = w.shape[:-2]
    K, N = w.shape[-2:]
    w = w.reshape(lead + (K // 128, 128, N // 128, 128))
    nl = len(lead)
    perm = tuple(range(nl)) + (nl + 2, nl + 1, nl + 0, nl + 3)
    return np.ascontiguousarray(w.transpose(perm)).reshape(lead + (N // 128, 128, K))


def make_in_map(inp, b):
    m = {"x": np.ascontiguousarray(inp["x"][b]), "cst": make_consts(), "cpk": _colpack(inp)}
    for n in ("gm_ln_g", "gm_ln_b", "gm_ws", "rw_w2", "rw_a2", "rw_g2", "router_w", "router_b"):
        m[n] = np.ascontiguousarray(inp[n], dtype=np.float32)
    for n in ("w_in", "p_gm", "p_rw", "p_sb", "w_o", "ffn_w1", "ffn_w3", "ffn_w2", "moe_w1", "moe_w3", "moe_w2"):
        m[n] = _tile_w(inp[n])
    m["gm_bs"] = np.ascontiguousarray(inp["gm_bs"], dtype=np.float32).reshape(NL, 1024)
    return m


_NC_CACHE = {}


def kernel(**inputs):
    inp = {k: np.asarray(v) for k, v in inputs.items()}
    if "nc" not in _NC_CACHE:
        _NC_CACHE["nc"] = Builder().build()
    nc = _NC_CACHE["nc"]
    base = make_in_map(inp, 0)
    in_maps = []
    for b in range(8):
        m = dict(base)
        m["x"] = np.ascontiguousarray(inp["x"][b], dtype=np.float32)
        in_maps.append(m)
    res = run_bass_kernel_spmd(nc, in_maps, core_ids=list(range(8)))
    return np.stack([r["out"] for r in res.results], axis=0).astype(np.float32)
```
